# Optimizing a Trainium2 kernel written in Bass

```python
import jax, jax.numpy as jnp
from jax import lax
import numpy as np

D_MODEL = 2048
BATCH = 4
SEQ = 4096
DEPTH = 2

MEM_LEN = 256
EPS = 1e-6
N_BRANCH = 3
BRANCH_WIDTH = 1024

CHUNK = 128
A_GROUPS = 8
A_GROUP_DIM = 128
A_WIDTH = A_GROUPS * A_GROUP_DIM

WINDOW = 128
BLOCK = 128
B_HEADS = 16
B_KV_HEADS = 4
B_HEAD_DIM = 64
B_Q_WIDTH = B_HEADS * B_HEAD_DIM
B_KV_WIDTH = B_KV_HEADS * B_HEAD_DIM

C_HEADS = 4
C_HEAD_DIM = 256
C_WIDTH = C_HEADS * C_HEAD_DIM

SPLIT_POINTS = (
    A_WIDTH,
    2 * A_WIDTH,
    2 * A_WIDTH + B_Q_WIDTH,
    2 * A_WIDTH + B_Q_WIDTH + B_KV_WIDTH,
    2 * A_WIDTH + B_Q_WIDTH + 2 * B_KV_WIDTH,
    2 * A_WIDTH + B_Q_WIDTH + 2 * B_KV_WIDTH + C_WIDTH,
)
IN_WIDTH = 2 * A_WIDTH + B_Q_WIDTH + 2 * B_KV_WIDTH + C_WIDTH + N_BRANCH * D_MODEL

N_GROUPS = 4
EXPERTS_PER_GROUP = 4
N_EXPERTS = N_GROUPS * EXPERTS_PER_GROUP
TOP_K = 2
D_FF_EXPERT = 512

kernel_name = "hybrid_gmlp_swa_sink_memxattn_hmoe"


def rms_norm(x, gain):
    xf = x.astype(jnp.float32)
    y = xf * lax.rsqrt(jnp.mean(xf * xf, axis=-1, keepdims=True) + EPS)
    return (y * gain.astype(jnp.float32)).astype(x.dtype)


def chunked_spatial_gating(u, v, v_gain, w_s, b_s):
    b, s, _ = u.shape
    n = s // CHUNK
    v = rms_norm(v.reshape(b, n, CHUNK, A_GROUPS, A_GROUP_DIM), v_gain)
    causal = jnp.tril(jnp.ones((CHUNK, CHUNK), dtype=bool))
    w = jnp.where(causal[None], w_s, 0.0).astype(v.dtype)
    mixed = jnp.einsum('gts,bnsgc->bntgc', w, v) + b_s.T[:, :, None].astype(v.dtype)
    out = u.reshape(b, n, CHUNK, A_GROUPS, A_GROUP_DIM) * mixed
    return out.reshape(b, s, A_WIDTH)


def sliding_window_sink_attention(q, k, v, sinks):
    b, s, _, _ = q.shape
    n = s // BLOCK
    rep = B_HEADS // B_KV_HEADS
    qb = q.reshape(b, n, BLOCK, B_KV_HEADS, rep, B_HEAD_DIM)
    kb = k.reshape(b, n, BLOCK, B_KV_HEADS, B_HEAD_DIM)
    vb = v.reshape(b, n, BLOCK, B_KV_HEADS, B_HEAD_DIM)

    def with_prev(t):
        prev = jnp.pad(t[:, :-1], ((0, 0), (1, 0), (0, 0), (0, 0), (0, 0)))
        return jnp.concatenate([prev, t], axis=2)

    kw, vw = with_prev(kb), with_prev(vb)
    scores = jnp.einsum('bnqkrd,bnskd->bnkrqs', qb, kw,
                        preferred_element_type=jnp.float32) * (B_HEAD_DIM ** -0.5)
    n_idx = jnp.arange(n)[:, None, None]
    qi = jnp.arange(BLOCK)[None, :, None]
    kj = jnp.arange(2 * BLOCK)[None, None, :]
    rel = BLOCK + qi - kj
    mask = (rel >= 0) & (rel < WINDOW) & ((n_idx > 0) | (kj >= BLOCK))
    scores = jnp.where(mask[None, :, None, None], scores, -jnp.inf)
    sink = sinks.astype(jnp.float32).reshape(1, 1, B_KV_HEADS, rep, 1, 1)
    m = jnp.maximum(jnp.max(scores, axis=-1, keepdims=True), sink)
    p = jnp.exp(scores - m)
    denom = jnp.sum(p, axis=-1, keepdims=True) + jnp.exp(sink - m)
    probs = (p / denom).astype(v.dtype)
    out = jnp.einsum('bnkrqs,bnskd->bnqkrd', probs, vw)
    return out.reshape(b, s, B_Q_WIDTH)


def memory_cross_attention(q, k, v):
    b, s, _, _ = q.shape
    scores = jnp.einsum('bshd,bmhd->bhsm', q, k,
                        preferred_element_type=jnp.float32) * (C_HEAD_DIM ** -0.5)
    probs = jax.nn.softmax(scores, axis=-1).astype(v.dtype)
    return jnp.einsum('bhsm,bmhd->bshd', probs, v).reshape(b, s, C_WIDTH)


def hierarchical_moe(h, w_rg, b_rg, w_re, b_re, w_gate, w_up, w_down):
    b, s, d = h.shape
    t = h.reshape(b * s, d)
    group_logits = (t @ w_rg).astype(jnp.float32) + b_rg.astype(jnp.float32)
    group_probs = jax.nn.softmax(group_logits, axis=-1)
    g = jnp.argmax(group_logits, axis=-1)
    p_g = jnp.take_along_axis(group_probs, g[:, None], axis=-1)
    expert_logits = ((t @ w_re).astype(jnp.float32) + b_re.astype(jnp.float32)
                     ).reshape(-1, N_GROUPS, EXPERTS_PER_GROUP)
    in_group = jnp.take_along_axis(expert_logits, g[:, None, None], axis=1)[:, 0]
    top_vals, top_idx = lax.top_k(in_group, TOP_K)
    weights = jax.nn.softmax(top_vals, axis=-1) * p_g
    expert_id = g[:, None] * EXPERTS_PER_GROUP + top_idx
    gates = jnp.sum(jax.nn.one_hot(expert_id, N_EXPERTS, dtype=jnp.float32)
                    * weights[..., None], axis=1)
    hidden = (jax.nn.silu(jnp.einsum('td,edf->tef', t, w_gate))
              * jnp.einsum('td,edf->tef', t, w_up))
    hidden = hidden * gates[..., None].astype(hidden.dtype)
    out = jnp.einsum('tef,efd->td', hidden, w_down)
    return out.reshape(b, s, d)


def setup_inputs(seed: int = 0) -> dict:
    key = jax.random.key(seed)
    ks = jax.random.split(key, 26)

    def nrm(k, shape, scale):
        return jax.random.normal(k, shape, dtype=jnp.float32) * scale

    def gain(k, shape):
        return 1.0 + 0.05 * jax.random.normal(k, shape, dtype=jnp.float32)

    L = DEPTH
    return {
        "x": nrm(ks[0], (BATCH, SEQ, D_MODEL), 1.0),
        "mem": nrm(ks[1], (BATCH, MEM_LEN, D_MODEL), 1.0),
        "norm_mix": gain(ks[2], (L, D_MODEL)),
        "norm_mem": gain(ks[3], (L, D_MODEL)),
        "norm_ffn": gain(ks[4], (L, D_MODEL)),
        "w_in": nrm(ks[5], (L, D_MODEL, IN_WIDTH), D_MODEL ** -0.5),
        "v_gain": gain(ks[6], (L, A_GROUPS, A_GROUP_DIM)),
        "w_spatial": nrm(ks[7], (L, A_GROUPS, CHUNK, CHUNK), CHUNK ** -0.5),
        "b_spatial": 1.0 + 0.02 * jax.random.normal(ks[8], (L, A_GROUPS, CHUNK), dtype=jnp.float32),
        "q_gain_b": gain(ks[9], (L, B_HEAD_DIM)),
        "k_gain_b": gain(ks[10], (L, B_HEAD_DIM)),
        "sinks": nrm(ks[11], (L, B_HEADS), 0.5),
        "q_gain_c": gain(ks[12], (L, C_HEAD_DIM)),
        "k_gain_c": gain(ks[13], (L, C_HEAD_DIM)),
        "w_mem_kv": nrm(ks[14], (L, D_MODEL, 2 * C_WIDTH), D_MODEL ** -0.5),
        "w_branch": nrm(ks[15], (L, N_BRANCH, BRANCH_WIDTH, D_MODEL), BRANCH_WIDTH ** -0.5),
        "w_out": nrm(ks[16], (L, D_MODEL, D_MODEL), D_MODEL ** -0.5),
        "w_router_group": nrm(ks[17], (L, D_MODEL, N_GROUPS), D_MODEL ** -0.5),
        "b_router_group": nrm(ks[18], (L, N_GROUPS), 0.01),
        "w_router_expert": nrm(ks[19], (L, D_MODEL, N_EXPERTS), D_MODEL ** -0.5),
        "b_router_expert": nrm(ks[20], (L, N_EXPERTS), 0.01),
        "w_gate_e": nrm(ks[21], (L, N_EXPERTS, D_MODEL, D_FF_EXPERT), D_MODEL ** -0.5),
        "w_up_e": nrm(ks[22], (L, N_EXPERTS, D_MODEL, D_FF_EXPERT), D_MODEL ** -0.5),
        "w_down_e": nrm(ks[23], (L, N_EXPERTS, D_FF_EXPERT, D_MODEL), D_FF_EXPERT ** -0.5),
    }


def reference(x, mem, norm_mix, norm_mem, norm_ffn, w_in, v_gain, w_spatial, b_spatial,
              q_gain_b, k_gain_b, sinks, q_gain_c, k_gain_c, w_mem_kv, w_branch, w_out,
              w_router_group, b_router_group, w_router_expert, b_router_expert,
              w_gate_e, w_up_e, w_down_e):
    b, s, _ = x.shape
    m_len = mem.shape[1]
    for layer in range(DEPTH):
        h = rms_norm(x, norm_mix[layer])
        proj = h @ w_in[layer]
        u_a, v_a, q_b, k_b, v_b, q_c, gate_logits = jnp.split(proj, SPLIT_POINTS, axis=-1)

        y_a = chunked_spatial_gating(jax.nn.gelu(u_a, approximate=False),
                                     jax.nn.gelu(v_a, approximate=False),
                                     v_gain[layer], w_spatial[layer], b_spatial[layer])

        qb = rms_norm(q_b.reshape(b, s, B_HEADS, B_HEAD_DIM), q_gain_b[layer])
        kb = rms_norm(k_b.reshape(b, s, B_KV_HEADS, B_HEAD_DIM), k_gain_b[layer])
        vb = v_b.reshape(b, s, B_KV_HEADS, B_HEAD_DIM)
        y_b = sliding_window_sink_attention(qb, kb, vb, sinks[layer])

        mem_h = rms_norm(mem, norm_mem[layer])
        k_c, v_c = jnp.split(mem_h @ w_mem_kv[layer], 2, axis=-1)
        kc = rms_norm(k_c.reshape(b, m_len, C_HEADS, C_HEAD_DIM), k_gain_c[layer])
        vc = v_c.reshape(b, m_len, C_HEADS, C_HEAD_DIM)
        qc = rms_norm(q_c.reshape(b, s, C_HEADS, C_HEAD_DIM), q_gain_c[layer])
        y_c = memory_cross_attention(qc, kc, vc)

        branches = jnp.stack([y_a, y_b, y_c], axis=2)
        widened = jnp.einsum('bsnw,nwd->bsnd', branches, w_branch[layer])
        gates = jax.nn.sigmoid(gate_logits.reshape(b, s, N_BRANCH, D_MODEL))
        merged = jnp.sum(gates * widened, axis=2)
        x = x + merged @ w_out[layer]

        h2 = rms_norm(x, norm_ffn[layer])
        x = x + hierarchical_moe(h2, w_router_group[layer], b_router_group[layer],
                                 w_router_expert[layer], b_router_expert[layer],
                                 w_gate_e[layer], w_up_e[layer], w_down_e[layer])
    return x
```

```python
import numpy as np
import ml_dtypes
from contextlib import ExitStack
import concourse.bass as bass
import concourse.mybir as mybir
from concourse.bass_utils import run_bass_kernel_spmd

F32 = mybir.dt.float32
BF16 = mybir.dt.bfloat16
AF = mybir.ActivationFunctionType
ALU = mybir.AluOpType
AX = mybir.AxisListType

D = 2048
NKC = 16
L = 2
IN_W = 10752
NB = 6
TT = NB * 128
HT = TT // 2
NT = 3
TOK = NT * TT
EPS = 1e-6
NEG = -30000.0
NW = 2
SAME_ENG_SYNC = True


class Sched:
    ENGS = ("pe", "act", "dve", "pool", "sp")

    def __init__(self):
        self.ops = {e: [] for e in self.ENGS}
        self.last_w = {}
        self.readers = {}
        self.waited = {e: {} for e in self.ENGS}
        self.dma_val = {}
        self.signal = set()
        self.halted = False

    def _need(self, eng, tok, waits):
        src, v = tok
        if src == eng and (eng == "pe" or not SAME_ENG_SYNC):
            return
        if self.waited[eng].get(src, -1) >= v:
            return
        self.waited[eng][src] = v
        waits.append(tok)
        if src in self.ENGS:
            self.signal.add((src, v))

    REGIONS = ("SC", "Y", "R", "kvcR")

    def op(self, eng, fn, r=(), w=(), dma=None, fence=False, force=False):
        if self.halted and not force:
            return None
        if not fence:
            mv = [k for k in w if k in self.REGIONS]
            if mv:
                w = [k for k in w if k not in self.REGIONS]
                r = list(r) + mv
        waits = []
        for k in r:
            t = self.last_w.get(k)
            if t is not None:
                self._need(eng, t, waits)
            if isinstance(k, tuple) and k[0] == "ps":
                for src, t in self.readers.get(k, {}).items():
                    if src != eng:
                        self._need(eng, t, waits)
        for k in w:
            t = self.last_w.get(k)
            if t is not None:
                self._need(eng, t, waits)
            for t in self.readers.get(k, {}).values():
                self._need(eng, t, waits)
        idx = len(self.ops[eng])
        if dma is not None:
            prev = self.dma_val.get(dma, 0)
            if prev:
                self._need(eng, (dma, prev), waits)
            tok = (dma, prev + 16)
            self.dma_val[dma] = prev + 16
        else:
            tok = (eng, idx)
        self.ops[eng].append((fn, waits, dma))
        for k in r:
            self.readers.setdefault(k, {})[tok[0]] = tok
        for k in w:
            self.last_w[k] = tok
            self.readers[k] = {}
        return tok

    def emit(self, nc, block, sems):
        sigval = {}
        for e in self.ENGS:
            c = 0
            for i in range(len(self.ops[e])):
                if (e, i) in self.signal:
                    c += 1
                    sigval[(e, i)] = c
        ENGS = self.ENGS

        def run(e, eng):
            for i, (fn, waits, dma) in enumerate(self.ops[e]):
                for (src, v) in waits:
                    if src in ENGS:
                        eng.wait_ge(sems[src], sigval[(src, v)])
                    else:
                        eng.wait_ge(sems[src], v)
                ins = fn(eng)
                if ins is None:
                    continue
                if dma is not None:
                    ins.then_inc(sems[dma], 16)
                elif (e, i) in self.signal:
                    ins.then_inc(sems[e], 1)

        @block.tensor
        def _(eng):
            run("pe", eng)

        @block.scalar
        def _(eng):
            run("act", eng)

        @block.vector
        def _(eng):
            run("dve", eng)

        @block.gpsimd
        def _(eng):
            run("pool", eng)

        @block.sync
        def _(eng):
            run("sp", eng)


def build_program(layers, n_tiles=NT, taps=None, out_tokens_from=256, stop_after=None):
    taps = taps or {}
    nc = bass.Bass("TRN2", target_bir_lowering=False)
    S = Sched()

    def din(name, shape, dt=F32):
        return nc.dram_tensor(name, list(shape), dt, kind="ExternalInput").ap()

    x_in = din("x_in", [TOK, D])
    mem_in = din("mem_in", [256, D])
    norm_mix = din("norm_mix", [L, D])
    norm_mem = din("norm_mem", [L, D])
    norm_ffn = din("norm_ffn", [L, D])
    w_in = din("w_in", [L, D, IN_W])
    v_gain = din("v_gain", [L, 8, 128])
    w_spatial = din("w_spatial", [L, 8, 128, 128])
    b_spatial = din("b_spatial", [L, 8, 128])
    q_gain_b = din("q_gain_b", [L, 64])
    k_gain_b = din("k_gain_b", [L, 64])
    sinks = din("sinks", [L, 16])
    q_gain_c = din("q_gain_c", [L, 256])
    k_gain_c = din("k_gain_c", [L, 256])
    w_mem_kv = din("w_mem_kv", [L, D, D])
    w_branch = din("w_branch", [L, 3, 1024, D])
    w_out = din("w_out", [L, D, D])
    w_rg = din("w_router_group", [L, D, 4])
    b_rg = din("b_router_group", [L, 4])
    w_re = din("w_router_expert", [L, D, 16])
    b_re = din("b_router_expert", [L, 16])
    w_gate_e = din("w_gate_e", [L, 16, D, 512])
    w_up_e = din("w_up_e", [L, 16, D, 512])
    w_down_e = din("w_down_e", [L, 16, 512, D])
    c_ident = din("c_ident", [128, 128])
    c_tril = din("c_tril", [128, 128])
    c_mb = din("c_mb", [4, 128, 128], BF16)
    n_out_tok = n_tiles * TT - out_tokens_from
    out = nc.dram_tensor("out", [n_out_tok, D], F32, kind="ExternalOutput").ap()
    kvscr = nc.dram_tensor("kvscr", [L, 128, 4096], BF16, kind="Internal").ap()
    tap_aps = {k: nc.dram_tensor("tap_" + k, list(shp), dt, kind="ExternalOutput").ap() for k, (shp, dt) in taps.items()}

    es = ExitStack()
    with es:
        def sb(name, shape, dt):
            return es.enter_context(nc.sbuf_tensor(name, list(shape), dt))

        xT = sb("xT", [128, NKC, TT], F32)
        hT = sb("hT", [128, NKC, TT], BF16)
        Yr = sb("Yr", [128, 24 * TT], BF16)
        Rr = sb("Rr", [128, 16 * TT], BF16)
        wbuf = [sb("wbuf%d" % i, [128, NKC * 512], BF16) for i in range(NW)]
        SCb = sb("SC", [128, 10752], BF16)
        kvc = sb("kvc", [128, 4096], BF16)
        identf = sb("identf", [128, 128], F32)
        identb = sb("identb", [128, 128], BF16)
        onesb = sb("onesb", [128, 128], BF16)
        bdiag = sb("bdiag", [128, 128], BF16)
        onesf = sb("onesf", [128, 128], F32)
        mbt = sb("mbt", [128, 4, 128], BF16)
        gmix = sb("gmix", [128, L, NKC], F32)
        gffn = sb("gffn", [128, L, NKC], F32)
        gmem = sb("gmem", [128, L, NKC], F32)
        gqb = sb("gqb", [128, L], F32)
        gkb = sb("gkb", [128, L], F32)
        esink = sb("esink", [128, L, 8], F32)
        gqc = sb("gqc", [128, L, 2], F32)
        gkc = sb("gkc", [128, L, 2], F32)
        WmT = sb("WmT", [128, L, 8, 128], BF16)
        wr = sb("wr", [128, NKC, 20], BF16)
        rb = sb("rb", [128, 20], F32)
        gates = sb("gates", [128, NB, 16], F32)
        rt = sb("rt", [128, 96], F32)
        savekv = sb("savekv", [128, L, 768], BF16)
        dummy = sb("dmy0", [128, 8], F32)
        ps = [es.enter_context(nc.psum_tensor("ps%d" % i, [128, 512], F32)) for i in range(8)]

        Y4 = Yr[:, :].rearrange("p (c t) -> p c t", t=TT)
        yA, yB, yC = Y4[:, 0:8], Y4[:, 8:16], Y4[:, 16:24]
        hidT = Y4[:, 0:16]
        Ystage = Yr[:, :].bitcast(F32)
        stage = [Ystage[:, i * 2048:(i + 1) * 2048] for i in range(4)]
        mT = Rr[:, :].rearrange("p (c t) -> p c t", t=TT)
        vn = Rr[:, 0:NB * 1024].rearrange("p (b c) -> p b c", c=1024)
        kn2 = Rr[:, 0:4 * 896].rearrange("p (k t) -> p k t", t=896)
        vb = Rr[:, 4 * 896:4 * 896 + 7 * 256].rearrange("p (s c) -> p s c", c=256)
        qbuf = Rr[:, 8 * TT:16 * TT].rearrange("p (c t) -> p c t", t=TT)
        kcT = kvc[:, 0:2048].rearrange("p (c m) -> p c m", m=256)
        vc = kvc[:, 2048:4096].rearrange("p (m c) -> p m c", c=1024)
        SCf = SCb[:, :].bitcast(F32)
        kvcf = kvc[:, :].bitcast(F32)
        RMS0 = 4608

        def CK(name):
            if stop_after == name:
                S.halted = True

        bank_ctr = [0]

        def bank():
            i = bank_ctr[0] % 8
            bank_ctr[0] += 1
            return i

        dsem_ctr = [0]

        def dsem():
            i = dsem_ctr[0] % 6
            dsem_ctr[0] += 1
            return "d%d" % i

        def MM(out, lhsT, rhs, start, stop, r, w, **kw):
            return S.op("pe", lambda e: e.matmul(out, lhsT=lhsT, rhs=rhs, start=start, stop=stop, **kw), r, w)

        def TR(out, in_, ident, r, w):
            return S.op("pe", lambda e: e.transpose(out=out, in_=in_, identity=ident), r, w)

        def ACTF(out, in_, func, r, w, **kw):
            return S.op("act", lambda e: e.activation(out=out, in_=in_, func=func, **kw), r, w)

        def TT_(eng, out, in0, in1, op, r, w):
            return S.op(eng, lambda e: e.tensor_tensor(out=out, in0=in0, in1=in1, op=op), r, w)

        def TS(eng, out, in0, s1, s2, op0, op1, r, w):
            if op1 is None:
                return S.op(eng, lambda e: e.tensor_scalar(out=out, in0=in0, scalar1=s1, scalar2=None, op0=op0), r, w)
            return S.op(eng, lambda e: e.tensor_scalar(out=out, in0=in0, scalar1=s1, scalar2=s2, op0=op0, op1=op1), r, w)

        def STT(out, in0, scalar, in1, op0, op1, r, w):
            return S.op("dve", lambda e: e.scalar_tensor_tensor(out=out, in0=in0, scalar=scalar, in1=in1, op0=op0, op1=op1), r, w)

        def CP(eng, out, in_, r, w):
            if eng == "act":
                return S.op("act", lambda e: e.copy(out=out, in_=in_), r, w)
            return S.op(eng, lambda e: e.tensor_copy(out=out, in_=in_), r, w)

        def RECIP(out, in_, r, w):
            return S.op("dve", lambda e: e.reciprocal(out=out, in_=in_), r, w)

        def RED(out, in_, op, r, w):
            return S.op("dve", lambda e: e.tensor_reduce(out=out, in_=in_, axis=AX.X, op=op), r, w)

        def DMA(q, out, in_, r, w, sem=None, **kw):
            return S.op(q, lambda e: e.dma_start(out=out, in_=in_, **kw), r, w, dma=sem or dsem())

        def FENCE(key, eng="dve"):
            return S.op(eng, lambda e: e.memset(dummy[:, 0:1], 0.0), r=(), w=[key, "dummy_" + eng], fence=True)

        def TAP(name, src, r):
            if name in tap_aps:
                DMA("sp", tap_aps[name], src, r=r, w=["tap_" + name])

        wctr = [0]

        def wload(src_ap, kparts, cols):
            i = wctr[0] % NW
            wctr[0] += 1
            view = wbuf[i][:, 0:kparts * cols].rearrange("p (k c) -> p k c", c=cols)
            DMA("pool", view, src_ap, r=(), w=[("w", i)], sem="w%d" % i)
            return view, ("w", i)

        def w_in_unit(l, c0, cols=512):
            return w_in[l].rearrange("(kc p) n -> p kc n", p=128)[:, :, c0:c0 + cols]

        DMA("sp", identf[:, :], c_ident[:, :], r=(), w=["identf"])
        DMA("sp", SCf[:, 0:128], c_tril[:, :], r=(), w=["tril", "SC"])
        DMA("sp", mbt[:, :, :], c_mb.rearrange("m k q -> k m q"), r=(), w=["mbt"])
        CP("dve", identb[:, :], identf[:, :], r=["identf"], w=["identb"])
        S.op("dve", lambda e: e.memset(onesb[:, :], 1.0), w=["onesb"])
        S.op("dve", lambda e: e.memset(onesf[:, :], 1.0), w=["onesf"])
        S.op("dve", lambda e: e.memset(bdiag[:, :], 0.0), w=["bdiag"])
        S.op("dve", lambda e: e.memset(bdiag[0:64, 0:64], 1.0), w=["bdiag"])
        S.op("dve", lambda e: e.memset(bdiag[64:128, 64:128], 1.0), w=["bdiag"])
        S.op("dve", lambda e: e.memset(savekv[:, :, :], 0.0), w=["savekv"])
        for (dst, src) in ((gmix, norm_mix), (gffn, norm_ffn), (gmem, norm_mem)):
            for l in range(L):
                DMA("sp", dst[:, l, :], src[l].rearrange("(kc p) -> p kc", p=128), r=(), w=["gains"],
                    allow_slow_non_contiguous=True)
        for l in range(L):
            for hf in range(2):
                DMA("sp", gqb[hf * 64:(hf + 1) * 64, l:l + 1], q_gain_b[l].rearrange("(d o) -> d o", o=1), r=(), w=["gains"])
                DMA("sp", gkb[hf * 64:(hf + 1) * 64, l:l + 1], k_gain_b[l].rearrange("(d o) -> d o", o=1), r=(), w=["gains"])
                DMA("sp", esink[hf * 64:(hf + 1) * 64, l, :],
                    sinks[l].rearrange("(c two) -> two c", two=2)[hf].partition_broadcast(64), r=(), w=["esink"],
                    allow_slow_non_contiguous=True)
            DMA("sp", gqc[:, l, :], q_gain_c[l].rearrange("(dc p) -> p dc", p=128), r=(), w=["gains"], allow_slow_non_contiguous=True)
            DMA("sp", gkc[:, l, :], k_gain_c[l].rearrange("(dc p) -> p dc", p=128), r=(), w=["gains"], allow_slow_non_contiguous=True)
        ACTF(esink[:, :, :], esink[:, :, :], AF.Exp, r=["esink"], w=["esink"])
        wsp = SCf[:, 128:128 + 1024].rearrange("p (g s) -> p g s", s=128)
        for l in range(L):
            DMA("sp", wsp, w_spatial[l].rearrange("g t s -> t g s"), r=(), w=["wsp", "SC"])
            TT_("dve", wsp, wsp, SCf[:, 0:128].unsqueeze(1).to_broadcast([128, 8, 128]), ALU.mult, r=["tril", "wsp"], w=["wsp", "SC"])
            for g4 in range(2):
                b = bank()
                for j in range(4):
                    g = g4 * 4 + j
                    TR(ps[b][:, j * 128:(j + 1) * 128], wsp[:, g, :], identf[:, :], r=["wsp", "SC", "identf"], w=[("ps", b)])
                CP("dve", WmT[:, l, g4 * 4:(g4 + 1) * 4, :], ps[b][:, :].rearrange("p (j t) -> p j t", t=128),
                   r=[("ps", b)], w=["WmT"])
        FENCE("SC")

        CK("params")
        def load_tokens_T(src_rows, nblk, dstT, dkey):
            for blk in range(nblk):
                st = stage[blk % 4]
                skey = ("stage", blk % 4)
                DMA("sp", st, src_rows[blk * 128:(blk + 1) * 128, :], r=["Y"], w=[skey])
                for k4 in range(4):
                    b = bank()
                    for j in range(4):
                        kc = k4 * 4 + j
                        TR(ps[b][:, j * 128:(j + 1) * 128], st[:, kc * 128:(kc + 1) * 128], identf[:, :],
                           r=[skey, "identf"], w=[("ps", b)])
                    CP("act" if k4 % 2 else "dve", dstT[:, k4 * 4:(k4 + 1) * 4, blk * 128:(blk + 1) * 128],
                       ps[b][:, :].rearrange("p (j t) -> p j t", t=128), r=[("ps", b)], w=[(dkey, blk * 128 // HT)])

        def rms_to_h(srcT, skey, gain_ap, halves, hw):
            rstd = SCf[:, 0:TT]
            sqr = [SCb[:, 2 * TT + i * HT:2 * TT + (i + 1) * HT] for i in range(3)]
            for hf in range(halves):
                sl = slice(hf * hw, (hf + 1) * hw)
                b = bank()
                for kc in range(NKC):
                    sq = sqr[kc % 3]
                    ACTF(sq[:, 0:hw], srcT[:, kc, sl], AF.Square, r=[(skey, hf)], w=[("sq", kc % 3), "SC"])
                    MM(ps[b][:, 0:hw], onesb[:, :], sq[:, 0:hw], kc == 0, kc == NKC - 1, r=[("sq", kc % 3), "onesb", "SC"], w=[("ps", b)])
                ACTF(rstd[:, sl], ps[b][:, 0:hw], AF.Sqrt, r=[("ps", b)], w=[("rstd", hf), "SC"], scale=1.0 / D, bias=EPS)
                RECIP(rstd[:, sl], rstd[:, sl], r=[("rstd", hf)], w=[("rstd", hf), "SC"])
                for kc in range(NKC):
                    STT(hT[:, kc, sl], srcT[:, kc, sl], gain_ap[:, kc:kc + 1], rstd[:, sl], ALU.mult, ALU.mult,
                        r=[(skey, hf), ("rstd", hf), "gains", "SC"], w=[("hT", hf)])
            FENCE("SC")

        deferred = []
        out_keys = []

        def defer(fn):
            deferred.append(fn)

        def flush(keep=0):
            while len(deferred) > keep:
                deferred.pop(0)()

        def fm_unit(wv, wkey, nchunks, ksz, rhsT, rkey, halves, hw, consumer):
            for c in range(nchunks):
                for hf in range(halves):
                    b = bank()
                    for k in range(ksz):
                        MM(ps[b][:, 0:hw], wv[:, k, c * 128:(c + 1) * 128], rhsT[:, k, hf * hw:(hf + 1) * hw], k == 0, k == ksz - 1,
                           r=[wkey, (rkey, hf)], w=[("ps", b)])
                    consumer(b, c, hf)

        def qk_norm_consumer(nsum, ones_ap, okey, invd, gain_fn, dst_fn, dkey, hw, scbase):
            st = {}
            cnt = [0]

            def consumer(b, c, hf):
                i = cnt[0]
                cnt[0] += 1
                slot = i % 4
                if any(slot in getattr(f, "slots", ()) for f in deferred):
                    flush()
                sqb = SCb[:, scbase + slot * hw: scbase + (slot + 1) * hw]
                qraw = SCf[:, (scbase + 4 * hw) // 2 + slot * hw:(scbase + 4 * hw) // 2 + (slot + 1) * hw]
                ACTF(sqb, ps[b][:, 0:hw], AF.Square, r=[("ps", b)], w=[("sqb", slot), "SC"])
                CP("dve", qraw, ps[b][:, 0:hw], r=[("ps", b)], w=[("qraw", slot), "SC"])
                st.setdefault(hf, []).append((c, slot))
                if len(st[hf]) == nsum:
                    group = st.pop(hf)

                    def fin(group=group, hf=hf):
                        b2 = bank()
                        for j, (c_, s_) in enumerate(group):
                            MM(ps[b2][:, 0:hw], ones_ap, SCb[:, scbase + s_ * hw: scbase + (s_ + 1) * hw], j == 0, j == len(group) - 1,
                               r=[("sqb", s_), okey, "SC"], w=[("ps", b2)])
                        rq = SCf[:, (scbase + 12 * hw) // 2 + (group[0][1] % 2) * hw:(scbase + 12 * hw) // 2 + (group[0][1] % 2 + 1) * hw]
                        rk = ("rq", group[0][1] % 2)
                        ACTF(rq, ps[b2][:, 0:hw], AF.Sqrt, r=[("ps", b2)], w=[rk, "SC"], scale=invd, bias=EPS)
                        RECIP(rq, rq, r=[rk], w=[rk, "SC"])
                        for (c_, s_) in group:
                            qr = SCf[:, (scbase + 4 * hw) // 2 + s_ * hw:(scbase + 4 * hw) // 2 + (s_ + 1) * hw]
                            STT(dst_fn(c_, hf), qr, gain_fn(c_), rq, ALU.mult, ALU.mult, r=[("qraw", s_), rk, "gains", "SC"], w=["R", dkey])
                    fin.slots = tuple(s_ for (_, s_) in group)
                    defer(fin)
                    flush(keep=1)
            return consumer

        def mem_prologue():
            load_tokens_T(mem_in, 2, xT, "xT")
            CK("mp_load")
            for l in range(L):
                rms_to_h(xT, "xT", gmem[:, l, :], 1, 256)
                CK("mp_rms")
                wkv = w_mem_kv[l].rearrange("(kc p) n -> p kc n", p=128)
                for u in range(2):
                    wv, wk = wload(wkv[:, :, u * 512:(u + 1) * 512], NKC, 512)
                    cons = qk_norm_consumer(2, onesb[:, :], "onesb", 1.0 / 256.0,
                                            lambda c, l=l: gkc[:, l, (c % 2):(c % 2) + 1],
                                            lambda c, hf, u=u: kcT[:, u * 4 + c, :], "kvc", 256, 0)
                    fm_unit(wv, wk, 4, NKC, hT, "hT", 1, 256, cons)
                flush()
                CK("mp_k")
                for u in range(2):
                    wv, wk = wload(wkv[:, :, 1024 + u * 512:1024 + (u + 1) * 512], NKC, 512)
                    for mb_ in range(2):
                        b = bank()
                        for k in range(NKC):
                            MM(ps[b][:, :], hT[:, k, mb_ * 128:(mb_ + 1) * 128], wv[:, k, :], k == 0, k == NKC - 1,
                               r=[wk, ("hT", 0)], w=[("ps", b)])
                        CP("act", vc[:, mb_, u * 512:(u + 1) * 512], ps[b][:, :], r=[("ps", b)], w=["kvc"])
                CK("mp_v")
                DMA("sp", kvscr[l], kvc[:, :], r=["kvc", "R"], w=[("kvscr", l)])
                FENCE("kvc")
                FENCE("SC")

        def mixer(ti, l, first_layer_in_launch):
            tok0 = ti * TT
            S.op("sp", lambda e: e.dma_start(out=kvc[:, :], in_=kvscr[l]), r=[("kvscr", l)], w=["kvc", "kvcR"], dma=dsem(), fence=True)
            if first_layer_in_launch:
                load_tokens_T(x_in[tok0:tok0 + TT, :], NB, xT, "xT")
                FENCE("Y")
            CK("load")
            rms_to_h(xT, "xT", gmix[:, l, :], 2, HT)
            if ti == 0 and l == layers[0]:
                TAP("hT", hT[:, :, :], r=[("hT", 0), ("hT", 1)])
            CK("rms")

            vgrep = SCf[:, 0:1024]
            brow = SCf[0:1, 1024:2048]
            DMA("sp", vgrep, v_gain[l].rearrange("g c -> (g c)").partition_broadcast(128), r=(), w=["SC", "vgrep"])
            DMA("sp", brow, b_spatial[l].rearrange("g t -> (g t)").unsqueeze(0), r=(), w=["SC", "brow"])

            def u_cons(u):
                def cons(b, c, hf):
                    ACTF(yA[:, u * 4 + c, hf * HT:(hf + 1) * HT], ps[b][:, 0:HT], AF.Gelu, r=[("ps", b)], w=["Y", ("yA", hf)])
                return cons
            for u in range(2):
                wv, wk = wload(w_in_unit(l, u * 512), NKC, 512)
                fm_unit(wv, wk, 4, NKC, hT, "hT", 2, HT, u_cons(u))
            CK("A_u")
            vg = [SCf[:, 2048 + i * 512:2048 + (i + 1) * 512] for i in range(2)]
            junk = SCb[:, 6144 + 1024:6144 + 1024 + 128]
            ssv = rt[:, 88:96]
            for u in range(2):
                wv, wk = wload(w_in_unit(l, 1024 + u * 512), NKC, 512)
                for blk in range(NB):
                    b = bank()
                    for k in range(NKC):
                        MM(ps[b][:, :], hT[:, k, blk * 128:(blk + 1) * 128], wv[:, k, :], k == 0, k == NKC - 1,
                           r=[wk, ("hT", blk // 3)], w=[("ps", b)])
                    i = (u * NB + blk) % 2
                    ACTF(vg[i], ps[b][:, :], AF.Gelu, r=[("ps", b)], w=[("vg", i), "SC"])
                    for g in range(4):
                        ACTF(junk, vg[i][:, g * 128:(g + 1) * 128], AF.Square, r=[("vg", i), "SC"], w=["junk", ("ssv", i)],
                             accum_out=ssv[:, i * 4 + g:i * 4 + g + 1])
                    ACTF(ssv[:, i * 4:(i + 1) * 4], ssv[:, i * 4:(i + 1) * 4], AF.Sqrt, r=[("ssv", i)], w=[("ssv", i)],
                         scale=1.0 / 128.0, bias=EPS)
                    RECIP(ssv[:, i * 4:(i + 1) * 4], ssv[:, i * 4:(i + 1) * 4], r=[("ssv", i)], w=[("ssv", i)])
                    for g in range(4):
                        gg = u * 4 + g
                        STT(vn[:, blk, gg * 128:(gg + 1) * 128], vg[i][:, g * 128:(g + 1) * 128], ssv[:, i * 4 + g:i * 4 + g + 1],
                            vgrep[:, gg * 128:(gg + 1) * 128], ALU.mult, ALU.mult,
                            r=[("vg", i), ("ssv", i), "vgrep", "SC"], w=["R", ("vn", blk // 3)])
            if ti == 0 and l == layers[0]:
                TAP("WmT", WmT[:, :, :, :], r=["WmT"])
                TAP("uT", yA, r=[("yA", 0), ("yA", 1)])
                TAP("vn", vn, r=[("vn", 0), ("vn", 1)])
            CK("A_v")
            for g in range(8):
                for hf in range(2):
                    b = bank()
                    for j in range(3):
                        MM(ps[b][:, j * 128:(j + 1) * 128], onesf[0:1, :], brow[:, g * 128:(g + 1) * 128], j == 0, False,
                           r=["onesf", "brow", "SC"], w=[("ps", b)])
                    for j in range(3):
                        blk = hf * 3 + j
                        MM(ps[b][:, j * 128:(j + 1) * 128], vn[:, blk, g * 128:(g + 1) * 128], WmT[:, l, g, :], False, j == 2,
                           r=[("vn", hf), "R", "WmT"], w=[("ps", b)])
                    TT_("dve", yA[:, g, hf * HT:(hf + 1) * HT], yA[:, g, hf * HT:(hf + 1) * HT], ps[b][:, 0:HT], ALU.mult,
                        r=[("ps", b), ("yA", hf)], w=["Y", ("yA", hf)])
            if ti == 0 and l == layers[0]:
                TAP("yA", yA, r=[("yA", 0), ("yA", 1)])
            FENCE("R")
            FENCE("SC")

            CK("A")
            CP("dve", kn2[:, :, 0:128], savekv[:, l, 0:512].rearrange("p (k t) -> p k t", t=128), r=["savekv"], w=["R", "kn2"])
            CP("dve", vb[:, 0, :], savekv[:, l, 512:768], r=["savekv"], w=["R", "vb"])
            for u in range(2):
                i = wctr[0] % NW
                wctr[0] += 1
                wv = wbuf[i][:, 0:NKC * 256].rearrange("p (k c) -> p k c", c=256)
                wk = ("w", i)
                src = w_in[l].rearrange("(kc p) n -> p kc n", p=128)
                for kv in range(2):
                    for dup in range(2):
                        c0 = 3072 + (u * 2 + kv) * 64
                        DMA("pool", wv[:, :, kv * 128 + dup * 64:kv * 128 + (dup + 1) * 64], src[:, :, c0:c0 + 64],
                            r=(), w=[wk], sem="w%d" % i)
                cons = qk_norm_consumer(1, bdiag[:, :], "bdiag", 1.0 / 64.0, lambda c, l=l: gkb[:, l:l + 1],
                                        lambda c, hf, u=u: kn2[:, u * 2 + c, 128 + hf * HT:128 + (hf + 1) * HT], "kn2", HT, 0)
                fm_unit(wv, wk, 2, NKC, hT, "hT", 2, HT, cons)
            flush()
            CK("B_k")
            wv, wk = wload(w_in_unit(l, 3328, 256), NKC, 256)
            for blk in range(NB):
                b = bank()
                for k in range(NKC):
                    MM(ps[b][:, 0:256], hT[:, k, blk * 128:(blk + 1) * 128], wv[:, k, :], k == 0, k == NKC - 1,
                       r=[wk, ("hT", blk // 3)], w=[("ps", b)])
                CP("act", vb[:, 1 + blk, :], ps[b][:, 0:256], r=[("ps", b)], w=["R", "vb"])
            CP("dve", savekv[:, l, 0:512].rearrange("p (k t) -> p k t", t=128), kn2[:, :, NB * 128:(NB + 1) * 128], r=["kn2", "R"], w=["savekv"])
            CP("dve", savekv[:, l, 512:768], vb[:, NB, :], r=["vb", "R"], w=["savekv"])
            CK("B_v")
            for u in range(2):
                wv, wk = wload(w_in_unit(l, 2048 + u * 512), NKC, 512)
                cons = qk_norm_consumer(1, bdiag[:, :], "bdiag", 1.0 / 64.0, lambda c, l=l: gqb[:, l:l + 1],
                                        lambda c, hf, u=u: qbuf[:, u * 4 + c, hf * HT:(hf + 1) * HT], "qbuf", HT, 0)
                fm_unit(wv, wk, 4, NKC, hT, "hT", 2, HT, cons)
            flush()
            if ti == 0 and l == layers[0]:
                TAP("qn", qbuf, r=["qbuf"])
                TAP("kn2", kn2, r=["kn2"])
            CK("B_q")
            PB = 6144
            Pbuf = [[SCb[:, PB + (i * 2 + j) * 512:PB + (i * 2 + j + 1) * 512] for j in range(2)] for i in range(2)]
            dnb = [SCf[:, 4096 + i * 256:4096 + (i + 1) * 256] for i in range(2)]
            step = [0]
            for blk in range(NB):
                for kvh in range(4):
                    i = step[0] % 2
                    step[0] += 1
                    gblk = ti * NB + blk
                    if gblk == 0:
                        mprev = 3
                    elif gblk == 2:
                        mprev = 2
                    else:
                        mprev = 1
                    for par in range(2):
                        b = bank()
                        MM(ps[b][:, 0:256], identb[:, :], mbt[:, 0, :].unsqueeze(1).to_broadcast([128, 2, 128]), True, False,
                           r=["identb", "mbt"], w=[("ps", b)])
                        MM(ps[b][:, 256:512], identb[:, :], mbt[:, mprev, :].unsqueeze(1).to_broadcast([128, 2, 128]), False, False,
                           r=["identb", "mbt"], w=[("ps", b)])
                        for which, slot in enumerate((1 + blk, blk)):
                            MM(ps[b][:, which * 256:(which + 1) * 256], kn2[par * 64:(par + 1) * 64, kvh, slot * 128:(slot + 1) * 128],
                               qbuf[par * 64:(par + 1) * 64, 2 * kvh:2 * kvh + 2, blk * 128:(blk + 1) * 128], False, which == 1,
                               r=["kn2", "qbuf", "R"], w=[("ps", b)])
                        ACTF(Pbuf[i][par], ps[b][:, :], AF.Exp, r=[("ps", b)], w=[("P", i, par), "SC"], scale=0.125)

                    def pv(i=i, blk=blk, kvh=kvh):
                        b = bank()
                        for par in range(2):
                            for which, slot in ((1, blk), (0, 1 + blk)):
                                MM(ps[b][par * 64:(par + 1) * 64, 0:256], vb[:, slot, kvh * 64:(kvh + 1) * 64],
                                   Pbuf[i][par][:, which * 256:(which + 1) * 256], which == 1, which == 0,
                                   r=["vb", "R", ("P", i, par), "SC"], w=[("ps", b)])
                            for which in (1, 0):
                                MM(ps[b][par * 64:(par + 1) * 64, 256:512], onesb[:, 0:64],
                                   Pbuf[i][par][:, which * 256:(which + 1) * 256], which == 1, which == 0,
                                   r=["onesb", ("P", i, par), "SC"], w=[("ps", b)])
                        for j in range(2):
                            TS("dve", dnb[i][:, j * 128:(j + 1) * 128], ps[b][:, 256 + j * 128:256 + (j + 1) * 128],
                               esink[:, l, 2 * kvh + j:2 * kvh + j + 1], None, ALU.add, None, r=[("ps", b), "esink"], w=[("dn", i), "SC"])
                        RECIP(dnb[i], dnb[i], r=[("dn", i)], w=[("dn", i), "SC"])
                        TT_("dve", yB[:, 2 * kvh:2 * kvh + 2, blk * 128:(blk + 1) * 128],
                            ps[b][:, 0:256].rearrange("p (j q) -> p j q", q=128), dnb[i].rearrange("p (j q) -> p j q", q=128), ALU.mult,
                            r=[("ps", b), ("dn", i), "SC"], w=["Y", ("yB", blk // 3)])
                    defer(pv)
                    flush(keep=1)
            flush()
            if ti == 0 and l == layers[0]:
                TAP("yB", yB, r=[("yB", 0), ("yB", 1)])
            FENCE("SC")

            CK("B")
            Pm = [[SCb[:, PB + (i * 2 + j) * HT:PB + (i * 2 + j + 1) * HT] for j in range(2)] for i in range(2)]
            rdb = [SCf[:, 4096 + i * HT:4096 + (i + 1) * HT] for i in range(1)]
            for u in range(2):
                wv, wk = wload(w_in_unit(l, 3584 + u * 512), NKC, 512)
                cons = qk_norm_consumer(2, onesb[:, :], "onesb", 1.0 / 256.0, lambda c, l=l: gqc[:, l, (c % 2):(c % 2) + 1],
                                        lambda c, hf: qbuf[:, c, hf * HT:(hf + 1) * HT], "qbuf", HT, 0)
                fm_unit(wv, wk, 4, NKC, hT, "hT", 2, HT, cons)
                flush()
                for hl in range(2):
                    hc = u * 2 + hl
                    for hf in range(2):
                        i = step[0] % 2
                        step[0] += 1
                        for m in range(2):
                            b = bank()
                            for dc in range(2):
                                MM(ps[b][:, 0:HT], kcT[:, 2 * hc + dc, m * 128:(m + 1) * 128], qbuf[:, 2 * hl + dc, hf * HT:(hf + 1) * HT],
                                   dc == 0, dc == 1, r=["kvc", "kvcR", "qbuf", "R"], w=[("ps", b)])
                            ACTF(Pm[i][m], ps[b][:, 0:HT], AF.Exp, r=[("ps", b)], w=[("P", i, m), "SC"], scale=1.0 / 16.0)

                        def pvc(i=i, hc=hc, hf=hf):
                            bd = bank()
                            for m in range(2):
                                MM(ps[bd][:, 0:HT], onesb[:, :], Pm[i][m], m == 0, m == 1, r=["onesb", ("P", i, m), "SC"], w=[("ps", bd)])
                            RECIP(rdb[0], ps[bd][:, 0:HT], r=[("ps", bd)], w=[("rd", 0), "SC"])
                            for dc in range(2):
                                b = bank()
                                for m in range(2):
                                    MM(ps[b][:, 0:HT], vc[:, m, hc * 256 + dc * 128:hc * 256 + (dc + 1) * 128], Pm[i][m], m == 0, m == 1,
                                       r=["kvc", "kvcR", ("P", i, m), "SC"], w=[("ps", b)])
                                TT_("dve", yC[:, 2 * hc + dc, hf * HT:(hf + 1) * HT], ps[b][:, 0:HT], rdb[0], ALU.mult,
                                    r=[("ps", b), ("rd", 0), "SC"], w=["Y", ("yC", hf)])
                        defer(pvc)
                        flush(keep=1)
                flush()
            if ti == 0 and l == layers[0]:
                TAP("yC", yC, r=[("yC", 0), ("yC", 1)])
            FENCE("R")
            FENCE("SC")

            CK("C")
            FENCE("kvcR")
            pacc = SCf[:, 0:4 * TT].rearrange("p (c t) -> p c t", t=TT)
            sgA = kvcf[:, 0:2 * TT].rearrange("p (c t) -> p c t", t=TT)
            sgB = SCf[:, 4 * TT:6 * TT].rearrange("p (c t) -> p c t", t=TT)
            tb = [SCf[:, 6 * TT + i * HT:6 * TT + (i + 1) * HT] for i in range(2)]
            ys = (yA, yB, yC)
            ykeys = ("yA", "yB", "yC")
            mstep = [0]

            def sg_ap(dcl, sl):
                return sgA[:, dcl, sl] if dcl < 2 else sgB[:, dcl - 2, sl]

            for d4 in range(4):
                for n in range(3):
                    wgv, wgk = wload(w_in_unit(l, 4608 + n * D + d4 * 512), NKC, 512)

                    def gcons(b_, c, hf):
                        sl = slice(hf * HT, (hf + 1) * HT)
                        ACTF(sg_ap(c, sl), ps[b_][:, 0:HT], AF.Sigmoid, r=[("ps", b_)], w=[("sg", c, hf), "SC", "kvcR"])
                    fm_unit(wgv, wgk, 4, NKC, hT, "hT", 2, HT, gcons)
                    wbv, wbk = wload(w_branch[l, n].rearrange("(wc p) d -> p wc d", p=128)[:, :, d4 * 512:(d4 + 1) * 512], 8, 512)

                    def wcons(b_, c, hf, n=n, d4=d4):
                        sl = slice(hf * HT, (hf + 1) * HT)
                        i = mstep[0] % 2
                        mstep[0] += 1
                        pk = ("pacc", c, hf)
                        if n == 0:
                            TT_("dve", pacc[:, c, sl], ps[b_][:, 0:HT], sg_ap(c, sl), ALU.mult,
                                r=[("ps", b_), ("sg", c, hf), "kvcR"], w=[pk, "SC"])
                        else:
                            TT_("dve", tb[i], ps[b_][:, 0:HT], sg_ap(c, sl), ALU.mult,
                                r=[("ps", b_), ("sg", c, hf), "kvcR"], w=[("tb", i), "SC"])
                            if n == 1:
                                TT_("dve", pacc[:, c, sl], pacc[:, c, sl], tb[i], ALU.add, r=[pk, ("tb", i)], w=[pk, "SC"])
                            else:
                                TT_("dve", mT[:, d4 * 4 + c, sl], pacc[:, c, sl], tb[i], ALU.add, r=[pk, ("tb", i), "SC"],
                                    w=["R", ("mT", hf)])
                    fm_unit(wbv, wbk, 4, 8, ys[n], ykeys[n], 2, HT, wcons)
            if ti == 0 and l == layers[0]:
                TAP("mT", mT, r=[("mT", 0), ("mT", 1)])
            CK("merge")
            for d4 in range(4):
                wv, wk = wload(w_out[l].rearrange("(kc p) n -> p kc n", p=128)[:, :, d4 * 512:(d4 + 1) * 512], NKC, 512)

                def ocons(b, c, hf, d4=d4):
                    dc = d4 * 4 + c
                    sl = slice(hf * HT, (hf + 1) * HT)
                    TT_("dve", xT[:, dc, sl], xT[:, dc, sl], ps[b][:, 0:HT], ALU.add, r=[("ps", b), ("xT", hf)], w=[("xT", hf)])
                fm_unit(wv, wk, 4, NKC, mT, "mT", 2, HT, ocons)
            if ti == 0 and l == layers[0]:
                TAP("x1", xT[:, :, :], r=[("xT", 0), ("xT", 1)])
            FENCE("R")
            FENCE("Y")
            FENCE("SC")

        def moe(ti, l, last_layer_in_launch):
            CK("mixer")
            DMA("pool", wr[:, :, 0:4], w_rg[l].rearrange("(kc p) g -> p kc g", p=128), r=(), w=["wr"], sem="wr",
                allow_slow_non_contiguous=True)
            DMA("pool", wr[:, :, 4:20], w_re[l].rearrange("(kc p) g -> p kc g", p=128), r=(), w=["wr"], sem="wr",
                allow_slow_non_contiguous=True)
            DMA("sp", rb[:, 0:4], b_rg[l].partition_broadcast(128), r=(), w=["rb"])
            DMA("sp", rb[:, 4:20], b_re[l].partition_broadcast(128), r=(), w=["rb"])
            rms_to_h(xT, "xT", gffn[:, l, :], 2, HT)
            for blk in range(NB):
                b = bank()
                for k in range(NKC):
                    MM(ps[b][:, 0:20], hT[:, k, blk * 128:(blk + 1) * 128], wr[:, k, :], k == 0, k == NKC - 1,
                       r=["wr", ("hT", blk // 3)], w=[("ps", b)])
                lg = rt[:, 0:20]
                gl = rt[:, 0:4]
                el = rt[:, 4:20]
                gmax = rt[:, 20:21]
                ngmax = rt[:, 21:22]
                oh = rt[:, 22:26]
                sumg = rt[:, 26:27]
                pen = rt[:, 27:31]
                m1 = rt[:, 31:32]
                m2 = rt[:, 32:33]
                dd = rt[:, 33:34]
                w1 = rt[:, 34:35]
                w2 = rt[:, 35:36]
                junk4 = rt[:, 36:40]
                em = rt[:, 40:56]
                oh1 = rt[:, 56:72]
                em2 = rt[:, 72:88]
                RT = ["rt"]
                TT_("dve", lg, ps[b][:, 0:20], rb[:, :], ALU.add, r=[("ps", b), "rb"], w=RT)
                RED(gmax, gl, ALU.max, r=RT, w=RT)
                TS("dve", oh, gl, gmax, None, ALU.is_equal, None, r=RT, w=RT)
                TS("dve", ngmax, gmax, -1.0, None, ALU.mult, None, r=RT, w=RT)
                ACTF(junk4, gl, AF.Exp, r=RT, w=RT, bias=ngmax, accum_out=sumg)
                TS("dve", pen, oh, -1.0, 1e9, ALU.add, ALU.mult, r=RT, w=RT)
                TT_("dve", em.rearrange("p (g j) -> p g j", j=4), el.rearrange("p (g j) -> p g j", j=4),
                    pen.unsqueeze(2).to_broadcast([128, 4, 4]), ALU.add, r=RT, w=RT)
                RED(m1, em, ALU.max, r=RT, w=RT)
                TS("dve", oh1, em, m1, None, ALU.is_equal, None, r=RT, w=RT)
                STT(em2, oh1, -1e9, em, ALU.mult, ALU.add, r=RT, w=RT)
                RED(m2, em2, ALU.max, r=RT, w=RT)
                TS("dve", em2, em2, m2, None, ALU.is_equal, None, r=RT, w=RT)
                TT_("dve", dd, m2, m1, ALU.subtract, r=RT, w=RT)
                ACTF(dd, dd, AF.Exp, r=RT, w=RT)
                TS("dve", w1, dd, 1.0, None, ALU.add, None, r=RT, w=RT)
                TT_("dve", w1, w1, sumg, ALU.mult, r=RT, w=RT)
                RECIP(w1, w1, r=RT, w=RT)
                TT_("dve", w2, w1, dd, ALU.mult, r=RT, w=RT)
                TS("dve", oh1, oh1, w1, None, ALU.mult, None, r=RT, w=RT)
                STT(gates[:, blk, :], em2, w2, oh1, ALU.mult, ALU.add, r=RT, w=["gates"])
            if ti == 0 and l == layers[0]:
                TAP("gates", gates[:, :, :], r=["gates"])
            CK("router")
            De = [SCf[:, i * 128:(i + 1) * 128] for i in range(3)]
            rep = SCf[:, 384:384 + TT]
            sgl = SCf[:, 1152:1152 + 4 * TT].rearrange("p (c t) -> p c t", t=TT)
            t1 = [SCf[:, 4224 + i * HT:4224 + (i + 1) * HT] for i in range(2)]
            dctr = [0]
            estep = [0]
            for G in range(4):
                for el_ in range(4):
                    e_ = G * 4 + el_
                    wgv, wgk = wload(w_gate_e[l, e_].rearrange("(kc p) f -> p kc f", p=128), NKC, 512)

                    def gcons(b_, c, hf):
                        ACTF(sgl[:, c, hf * HT:(hf + 1) * HT], ps[b_][:, 0:HT], AF.Silu, r=[("ps", b_)], w=[("sgl", c, hf), "SC"])
                    fm_unit(wgv, wgk, 4, NKC, hT, "hT", 2, HT, gcons)
                    for hf in range(2):
                        b = bank()
                        for j in range(3):
                            blk = hf * 3 + j
                            di = dctr[0] % 3
                            dctr[0] += 1
                            TS("dve", De[di], identf[:, :], gates[:, blk, e_:e_ + 1], None, ALU.mult, None,
                               r=["identf", "gates"], w=[("De", di), "SC"])
                            MM(ps[b][:, j * 128:(j + 1) * 128], onesf[:, :], De[di], True, True, r=["onesf", ("De", di), "SC"], w=[("ps", b)])
                        CP("act", rep[:, hf * HT:(hf + 1) * HT], ps[b][:, 0:HT], r=[("ps", b)], w=[("rep", hf), "SC"])
                    wuv, wuk = wload(w_up_e[l, e_].rearrange("(kc p) f -> p kc f", p=128), NKC, 512)

                    def ucons(b_, c, hf, el_=el_):
                        sl = slice(hf * HT, (hf + 1) * HT)
                        i = estep[0] % 2
                        estep[0] += 1
                        TT_("dve", t1[i], ps[b_][:, 0:HT], sgl[:, c, sl], ALU.mult, r=[("ps", b_), ("sgl", c, hf)], w=[("t1", i), "SC"])
                        TT_("dve", hidT[:, el_ * 4 + c, sl], t1[i], rep[:, sl], ALU.mult,
                            r=[("t1", i), ("rep", hf), "SC"], w=["Y", ("hid", hf)])
                    fm_unit(wuv, wuk, 4, NKC, hT, "hT", 2, HT, ucons)
                if ti == 0 and l == layers[0] and G == 0:
                    TAP("hid", hidT, r=[("hid", 0), ("hid", 1)])
                wd = w_down_e[l].rearrange("e f d -> (e f) d").rearrange("(k p) d -> p k d", p=128)
                for d4 in range(4):
                    wv, wk = wload(wd[:, G * 16:(G + 1) * 16, d4 * 512:(d4 + 1) * 512], 16, 512)

                    def dcons(b, c, hf, d4=d4):
                        dc = d4 * 4 + c
                        sl = slice(hf * HT, (hf + 1) * HT)
                        TT_("dve", xT[:, dc, sl], xT[:, dc, sl], ps[b][:, 0:HT], ALU.add, r=[("ps", b), ("xT", hf)], w=[("xT", hf)])
                    fm_unit(wv, wk, 4, 16, hidT, "hid", 2, HT, dcons)
            if ti == 0 and l == layers[0]:
                TAP("x2", xT[:, :, :], r=[("xT", 0), ("xT", 1)])
            FENCE("Y")
            FENCE("SC")
            CK("moe")
            if last_layer_in_launch:
                for blk in range(NB):
                    gtok = ti * TT + blk * 128
                    if gtok < out_tokens_from:
                        continue
                    st = stage[blk % 4]
                    skey = ("stage", blk % 4)
                    for k4 in range(4):
                        b = bank()
                        for j in range(4):
                            kc = k4 * 4 + j
                            TR(ps[b][:, j * 128:(j + 1) * 128], xT[:, kc, blk * 128:(blk + 1) * 128], identf[:, :],
                               r=[("xT", blk // 3), "identf"], w=[("ps", b)])
                        CP("act" if k4 % 2 else "dve", st[:, k4 * 512:(k4 + 1) * 512], ps[b][:, :], r=[("ps", b)], w=[skey, "Y"])
                    okey_ = ("out", len(out_keys))
                    out_keys.append(okey_)
                    DMA("sp", out[gtok - out_tokens_from:gtok - out_tokens_from + 128, :], st, r=[skey], w=[okey_])
                FENCE("Y")

        mem_prologue()
        CK("memprol")
        for ti in range(n_tiles):
            for li, l in enumerate(layers):
                mixer(ti, l, li == 0)
                moe(ti, l, li == len(layers) - 1)
        fin_keys = out_keys + ["tap_" + k for k in tap_aps]
        S.op("sp", lambda e: None, r=fin_keys, w=(), force=True)

        semnames = list(Sched.ENGS) + ["w%d" % i for i in range(NW)] + ["d%d" % i for i in range(6)] + ["wr"]
        sems = {n: es.enter_context(nc.semaphore("s_" + n)) for n in semnames}
        block = es.enter_context(nc.Block())
        S.emit(nc, block, sems)
    return nc


def _consts():
    ident = np.eye(128, dtype=np.float32)
    tril = np.tril(np.ones((128, 128), dtype=np.float32))
    j = np.arange(128)[:, None]
    i = np.arange(128)[None, :]
    cur = np.where(i >= j, 0.0, NEG).astype(np.float32)
    prev = np.where(i < j, 0.0, NEG).astype(np.float32)
    allm = np.full((128, 128), NEG, dtype=np.float32)
    return ident, tril, cur, prev, allm


WEIGHT_KEYS = ["norm_mix", "norm_mem", "norm_ffn", "w_in", "v_gain", "w_spatial", "b_spatial", "q_gain_b", "k_gain_b",
               "sinks", "q_gain_c", "k_gain_c", "w_mem_kv", "w_branch", "w_out", "w_router_group", "b_router_group",
               "w_router_expert", "b_router_expert", "w_gate_e", "w_up_e", "w_down_e"]

FUSED = True
_NC_CACHE = {}


def _get_nc(layers):
    key = tuple(layers)
    if key not in _NC_CACHE:
        _NC_CACHE[key] = build_program(list(layers))
    return _NC_CACHE[key]


def _in_maps(xfull, inputs, n_cores=8):
    ident, tril, cur, prev, allm = _consts()
    maps = []
    for c in range(n_cores):
        b, half = c // 2, c % 2
        xs = np.zeros((TOK, D), dtype=np.float32)
        if half == 0:
            xs[256:] = xfull[b, 0:2048]
            pm = allm
        else:
            xs[:] = xfull[b, 2048 - 256:4096]
            pm = prev
        m = {"x_in": xs, "mem_in": np.ascontiguousarray(inputs["mem"][b]),
             "c_ident": ident, "c_tril": tril,
             "c_mb": np.stack([cur, prev, pm, allm]).astype(ml_dtypes.bfloat16)}
        for k in WEIGHT_KEYS:
            m[k] = inputs[k]
        maps.append(m)
    return maps


def kernel(**inputs):
    inputs = {k: np.ascontiguousarray(np.asarray(v)) for k, v in inputs.items()}
    x = inputs["x"]
    launches = [[0, 1]] if FUSED else [[0], [1]]
    for layers in launches:
        nc = _get_nc(layers)
        res = run_bass_kernel_spmd(nc, _in_maps(x, inputs), core_ids=list(range(8)))
        xn = np.empty_like(x)
        for c in range(8):
            b, half = c // 2, c % 2
            xn[b, half * 2048:(half + 1) * 2048] = res.results[c]["out"]
        x = xn
    return x
```

```python
import numpy as np
import ml_dtypes
from contextlib import ExitStack
import concourse.bass as bass
import concourse.mybir as mybir
from concourse.bass_utils import run_bass_kernel_spmd

F32 = mybir.dt.float32
BF16 = mybir.dt.bfloat16
AF = mybir.ActivationFunctionType
ALU = mybir.AluOpType
AX = mybir.AxisListType

D = 2048
NKC = 16
L = 2
IN_W = 10752
NB = 6
TT = NB * 128
HT = TT // 2
NT = 3
TOK = NT * TT
EPS = 1e-6
NEG = -30000.0
NW = 4
SAME_ENG_SYNC = True


class Sched:
    ENGS = ("pe", "act", "dve", "pool", "sp")

    def __init__(self):
        self.ops = {e: [] for e in self.ENGS}
        self.last_w = {}
        self.readers = {}
        self.waited = {e: {} for e in self.ENGS}
        self.dma_val = {}
        self.signal = set()
        self.halted = False

    def _need(self, eng, tok, waits):
        src, v = tok
        if src == eng and (eng == "pe" or not SAME_ENG_SYNC):
            return
        if self.waited[eng].get(src, -1) >= v:
            return
        self.waited[eng][src] = v
        waits.append(tok)
        if src in self.ENGS:
            self.signal.add((src, v))

    REGIONS = ("SC", "Y", "R", "kvcR")

    def op(self, eng, fn, r=(), w=(), dma=None, fence=False, force=False):
        if self.halted and not force:
            return None
        if not fence:
            mv = [k for k in w if k in self.REGIONS]
            if mv:
                w = [k for k in w if k not in self.REGIONS]
                r = list(r) + mv
        waits = []
        for k in r:
            t = self.last_w.get(k)
            if t is not None:
                self._need(eng, t, waits)
            if isinstance(k, tuple) and k[0] == "ps":
                for src, t in self.readers.get(k, {}).items():
                    if src != eng:
                        self._need(eng, t, waits)
        for k in w:
            t = self.last_w.get(k)
            if t is not None:
                self._need(eng, t, waits)
            for t in self.readers.get(k, {}).values():
                self._need(eng, t, waits)
        idx = len(self.ops[eng])
        if dma is not None:
            prev = self.dma_val.get(dma, 0)
            if prev:
                self._need(eng, (dma, prev), waits)
            tok = (dma, prev + 16)
            self.dma_val[dma] = prev + 16
        else:
            tok = (eng, idx)
        self.ops[eng].append((fn, waits, dma))
        for k in r:
            self.readers.setdefault(k, {})[tok[0]] = tok
        for k in w:
            self.last_w[k] = tok
            self.readers[k] = {}
        return tok

    def emit(self, nc, block, sems):
        sigval = {}
        for e in self.ENGS:
            c = 0
            for i in range(len(self.ops[e])):
                if (e, i) in self.signal:
                    c += 1
                    sigval[(e, i)] = c
        ENGS = self.ENGS

        def run(e, eng):
            for i, (fn, waits, dma) in enumerate(self.ops[e]):
                for (src, v) in waits:
                    if src in ENGS:
                        eng.wait_ge(sems[src], sigval[(src, v)])
                    else:
                        eng.wait_ge(sems[src], v)
                ins = fn(eng)
                if ins is None:
                    continue
                if dma is not None:
                    ins.then_inc(sems[dma], 16)
                elif (e, i) in self.signal:
                    ins.then_inc(sems[e], 1)

        @block.tensor
        def _(eng):
            run("pe", eng)

        @block.scalar
        def _(eng):
            run("act", eng)

        @block.vector
        def _(eng):
            run("dve", eng)

        @block.gpsimd
        def _(eng):
            run("pool", eng)

        @block.sync
        def _(eng):
            run("sp", eng)


def build_program(layers, n_tiles=NT, taps=None, out_tokens_from=256, stop_after=None):
    taps = taps or {}
    nc = bass.Bass("TRN2", target_bir_lowering=False)
    S = Sched()

    def din(name, shape, dt=F32):
        return nc.dram_tensor(name, list(shape), dt, kind="ExternalInput").ap()

    x_in = din("x_in", [TOK, D])
    mem_in = din("mem_in", [256, D])
    norm_mix = din("norm_mix", [L, D])
    norm_mem = din("norm_mem", [L, D])
    norm_ffn = din("norm_ffn", [L, D])
    w_in = din("w_in", [L, D, IN_W])
    v_gain = din("v_gain", [L, 8, 128])
    w_spatial = din("w_spatial", [L, 8, 128, 128])
    b_spatial = din("b_spatial", [L, 8, 128])
    q_gain_b = din("q_gain_b", [L, 64])
    k_gain_b = din("k_gain_b", [L, 64])
    sinks = din("sinks", [L, 16])
    q_gain_c = din("q_gain_c", [L, 256])
    k_gain_c = din("k_gain_c", [L, 256])
    w_mem_kv = din("w_mem_kv", [L, D, D])
    w_branch = din("w_branch", [L, 3, 1024, D])
    w_out = din("w_out", [L, D, D])
    w_rg = din("w_router_group", [L, D, 4])
    b_rg = din("b_router_group", [L, 4])
    w_re = din("w_router_expert", [L, D, 16])
    b_re = din("b_router_expert", [L, 16])
    w_gate_e = din("w_gate_e", [L, 16, D, 512])
    w_up_e = din("w_up_e", [L, 16, D, 512])
    w_down_e = din("w_down_e", [L, 16, 512, D])
    c_ident = din("c_ident", [128, 128])
    c_tril = din("c_tril", [128, 128])
    c_mb = din("c_mb", [4, 128, 128], BF16)
    n_out_tok = n_tiles * TT - out_tokens_from
    out = nc.dram_tensor("out", [n_out_tok, D], F32, kind="ExternalOutput").ap()
    kvscr = nc.dram_tensor("kvscr", [L, 128, 4096], BF16, kind="Internal").ap()
    tap_aps = {k: nc.dram_tensor("tap_" + k, list(shp), dt, kind="ExternalOutput").ap() for k, (shp, dt) in taps.items()}

    es = ExitStack()
    with es:
        def sb(name, shape, dt):
            return es.enter_context(nc.sbuf_tensor(name, list(shape), dt))

        xT = sb("xT", [128, NKC, TT], F32)
        hT = sb("hT", [128, NKC, TT], BF16)
        Yr = sb("Yr", [128, 24 * TT], BF16)
        Rr = sb("Rr", [128, 16 * TT], BF16)
        wbuf = [sb("wbuf%d" % i, [128, 8 * 512], BF16) for i in range(NW)]
        SCb = sb("SC", [128, 10752], BF16)
        kvc = sb("kvc", [128, 4096], BF16)
        identf = sb("identf", [128, 128], F32)
        identb = sb("identb", [128, 128], BF16)
        onesb = sb("onesb", [128, 128], BF16)
        bdiag = sb("bdiag", [128, 128], BF16)
        onesf = sb("onesf", [128, 128], F32)
        mbt = sb("mbt", [128, 4, 128], BF16)
        gmix = sb("gmix", [128, L, NKC], F32)
        gffn = sb("gffn", [128, L, NKC], F32)
        gmem = sb("gmem", [128, L, NKC], F32)
        gqb = sb("gqb", [128, L], F32)
        gkb = sb("gkb", [128, L], F32)
        esink = sb("esink", [128, L, 8], F32)
        gqc = sb("gqc", [128, L, 2], F32)
        gkc = sb("gkc", [128, L, 2], F32)
        WmT = sb("WmT", [128, L, 8, 128], BF16)
        wr = sb("wr", [128, NKC, 20], BF16)
        rb = sb("rb", [128, 20], F32)
        gates = sb("gates", [128, NB, 16], F32)
        rt = sb("rt", [128, 96], F32)
        savekv = sb("savekv", [128, L, 768], BF16)
        dummy = sb("dmy0", [128, 8], F32)
        ps = [es.enter_context(nc.psum_tensor("ps%d" % i, [128, 512], F32)) for i in range(8)]

        Y4 = Yr[:, :].rearrange("p (c t) -> p c t", t=TT)
        yA, yB, yC = Y4[:, 0:8], Y4[:, 8:16], Y4[:, 16:24]
        hidT = Y4[:, 0:16]
        Ystage = Yr[:, :].bitcast(F32)
        stage = [Ystage[:, i * 2048:(i + 1) * 2048] for i in range(4)]
        mT = Rr[:, :].rearrange("p (c t) -> p c t", t=TT)
        vn = Rr[:, 0:NB * 1024].rearrange("p (b c) -> p b c", c=1024)
        kn2 = Rr[:, 0:4 * 896].rearrange("p (k t) -> p k t", t=896)
        vb = Rr[:, 4 * 896:4 * 896 + 7 * 256].rearrange("p (s c) -> p s c", c=256)
        qbuf = Rr[:, 8 * TT:16 * TT].rearrange("p (c t) -> p c t", t=TT)
        kcT = kvc[:, 0:2048].rearrange("p (c m) -> p c m", m=256)
        vc = kvc[:, 2048:4096].rearrange("p (m c) -> p m c", c=1024)
        SCf = SCb[:, :].bitcast(F32)
        kvcf = kvc[:, :].bitcast(F32)
        RMS0 = 4608

        def CK(name):
            if stop_after == name:
                S.halted = True

        bank_ctr = [0]

        def bank():
            i = bank_ctr[0] % 8
            bank_ctr[0] += 1
            return i

        dsem_ctr = [0]

        def dsem():
            i = dsem_ctr[0] % 6
            dsem_ctr[0] += 1
            return "d%d" % i

        def MM(out, lhsT, rhs, start, stop, r, w, **kw):
            return S.op("pe", lambda e: e.matmul(out, lhsT=lhsT, rhs=rhs, start=start, stop=stop, **kw), r, w)

        def TR(out, in_, ident, r, w):
            return S.op("pe", lambda e: e.transpose(out=out, in_=in_, identity=ident), r, w)

        def ACTF(out, in_, func, r, w, **kw):
            return S.op("act", lambda e: e.activation(out=out, in_=in_, func=func, **kw), r, w)

        def TT_(eng, out, in0, in1, op, r, w):
            return S.op(eng, lambda e: e.tensor_tensor(out=out, in0=in0, in1=in1, op=op), r, w)

        def TS(eng, out, in0, s1, s2, op0, op1, r, w):
            if op1 is None:
                return S.op(eng, lambda e: e.tensor_scalar(out=out, in0=in0, scalar1=s1, scalar2=None, op0=op0), r, w)
            return S.op(eng, lambda e: e.tensor_scalar(out=out, in0=in0, scalar1=s1, scalar2=s2, op0=op0, op1=op1), r, w)

        def STT(out, in0, scalar, in1, op0, op1, r, w):
            return S.op("dve", lambda e: e.scalar_tensor_tensor(out=out, in0=in0, scalar=scalar, in1=in1, op0=op0, op1=op1), r, w)

        def CP(eng, out, in_, r, w):
            if eng == "act":
                return S.op("act", lambda e: e.copy(out=out, in_=in_), r, w)
            return S.op(eng, lambda e: e.tensor_copy(out=out, in_=in_), r, w)

        def RECIP(out, in_, r, w):
            return S.op("dve", lambda e: e.reciprocal(out=out, in_=in_), r, w)

        def RED(out, in_, op, r, w):
            return S.op("dve", lambda e: e.tensor_reduce(out=out, in_=in_, axis=AX.X, op=op), r, w)

        def DMA(q, out, in_, r, w, sem=None, **kw):
            return S.op(q, lambda e: e.dma_start(out=out, in_=in_, **kw), r, w, dma=sem or dsem())

        def FENCE(key, eng="dve"):
            return S.op(eng, lambda e: e.memset(dummy[:, 0:1], 0.0), r=(), w=[key, "dummy_" + eng], fence=True)

        def TAP(name, src, r):
            if name in tap_aps:
                DMA("sp", tap_aps[name], src, r=r, w=["tap_" + name])

        wctr = [0]

        class WUnit:
            def __init__(self, parts, kh):
                self.parts = parts
                self.kh = kh

            def row(self, k):
                return self.parts[k // self.kh][0][:, k % self.kh, :]

            def key(self, k):
                return self.parts[k // self.kh][1]

        def wslot():
            i = wctr[0] % NW
            wctr[0] += 1
            return i

        def wload(src_ap, kparts, cols):
            nparts = 1 if kparts * cols <= 4096 else 2
            kh = kparts // nparts
            parts = []
            for pi in range(nparts):
                i = wslot()
                view = wbuf[i][:, 0:kh * cols].rearrange("p (k c) -> p k c", c=cols)
                DMA("pool", view, src_ap[:, pi * kh:(pi + 1) * kh, :], r=(), w=[("w", i)], sem="w%d" % i)
                parts.append((view, ("w", i)))
            return WUnit(parts, kh), None

        def w_in_unit(l, c0, cols=512):
            return w_in[l].rearrange("(kc p) n -> p kc n", p=128)[:, :, c0:c0 + cols]

        DMA("sp", identf[:, :], c_ident[:, :], r=(), w=["identf"])
        DMA("sp", SCf[:, 0:128], c_tril[:, :], r=(), w=["tril", "SC"])
        DMA("sp", mbt[:, :, :], c_mb.rearrange("m k q -> k m q"), r=(), w=["mbt"])
        CP("dve", identb[:, :], identf[:, :], r=["identf"], w=["identb"])
        S.op("dve", lambda e: e.memset(onesb[:, :], 1.0), w=["onesb"])
        S.op("dve", lambda e: e.memset(onesf[:, :], 1.0), w=["onesf"])
        S.op("dve", lambda e: e.memset(bdiag[:, :], 0.0), w=["bdiag"])
        S.op("dve", lambda e: e.memset(bdiag[0:64, 0:64], 1.0), w=["bdiag"])
        S.op("dve", lambda e: e.memset(bdiag[64:128, 64:128], 1.0), w=["bdiag"])
        S.op("dve", lambda e: e.memset(savekv[:, :, :], 0.0), w=["savekv"])
        for (dst, src) in ((gmix, norm_mix), (gffn, norm_ffn), (gmem, norm_mem)):
            for l in range(L):
                DMA("sp", dst[:, l, :], src[l].rearrange("(kc p) -> p kc", p=128), r=(), w=["gains"],
                    allow_slow_non_contiguous=True)
        for l in range(L):
            for hf in range(2):
                DMA("sp", gqb[hf * 64:(hf + 1) * 64, l:l + 1], q_gain_b[l].rearrange("(d o) -> d o", o=1), r=(), w=["gains"])
                DMA("sp", gkb[hf * 64:(hf + 1) * 64, l:l + 1], k_gain_b[l].rearrange("(d o) -> d o", o=1), r=(), w=["gains"])
                DMA("sp", esink[hf * 64:(hf + 1) * 64, l, :],
                    sinks[l].rearrange("(c two) -> two c", two=2)[hf].partition_broadcast(64), r=(), w=["esink"],
                    allow_slow_non_contiguous=True)
            DMA("sp", gqc[:, l, :], q_gain_c[l].rearrange("(dc p) -> p dc", p=128), r=(), w=["gains"], allow_slow_non_contiguous=True)
            DMA("sp", gkc[:, l, :], k_gain_c[l].rearrange("(dc p) -> p dc", p=128), r=(), w=["gains"], allow_slow_non_contiguous=True)
        ACTF(esink[:, :, :], esink[:, :, :], AF.Exp, r=["esink"], w=["esink"])
        wsp = SCf[:, 128:128 + 1024].rearrange("p (g s) -> p g s", s=128)
        for l in range(L):
            DMA("sp", wsp, w_spatial[l].rearrange("g t s -> t g s"), r=(), w=["wsp", "SC"])
            TT_("dve", wsp, wsp, SCf[:, 0:128].unsqueeze(1).to_broadcast([128, 8, 128]), ALU.mult, r=["tril", "wsp"], w=["wsp", "SC"])
            for g4 in range(2):
                b = bank()
                for j in range(4):
                    g = g4 * 4 + j
                    TR(ps[b][:, j * 128:(j + 1) * 128], wsp[:, g, :], identf[:, :], r=["wsp", "SC", "identf"], w=[("ps", b)])
                CP("dve", WmT[:, l, g4 * 4:(g4 + 1) * 4, :], ps[b][:, :].rearrange("p (j t) -> p j t", t=128),
                   r=[("ps", b)], w=["WmT"])
        FENCE("SC")

        CK("params")
        def load_tokens_T(src_rows, nblk, dstT, dkey):
            for blk in range(nblk):
                st = stage[blk % 4]
                skey = ("stage", blk % 4)
                DMA("sp", st, src_rows[blk * 128:(blk + 1) * 128, :], r=["Y"], w=[skey])
                for k4 in range(4):
                    b = bank()
                    for j in range(4):
                        kc = k4 * 4 + j
                        TR(ps[b][:, j * 128:(j + 1) * 128], st[:, kc * 128:(kc + 1) * 128], identf[:, :],
                           r=[skey, "identf"], w=[("ps", b)])
                    CP("act" if k4 % 2 else "dve", dstT[:, k4 * 4:(k4 + 1) * 4, blk * 128:(blk + 1) * 128],
                       ps[b][:, :].rearrange("p (j t) -> p j t", t=128), r=[("ps", b)], w=[(dkey, blk * 128 // HT)])

        def rms_to_h(srcT, skey, gain_ap, halves, hw):
            rstd = SCf[:, 0:TT]
            sqr = [SCb[:, 2 * TT + i * HT:2 * TT + (i + 1) * HT] for i in range(3)]
            for hf in range(halves):
                sl = slice(hf * hw, (hf + 1) * hw)
                b = bank()
                for kc in range(NKC):
                    sq = sqr[kc % 3]
                    ACTF(sq[:, 0:hw], srcT[:, kc, sl], AF.Square, r=[(skey, hf)], w=[("sq", kc % 3), "SC"])
                    MM(ps[b][:, 0:hw], onesb[:, :], sq[:, 0:hw], kc == 0, kc == NKC - 1, r=[("sq", kc % 3), "onesb", "SC"], w=[("ps", b)])
                ACTF(rstd[:, sl], ps[b][:, 0:hw], AF.Sqrt, r=[("ps", b)], w=[("rstd", hf), "SC"], scale=1.0 / D, bias=EPS)
                RECIP(rstd[:, sl], rstd[:, sl], r=[("rstd", hf)], w=[("rstd", hf), "SC"])
                for kc in range(NKC):
                    STT(hT[:, kc, sl], srcT[:, kc, sl], gain_ap[:, kc:kc + 1], rstd[:, sl], ALU.mult, ALU.mult,
                        r=[(skey, hf), ("rstd", hf), "gains", "SC"], w=[("hT", hf)])
            FENCE("SC")

        deferred = []
        out_keys = []

        def defer(fn):
            deferred.append(fn)

        def flush(keep=0):
            while len(deferred) > keep:
                deferred.pop(0)()

        def fm_unit(wv, wkey, nchunks, ksz, rhsT, rkey, halves, hw, consumer, hf_outer=False):
            order = [(c, hf) for hf in range(halves) for c in range(nchunks)] if hf_outer else \
                    [(c, hf) for c in range(nchunks) for hf in range(halves)]
            for (c, hf) in order:
                b = bank()
                for k in range(ksz):
                    MM(ps[b][:, 0:hw], wv.row(k)[:, c * 128:(c + 1) * 128], rhsT[:, k, hf * hw:(hf + 1) * hw], k == 0, k == ksz - 1,
                       r=[wv.key(k), (rkey, hf)], w=[("ps", b)])
                consumer(b, c, hf)

        def qk_norm_consumer(nsum, ones_ap, okey, invd, gain_fn, dst_fn, dkey, hw, scbase):
            st = {}
            cnt = [0]

            def consumer(b, c, hf):
                i = cnt[0]
                cnt[0] += 1
                slot = i % 4
                if any(slot in getattr(f, "slots", ()) for f in deferred):
                    flush()
                sqb = SCb[:, scbase + slot * hw: scbase + (slot + 1) * hw]
                qraw = SCf[:, (scbase + 4 * hw) // 2 + slot * hw:(scbase + 4 * hw) // 2 + (slot + 1) * hw]
                ACTF(sqb, ps[b][:, 0:hw], AF.Square, r=[("ps", b)], w=[("sqb", slot), "SC"])
                CP("dve", qraw, ps[b][:, 0:hw], r=[("ps", b)], w=[("qraw", slot), "SC"])
                st.setdefault(hf, []).append((c, slot))
                if len(st[hf]) == nsum:
                    group = st.pop(hf)

                    def fin(group=group, hf=hf):
                        b2 = bank()
                        for j, (c_, s_) in enumerate(group):
                            MM(ps[b2][:, 0:hw], ones_ap, SCb[:, scbase + s_ * hw: scbase + (s_ + 1) * hw], j == 0, j == len(group) - 1,
                               r=[("sqb", s_), okey, "SC"], w=[("ps", b2)])
                        rq = SCf[:, (scbase + 12 * hw) // 2 + (group[0][1] % 2) * hw:(scbase + 12 * hw) // 2 + (group[0][1] % 2 + 1) * hw]
                        rk = ("rq", group[0][1] % 2)
                        ACTF(rq, ps[b2][:, 0:hw], AF.Sqrt, r=[("ps", b2)], w=[rk, "SC"], scale=invd, bias=EPS)
                        RECIP(rq, rq, r=[rk], w=[rk, "SC"])
                        for (c_, s_) in group:
                            qr = SCf[:, (scbase + 4 * hw) // 2 + s_ * hw:(scbase + 4 * hw) // 2 + (s_ + 1) * hw]
                            STT(dst_fn(c_, hf), qr, gain_fn(c_), rq, ALU.mult, ALU.mult, r=[("qraw", s_), rk, "gains", "SC"], w=["R", dkey])
                    fin.slots = tuple(s_ for (_, s_) in group)
                    defer(fin)
                    flush(keep=1)
            return consumer

        def mem_prologue():
            load_tokens_T(mem_in, 2, xT, "xT")
            CK("mp_load")
            for l in range(L):
                rms_to_h(xT, "xT", gmem[:, l, :], 1, 256)
                CK("mp_rms")
                wkv = w_mem_kv[l].rearrange("(kc p) n -> p kc n", p=128)
                for u in range(2):
                    wv, wk = wload(wkv[:, :, u * 512:(u + 1) * 512], NKC, 512)
                    cons = qk_norm_consumer(2, onesb[:, :], "onesb", 1.0 / 256.0,
                                            lambda c, l=l: gkc[:, l, (c % 2):(c % 2) + 1],
                                            lambda c, hf, u=u: kcT[:, u * 4 + c, :], "kvc", 256, 0)
                    fm_unit(wv, wk, 4, NKC, hT, "hT", 1, 256, cons)
                flush()
                CK("mp_k")
                for u in range(2):
                    wv, wk = wload(wkv[:, :, 1024 + u * 512:1024 + (u + 1) * 512], NKC, 512)
                    for mb_ in range(2):
                        b = bank()
                        for k in range(NKC):
                            MM(ps[b][:, :], hT[:, k, mb_ * 128:(mb_ + 1) * 128], wv.row(k), k == 0, k == NKC - 1,
                               r=[wv.key(k), ("hT", 0)], w=[("ps", b)])
                        CP("act", vc[:, mb_, u * 512:(u + 1) * 512], ps[b][:, :], r=[("ps", b)], w=["kvc"])
                CK("mp_v")
                DMA("sp", kvscr[l], kvc[:, :], r=["kvc", "R"], w=[("kvscr", l)])
                FENCE("kvc")
                FENCE("SC")

        def mixer(ti, l, first_layer_in_launch):
            tok0 = ti * TT
            S.op("sp", lambda e: e.dma_start(out=kvc[:, :], in_=kvscr[l]), r=[("kvscr", l)], w=["kvc", "kvcR"], dma=dsem(), fence=True)
            if first_layer_in_launch:
                load_tokens_T(x_in[tok0:tok0 + TT, :], NB, xT, "xT")
                FENCE("Y")
            CK("load")
            rms_to_h(xT, "xT", gmix[:, l, :], 2, HT)
            if ti == 0 and l == layers[0]:
                TAP("hT", hT[:, :, :], r=[("hT", 0), ("hT", 1)])
            CK("rms")

            vgrep = SCf[:, 0:1024]
            brow = SCf[0:1, 1024:2048]
            DMA("sp", vgrep, v_gain[l].rearrange("g c -> (g c)").partition_broadcast(128), r=(), w=["SC", "vgrep"])
            DMA("sp", brow, b_spatial[l].rearrange("g t -> (g t)").unsqueeze(0), r=(), w=["SC", "brow"])

            def u_cons(u):
                def cons(b, c, hf):
                    ACTF(yA[:, u * 4 + c, hf * HT:(hf + 1) * HT], ps[b][:, 0:HT], AF.Gelu, r=[("ps", b)], w=["Y", ("yA", hf)])
                return cons
            for u in range(2):
                wv, wk = wload(w_in_unit(l, u * 512), NKC, 512)
                fm_unit(wv, wk, 4, NKC, hT, "hT", 2, HT, u_cons(u), hf_outer=(u == 0))
            CK("A_u")
            vg = [SCf[:, 2048 + i * 512:2048 + (i + 1) * 512] for i in range(2)]
            junk = SCb[:, 6144 + 1024:6144 + 1024 + 128]
            ssv = rt[:, 88:96]
            for u in range(2):
                wv, wk = wload(w_in_unit(l, 1024 + u * 512), NKC, 512)
                for blk in range(NB):
                    b = bank()
                    for k in range(NKC):
                        MM(ps[b][:, :], hT[:, k, blk * 128:(blk + 1) * 128], wv.row(k), k == 0, k == NKC - 1,
                           r=[wv.key(k), ("hT", blk // 3)], w=[("ps", b)])
                    i = (u * NB + blk) % 2
                    ACTF(vg[i], ps[b][:, :], AF.Gelu, r=[("ps", b)], w=[("vg", i), "SC"])
                    for g in range(4):
                        ACTF(junk, vg[i][:, g * 128:(g + 1) * 128], AF.Square, r=[("vg", i), "SC"], w=["junk", ("ssv", i)],
                             accum_out=ssv[:, i * 4 + g:i * 4 + g + 1])
                    ACTF(ssv[:, i * 4:(i + 1) * 4], ssv[:, i * 4:(i + 1) * 4], AF.Sqrt, r=[("ssv", i)], w=[("ssv", i)],
                         scale=1.0 / 128.0, bias=EPS)
                    RECIP(ssv[:, i * 4:(i + 1) * 4], ssv[:, i * 4:(i + 1) * 4], r=[("ssv", i)], w=[("ssv", i)])
                    for g in range(4):
                        gg = u * 4 + g
                        STT(vn[:, blk, gg * 128:(gg + 1) * 128], vg[i][:, g * 128:(g + 1) * 128], ssv[:, i * 4 + g:i * 4 + g + 1],
                            vgrep[:, gg * 128:(gg + 1) * 128], ALU.mult, ALU.mult,
                            r=[("vg", i), ("ssv", i), "vgrep", "SC"], w=["R", ("vn", blk // 3)])
            if ti == 0 and l == layers[0]:
                TAP("WmT", WmT[:, :, :, :], r=["WmT"])
                TAP("uT", yA, r=[("yA", 0), ("yA", 1)])
                TAP("vn", vn, r=[("vn", 0), ("vn", 1)])
            CK("A_v")
            for g in range(8):
                for hf in range(2):
                    b = bank()
                    for j in range(3):
                        MM(ps[b][:, j * 128:(j + 1) * 128], onesf[0:1, :], brow[:, g * 128:(g + 1) * 128], j == 0, False,
                           r=["onesf", "brow", "SC"], w=[("ps", b)])
                    for j in range(3):
                        blk = hf * 3 + j
                        MM(ps[b][:, j * 128:(j + 1) * 128], vn[:, blk, g * 128:(g + 1) * 128], WmT[:, l, g, :], False, j == 2,
                           r=[("vn", hf), "R", "WmT"], w=[("ps", b)])
                    TT_("dve", yA[:, g, hf * HT:(hf + 1) * HT], yA[:, g, hf * HT:(hf + 1) * HT], ps[b][:, 0:HT], ALU.mult,
                        r=[("ps", b), ("yA", hf)], w=["Y", ("yA", hf)])
            if ti == 0 and l == layers[0]:
                TAP("yA", yA, r=[("yA", 0), ("yA", 1)])
            FENCE("R")
            FENCE("SC")

            CK("A")
            CP("dve", kn2[:, :, 0:128], savekv[:, l, 0:512].rearrange("p (k t) -> p k t", t=128), r=["savekv"], w=["R", "kn2"])
            CP("dve", vb[:, 0, :], savekv[:, l, 512:768], r=["savekv"], w=["R", "vb"])
            for u in range(2):
                i = wslot()
                wview = wbuf[i][:, 0:NKC * 256].rearrange("p (k c) -> p k c", c=256)
                wk = ("w", i)
                src = w_in[l].rearrange("(kc p) n -> p kc n", p=128)
                for kv in range(2):
                    for dup in range(2):
                        c0 = 3072 + (u * 2 + kv) * 64
                        DMA("pool", wview[:, :, kv * 128 + dup * 64:kv * 128 + (dup + 1) * 64], src[:, :, c0:c0 + 64],
                            r=(), w=[wk], sem="w%d" % i)
                wv = WUnit([(wview, wk)], NKC)
                cons = qk_norm_consumer(1, bdiag[:, :], "bdiag", 1.0 / 64.0, lambda c, l=l: gkb[:, l:l + 1],
                                        lambda c, hf, u=u: kn2[:, u * 2 + c, 128 + hf * HT:128 + (hf + 1) * HT], "kn2", HT, 0)
                fm_unit(wv, wk, 2, NKC, hT, "hT", 2, HT, cons)
            flush()
            CK("B_k")
            wv, wk = wload(w_in_unit(l, 3328, 256), NKC, 256)
            for blk in range(NB):
                b = bank()
                for k in range(NKC):
                    MM(ps[b][:, 0:256], hT[:, k, blk * 128:(blk + 1) * 128], wv.row(k), k == 0, k == NKC - 1,
                       r=[wv.key(k), ("hT", blk // 3)], w=[("ps", b)])
                CP("act", vb[:, 1 + blk, :], ps[b][:, 0:256], r=[("ps", b)], w=["R", "vb"])
            CP("dve", savekv[:, l, 0:512].rearrange("p (k t) -> p k t", t=128), kn2[:, :, NB * 128:(NB + 1) * 128], r=["kn2", "R"], w=["savekv"])
            CP("dve", savekv[:, l, 512:768], vb[:, NB, :], r=["vb", "R"], w=["savekv"])
            CK("B_v")
            for u in range(2):
                wv, wk = wload(w_in_unit(l, 2048 + u * 512), NKC, 512)
                cons = qk_norm_consumer(1, bdiag[:, :], "bdiag", 1.0 / 64.0, lambda c, l=l: gqb[:, l:l + 1],
                                        lambda c, hf, u=u: qbuf[:, u * 4 + c, hf * HT:(hf + 1) * HT], "qbuf", HT, 0)
                fm_unit(wv, wk, 4, NKC, hT, "hT", 2, HT, cons)
            flush()
            if ti == 0 and l == layers[0]:
                TAP("qn", qbuf, r=["qbuf"])
                TAP("kn2", kn2, r=["kn2"])
            CK("B_q")
            PB = 6144
            Pbuf = [[SCb[:, PB + (i * 2 + j) * 512:PB + (i * 2 + j + 1) * 512] for j in range(2)] for i in range(2)]
            dnb = [SCf[:, 4096 + i * 256:4096 + (i + 1) * 256] for i in range(2)]
            step = [0]
            for blk in range(NB):
                for kvh in range(4):
                    i = step[0] % 2
                    step[0] += 1
                    gblk = ti * NB + blk
                    if gblk == 0:
                        mprev = 3
                    elif gblk == 2:
                        mprev = 2
                    else:
                        mprev = 1
                    for par in range(2):
                        b = bank()
                        MM(ps[b][:, 0:256], identb[:, :], mbt[:, 0, :].unsqueeze(1).to_broadcast([128, 2, 128]), True, False,
                           r=["identb", "mbt"], w=[("ps", b)])
                        MM(ps[b][:, 256:512], identb[:, :], mbt[:, mprev, :].unsqueeze(1).to_broadcast([128, 2, 128]), False, False,
                           r=["identb", "mbt"], w=[("ps", b)])
                        for which, slot in enumerate((1 + blk, blk)):
                            MM(ps[b][:, which * 256:(which + 1) * 256], kn2[par * 64:(par + 1) * 64, kvh, slot * 128:(slot + 1) * 128],
                               qbuf[par * 64:(par + 1) * 64, 2 * kvh:2 * kvh + 2, blk * 128:(blk + 1) * 128], False, which == 1,
                               r=["kn2", "qbuf", "R"], w=[("ps", b)])
                        ACTF(Pbuf[i][par], ps[b][:, :], AF.Exp, r=[("ps", b)], w=[("P", i, par), "SC"], scale=0.125)

                    def pv(i=i, blk=blk, kvh=kvh):
                        b = bank()
                        for par in range(2):
                            for which, slot in ((1, blk), (0, 1 + blk)):
                                MM(ps[b][par * 64:(par + 1) * 64, 0:256], vb[:, slot, kvh * 64:(kvh + 1) * 64],
                                   Pbuf[i][par][:, which * 256:(which + 1) * 256], which == 1, which == 0,
                                   r=["vb", "R", ("P", i, par), "SC"], w=[("ps", b)])
                            for which in (1, 0):
                                MM(ps[b][par * 64:(par + 1) * 64, 256:512], onesb[:, 0:64],
                                   Pbuf[i][par][:, which * 256:(which + 1) * 256], which == 1, which == 0,
                                   r=["onesb", ("P", i, par), "SC"], w=[("ps", b)])
                        for j in range(2):
                            TS("dve", dnb[i][:, j * 128:(j + 1) * 128], ps[b][:, 256 + j * 128:256 + (j + 1) * 128],
                               esink[:, l, 2 * kvh + j:2 * kvh + j + 1], None, ALU.add, None, r=[("ps", b), "esink"], w=[("dn", i), "SC"])
                        RECIP(dnb[i], dnb[i], r=[("dn", i)], w=[("dn", i), "SC"])
                        TT_("dve", yB[:, 2 * kvh:2 * kvh + 2, blk * 128:(blk + 1) * 128],
                            ps[b][:, 0:256].rearrange("p (j q) -> p j q", q=128), dnb[i].rearrange("p (j q) -> p j q", q=128), ALU.mult,
                            r=[("ps", b), ("dn", i), "SC"], w=["Y", ("yB", blk // 3)])
                    defer(pv)
                    flush(keep=1)
            flush()
            if ti == 0 and l == layers[0]:
                TAP("yB", yB, r=[("yB", 0), ("yB", 1)])
            FENCE("SC")

            CK("B")
            Pm = [[SCb[:, PB + (i * 2 + j) * HT:PB + (i * 2 + j + 1) * HT] for j in range(2)] for i in range(2)]
            rdb = [SCf[:, 4096 + i * HT:4096 + (i + 1) * HT] for i in range(1)]
            for u in range(2):
                wv, wk = wload(w_in_unit(l, 3584 + u * 512), NKC, 512)
                cons = qk_norm_consumer(2, onesb[:, :], "onesb", 1.0 / 256.0, lambda c, l=l: gqc[:, l, (c % 2):(c % 2) + 1],
                                        lambda c, hf: qbuf[:, c, hf * HT:(hf + 1) * HT], "qbuf", HT, 0)
                fm_unit(wv, wk, 4, NKC, hT, "hT", 2, HT, cons)
                flush()
                for hl in range(2):
                    hc = u * 2 + hl
                    for hf in range(2):
                        i = step[0] % 2
                        step[0] += 1
                        for m in range(2):
                            b = bank()
                            for dc in range(2):
                                MM(ps[b][:, 0:HT], kcT[:, 2 * hc + dc, m * 128:(m + 1) * 128], qbuf[:, 2 * hl + dc, hf * HT:(hf + 1) * HT],
                                   dc == 0, dc == 1, r=["kvc", "kvcR", "qbuf", "R"], w=[("ps", b)])
                            ACTF(Pm[i][m], ps[b][:, 0:HT], AF.Exp, r=[("ps", b)], w=[("P", i, m), "SC"], scale=1.0 / 16.0)

                        def pvc(i=i, hc=hc, hf=hf):
                            bd = bank()
                            for m in range(2):
                                MM(ps[bd][:, 0:HT], onesb[:, :], Pm[i][m], m == 0, m == 1, r=["onesb", ("P", i, m), "SC"], w=[("ps", bd)])
                            RECIP(rdb[0], ps[bd][:, 0:HT], r=[("ps", bd)], w=[("rd", 0), "SC"])
                            for dc in range(2):
                                b = bank()
                                for m in range(2):
                                    MM(ps[b][:, 0:HT], vc[:, m, hc * 256 + dc * 128:hc * 256 + (dc + 1) * 128], Pm[i][m], m == 0, m == 1,
                                       r=["kvc", "kvcR", ("P", i, m), "SC"], w=[("ps", b)])
                                TT_("dve", yC[:, 2 * hc + dc, hf * HT:(hf + 1) * HT], ps[b][:, 0:HT], rdb[0], ALU.mult,
                                    r=[("ps", b), ("rd", 0), "SC"], w=["Y", ("yC", hf)])
                        defer(pvc)
                        flush(keep=1)
                flush()
            if ti == 0 and l == layers[0]:
                TAP("yC", yC, r=[("yC", 0), ("yC", 1)])
            FENCE("R")
            FENCE("SC")

            CK("C")
            FENCE("kvcR")
            pacc = SCf[:, 0:4 * TT].rearrange("p (c t) -> p c t", t=TT)
            sgA = kvcf[:, 0:2 * TT].rearrange("p (c t) -> p c t", t=TT)
            sgB = SCf[:, 4 * TT:6 * TT].rearrange("p (c t) -> p c t", t=TT)
            tb = [SCf[:, 6 * TT + i * HT:6 * TT + (i + 1) * HT] for i in range(2)]
            ys = (yA, yB, yC)
            ykeys = ("yA", "yB", "yC")
            mstep = [0]

            def sg_ap(dcl, sl):
                return sgA[:, dcl, sl] if dcl < 2 else sgB[:, dcl - 2, sl]

            for d4 in range(4):
                for n in range(3):
                    wgv, wgk = wload(w_in_unit(l, 4608 + n * D + d4 * 512), NKC, 512)

                    def gcons(b_, c, hf):
                        sl = slice(hf * HT, (hf + 1) * HT)
                        ACTF(sg_ap(c, sl), ps[b_][:, 0:HT], AF.Sigmoid, r=[("ps", b_)], w=[("sg", c, hf), "SC", "kvcR"])
                    fm_unit(wgv, wgk, 4, NKC, hT, "hT", 2, HT, gcons)
                    wbv, wbk = wload(w_branch[l, n].rearrange("(wc p) d -> p wc d", p=128)[:, :, d4 * 512:(d4 + 1) * 512], 8, 512)

                    def wcons(b_, c, hf, n=n, d4=d4):
                        sl = slice(hf * HT, (hf + 1) * HT)
                        i = mstep[0] % 2
                        mstep[0] += 1
                        pk = ("pacc", c, hf)
                        if n == 0:
                            TT_("dve", pacc[:, c, sl], ps[b_][:, 0:HT], sg_ap(c, sl), ALU.mult,
                                r=[("ps", b_), ("sg", c, hf), "kvcR"], w=[pk, "SC"])
                        else:
                            TT_("dve", tb[i], ps[b_][:, 0:HT], sg_ap(c, sl), ALU.mult,
                                r=[("ps", b_), ("sg", c, hf), "kvcR"], w=[("tb", i), "SC"])
                            if n == 1:
                                TT_("dve", pacc[:, c, sl], pacc[:, c, sl], tb[i], ALU.add, r=[pk, ("tb", i)], w=[pk, "SC"])
                            else:
                                TT_("dve", mT[:, d4 * 4 + c, sl], pacc[:, c, sl], tb[i], ALU.add, r=[pk, ("tb", i), "SC"],
                                    w=["R", ("mT", hf)])
                    fm_unit(wbv, wbk, 4, 8, ys[n], ykeys[n], 2, HT, wcons)
            if ti == 0 and l == layers[0]:
                TAP("mT", mT, r=[("mT", 0), ("mT", 1)])
            CK("merge")
            for d4 in range(4):
                wv, wk = wload(w_out[l].rearrange("(kc p) n -> p kc n", p=128)[:, :, d4 * 512:(d4 + 1) * 512], NKC, 512)

                def ocons(b, c, hf, d4=d4):
                    dc = d4 * 4 + c
                    sl = slice(hf * HT, (hf + 1) * HT)
                    TT_("dve", xT[:, dc, sl], xT[:, dc, sl], ps[b][:, 0:HT], ALU.add, r=[("ps", b), ("xT", hf)], w=[("xT", hf)])
                fm_unit(wv, wk, 4, NKC, mT, "mT", 2, HT, ocons)
            if ti == 0 and l == layers[0]:
                TAP("x1", xT[:, :, :], r=[("xT", 0), ("xT", 1)])
            FENCE("R")
            FENCE("Y")
            FENCE("SC")

        def moe(ti, l, last_layer_in_launch):
            CK("mixer")
            DMA("pool", wr[:, :, 0:4], w_rg[l].rearrange("(kc p) g -> p kc g", p=128), r=(), w=["wr"], sem="wr",
                allow_slow_non_contiguous=True)
            DMA("pool", wr[:, :, 4:20], w_re[l].rearrange("(kc p) g -> p kc g", p=128), r=(), w=["wr"], sem="wr",
                allow_slow_non_contiguous=True)
            DMA("sp", rb[:, 0:4], b_rg[l].partition_broadcast(128), r=(), w=["rb"])
            DMA("sp", rb[:, 4:20], b_re[l].partition_broadcast(128), r=(), w=["rb"])
            rms_to_h(xT, "xT", gffn[:, l, :], 2, HT)
            for blk in range(NB):
                b = bank()
                for k in range(NKC):
                    MM(ps[b][:, 0:20], hT[:, k, blk * 128:(blk + 1) * 128], wr[:, k, :], k == 0, k == NKC - 1,
                       r=["wr", ("hT", blk // 3)], w=[("ps", b)])
                lg = rt[:, 0:20]
                gl = rt[:, 0:4]
                el = rt[:, 4:20]
                gmax = rt[:, 20:21]
                ngmax = rt[:, 21:22]
                oh = rt[:, 22:26]
                sumg = rt[:, 26:27]
                pen = rt[:, 27:31]
                m1 = rt[:, 31:32]
                m2 = rt[:, 32:33]
                dd = rt[:, 33:34]
                w1 = rt[:, 34:35]
                w2 = rt[:, 35:36]
                junk4 = rt[:, 36:40]
                em = rt[:, 40:56]
                oh1 = rt[:, 56:72]
                em2 = rt[:, 72:88]
                RT = ["rt"]
                TT_("dve", lg, ps[b][:, 0:20], rb[:, :], ALU.add, r=[("ps", b), "rb"], w=RT)
                RED(gmax, gl, ALU.max, r=RT, w=RT)
                TS("dve", oh, gl, gmax, None, ALU.is_equal, None, r=RT, w=RT)
                TS("dve", ngmax, gmax, -1.0, None, ALU.mult, None, r=RT, w=RT)
                ACTF(junk4, gl, AF.Exp, r=RT, w=RT, bias=ngmax, accum_out=sumg)
                TS("dve", pen, oh, -1.0, 1e9, ALU.add, ALU.mult, r=RT, w=RT)
                TT_("dve", em.rearrange("p (g j) -> p g j", j=4), el.rearrange("p (g j) -> p g j", j=4),
                    pen.unsqueeze(2).to_broadcast([128, 4, 4]), ALU.add, r=RT, w=RT)
                RED(m1, em, ALU.max, r=RT, w=RT)
                TS("dve", oh1, em, m1, None, ALU.is_equal, None, r=RT, w=RT)
                STT(em2, oh1, -1e9, em, ALU.mult, ALU.add, r=RT, w=RT)
                RED(m2, em2, ALU.max, r=RT, w=RT)
                TS("dve", em2, em2, m2, None, ALU.is_equal, None, r=RT, w=RT)
                TT_("dve", dd, m2, m1, ALU.subtract, r=RT, w=RT)
                ACTF(dd, dd, AF.Exp, r=RT, w=RT)
                TS("dve", w1, dd, 1.0, None, ALU.add, None, r=RT, w=RT)
                TT_("dve", w1, w1, sumg, ALU.mult, r=RT, w=RT)
                RECIP(w1, w1, r=RT, w=RT)
                TT_("dve", w2, w1, dd, ALU.mult, r=RT, w=RT)
                TS("dve", oh1, oh1, w1, None, ALU.mult, None, r=RT, w=RT)
                STT(gates[:, blk, :], em2, w2, oh1, ALU.mult, ALU.add, r=RT, w=["gates"])
            if ti == 0 and l == layers[0]:
                TAP("gates", gates[:, :, :], r=["gates"])
            CK("router")
            De = [SCf[:, i * 128:(i + 1) * 128] for i in range(3)]
            rep = SCf[:, 384:384 + TT]
            sgl = SCf[:, 1152:1152 + 4 * TT].rearrange("p (c t) -> p c t", t=TT)
            t1 = [SCf[:, 4224 + i * HT:4224 + (i + 1) * HT] for i in range(2)]
            dctr = [0]
            estep = [0]
            for G in range(4):
                for el_ in range(4):
                    e_ = G * 4 + el_
                    wgv, wgk = wload(w_gate_e[l, e_].rearrange("(kc p) f -> p kc f", p=128), NKC, 512)

                    def gcons(b_, c, hf):
                        ACTF(sgl[:, c, hf * HT:(hf + 1) * HT], ps[b_][:, 0:HT], AF.Silu, r=[("ps", b_)], w=[("sgl", c, hf), "SC"])
                    fm_unit(wgv, wgk, 4, NKC, hT, "hT", 2, HT, gcons, hf_outer=(e_ == 0))
                    for hf in range(2):
                        b = bank()
                        for j in range(3):
                            blk = hf * 3 + j
                            di = dctr[0] % 3
                            dctr[0] += 1
                            TS("dve", De[di], identf[:, :], gates[:, blk, e_:e_ + 1], None, ALU.mult, None,
                               r=["identf", "gates"], w=[("De", di), "SC"])
                            MM(ps[b][:, j * 128:(j + 1) * 128], onesf[:, :], De[di], True, True, r=["onesf", ("De", di), "SC"], w=[("ps", b)])
                        CP("act", rep[:, hf * HT:(hf + 1) * HT], ps[b][:, 0:HT], r=[("ps", b)], w=[("rep", hf), "SC"])
                    wuv, wuk = wload(w_up_e[l, e_].rearrange("(kc p) f -> p kc f", p=128), NKC, 512)

                    def ucons(b_, c, hf, el_=el_):
                        sl = slice(hf * HT, (hf + 1) * HT)
                        i = estep[0] % 2
                        estep[0] += 1
                        TT_("dve", t1[i], ps[b_][:, 0:HT], sgl[:, c, sl], ALU.mult, r=[("ps", b_), ("sgl", c, hf)], w=[("t1", i), "SC"])
                        TT_("dve", hidT[:, el_ * 4 + c, sl], t1[i], rep[:, sl], ALU.mult,
                            r=[("t1", i), ("rep", hf), "SC"], w=["Y", ("hid", hf)])
                    fm_unit(wuv, wuk, 4, NKC, hT, "hT", 2, HT, ucons)
                if ti == 0 and l == layers[0] and G == 0:
                    TAP("hid", hidT, r=[("hid", 0), ("hid", 1)])
                wd = w_down_e[l].rearrange("e f d -> (e f) d").rearrange("(k p) d -> p k d", p=128)
                for d4 in range(4):
                    wv, wk = wload(wd[:, G * 16:(G + 1) * 16, d4 * 512:(d4 + 1) * 512], 16, 512)

                    def dcons(b, c, hf, d4=d4):
                        dc = d4 * 4 + c
                        sl = slice(hf * HT, (hf + 1) * HT)
                        TT_("dve", xT[:, dc, sl], xT[:, dc, sl], ps[b][:, 0:HT], ALU.add, r=[("ps", b), ("xT", hf)], w=[("xT", hf)])
                    fm_unit(wv, wk, 4, 16, hidT, "hid", 2, HT, dcons)
            if ti == 0 and l == layers[0]:
                TAP("x2", xT[:, :, :], r=[("xT", 0), ("xT", 1)])
            FENCE("Y")
            FENCE("SC")
            CK("moe")
            if last_layer_in_launch:
                for blk in range(NB):
                    gtok = ti * TT + blk * 128
                    if gtok < out_tokens_from:
                        continue
                    st = stage[blk % 4]
                    skey = ("stage", blk % 4)
                    for k4 in range(4):
                        b = bank()
                        for j in range(4):
                            kc = k4 * 4 + j
                            TR(ps[b][:, j * 128:(j + 1) * 128], xT[:, kc, blk * 128:(blk + 1) * 128], identf[:, :],
                               r=[("xT", blk // 3), "identf"], w=[("ps", b)])
                        CP("act" if k4 % 2 else "dve", st[:, k4 * 512:(k4 + 1) * 512], ps[b][:, :], r=[("ps", b)], w=[skey, "Y"])
                    okey_ = ("out", len(out_keys))
                    out_keys.append(okey_)
                    DMA("sp", out[gtok - out_tokens_from:gtok - out_tokens_from + 128, :], st, r=[skey], w=[okey_])
                FENCE("Y")

        mem_prologue()
        CK("memprol")
        for ti in range(n_tiles):
            for li, l in enumerate(layers):
                mixer(ti, l, li == 0)
                moe(ti, l, li == len(layers) - 1)
        fin_keys = out_keys + ["tap_" + k for k in tap_aps]
        S.op("sp", lambda e: None, r=fin_keys, w=(), force=True)

        semnames = list(Sched.ENGS) + ["w%d" % i for i in range(NW)] + ["d%d" % i for i in range(6)] + ["wr"]
        sems = {n: es.enter_context(nc.semaphore("s_" + n)) for n in semnames}
        block = es.enter_context(nc.Block())
        S.emit(nc, block, sems)
    return nc


def _consts():
    ident = np.eye(128, dtype=np.float32)
    tril = np.tril(np.ones((128, 128), dtype=np.float32))
    j = np.arange(128)[:, None]
    i = np.arange(128)[None, :]
    cur = np.where(i >= j, 0.0, NEG).astype(np.float32)
    prev = np.where(i < j, 0.0, NEG).astype(np.float32)
    allm = np.full((128, 128), NEG, dtype=np.float32)
    return ident, tril, cur, prev, allm


WEIGHT_KEYS = ["norm_mix", "norm_mem", "norm_ffn", "w_in", "v_gain", "w_spatial", "b_spatial", "q_gain_b", "k_gain_b",
               "sinks", "q_gain_c", "k_gain_c", "w_mem_kv", "w_branch", "w_out", "w_router_group", "b_router_group",
               "w_router_expert", "b_router_expert", "w_gate_e", "w_up_e", "w_down_e"]

FUSED = True
_NC_CACHE = {}


def _get_nc(layers):
    key = tuple(layers)
    if key not in _NC_CACHE:
        _NC_CACHE[key] = build_program(list(layers))
    return _NC_CACHE[key]


def _in_maps(xfull, inputs, n_cores=8):
    ident, tril, cur, prev, allm = _consts()
    maps = []
    for c in range(n_cores):
        b, half = c // 2, c % 2
        xs = np.zeros((TOK, D), dtype=np.float32)
        if half == 0:
            xs[256:] = xfull[b, 0:2048]
            pm = allm
        else:
            xs[:] = xfull[b, 2048 - 256:4096]
            pm = prev
        m = {"x_in": xs, "mem_in": np.ascontiguousarray(inputs["mem"][b]),
             "c_ident": ident, "c_tril": tril,
             "c_mb": np.stack([cur, prev, pm, allm]).astype(ml_dtypes.bfloat16)}
        for k in WEIGHT_KEYS:
            m[k] = inputs[k]
        maps.append(m)
    return maps


def kernel(**inputs):
    inputs = {k: np.ascontiguousarray(np.asarray(v)) for k, v in inputs.items()}
    x = inputs["x"]
    launches = [[0, 1]] if FUSED else [[0], [1]]
    for layers in launches:
        nc = _get_nc(layers)
        res = run_bass_kernel_spmd(nc, _in_maps(x, inputs), core_ids=list(range(8)))
        xn = np.empty_like(x)
        for c in range(8):
            b, half = c // 2, c % 2
            xn[b, half * 2048:(half + 1) * 2048] = res.results[c]["out"]
        x = xn
    return x
```

```python
import numpy as np
import ml_dtypes
from contextlib import ExitStack
import concourse.bass as bass
import concourse.mybir as mybir
from concourse.bass_utils import run_bass_kernel_spmd

F32 = mybir.dt.float32
BF16 = mybir.dt.bfloat16
AF = mybir.ActivationFunctionType
ALU = mybir.AluOpType
AX = mybir.AxisListType

D = 2048
NKC = 16
L = 2
IN_W = 10752
NB = 6
TT = NB * 128
HT = TT // 2
NT = 3
TOK = NT * TT
EPS = 1e-6
NEG = -30000.0
NW = 4
SAME_ENG_SYNC = True


class Sched:
    ENGS = ("pe", "act", "dve", "pool", "sp")

    def __init__(self):
        self.ops = {e: [] for e in self.ENGS}
        self.last_w = {}
        self.readers = {}
        self.waited = {e: {} for e in self.ENGS}
        self.dma_val = {}
        self.signal = set()
        self.halted = False

    def _need(self, eng, tok, waits):
        src, v = tok
        if src == eng and (eng == "pe" or not SAME_ENG_SYNC):
            return
        if self.waited[eng].get(src, -1) >= v:
            return
        self.waited[eng][src] = v
        waits.append(tok)
        if src in self.ENGS:
            self.signal.add((src, v))

    REGIONS = ("SC", "Y", "R", "kvcR")

    def op(self, eng, fn, r=(), w=(), dma=None, fence=False, force=False):
        if self.halted and not force:
            return None
        if not fence:
            mv = [k for k in w if k in self.REGIONS]
            if mv:
                w = [k for k in w if k not in self.REGIONS]
                r = list(r) + mv
        waits = []
        for k in r:
            t = self.last_w.get(k)
            if t is not None:
                self._need(eng, t, waits)
            if isinstance(k, tuple) and k[0] == "ps":
                for src, t in self.readers.get(k, {}).items():
                    if src != eng:
                        self._need(eng, t, waits)
        for k in w:
            t = self.last_w.get(k)
            if t is not None:
                self._need(eng, t, waits)
            for t in self.readers.get(k, {}).values():
                self._need(eng, t, waits)
        idx = len(self.ops[eng])
        if dma is not None:
            prev = self.dma_val.get(dma, 0)
            if prev:
                self._need(eng, (dma, prev), waits)
            tok = (dma, prev + 16)
            self.dma_val[dma] = prev + 16
        else:
            tok = (eng, idx)
        self.ops[eng].append((fn, waits, dma))
        for k in r:
            self.readers.setdefault(k, {})[tok[0]] = tok
        for k in w:
            self.last_w[k] = tok
            self.readers[k] = {}
        return tok

    def emit(self, nc, block, sems):
        sigval = {}
        for e in self.ENGS:
            c = 0
            for i in range(len(self.ops[e])):
                if (e, i) in self.signal:
                    c += 1
                    sigval[(e, i)] = c
        ENGS = self.ENGS

        def run(e, eng):
            for i, (fn, waits, dma) in enumerate(self.ops[e]):
                for (src, v) in waits:
                    if src in ENGS:
                        eng.wait_ge(sems[src], sigval[(src, v)])
                    else:
                        eng.wait_ge(sems[src], v)
                ins = fn(eng)
                if ins is None:
                    continue
                if dma is not None:
                    ins.then_inc(sems[dma], 16)
                elif (e, i) in self.signal:
                    ins.then_inc(sems[e], 1)

        @block.tensor
        def _(eng):
            run("pe", eng)

        @block.scalar
        def _(eng):
            run("act", eng)

        @block.vector
        def _(eng):
            run("dve", eng)

        @block.gpsimd
        def _(eng):
            run("pool", eng)

        @block.sync
        def _(eng):
            run("sp", eng)


def build_program(layers, n_tiles=NT, taps=None, out_tokens_from=256, stop_after=None):
    taps = taps or {}
    nc = bass.Bass("TRN2", target_bir_lowering=False)
    S = Sched()

    def din(name, shape, dt=F32):
        return nc.dram_tensor(name, list(shape), dt, kind="ExternalInput").ap()

    x_in = din("x_in", [TOK, D])
    mem_in = din("mem_in", [256, D])
    norm_mix = din("norm_mix", [L, D])
    norm_mem = din("norm_mem", [L, D])
    norm_ffn = din("norm_ffn", [L, D])
    w_in = din("w_in", [L, D, IN_W])
    v_gain = din("v_gain", [L, 8, 128])
    w_spatial = din("w_spatial", [L, 8, 128, 128])
    b_spatial = din("b_spatial", [L, 8, 128])
    q_gain_b = din("q_gain_b", [L, 64])
    k_gain_b = din("k_gain_b", [L, 64])
    sinks = din("sinks", [L, 16])
    q_gain_c = din("q_gain_c", [L, 256])
    k_gain_c = din("k_gain_c", [L, 256])
    w_mem_kv = din("w_mem_kv", [L, D, D])
    w_branch = din("w_branch", [L, 3, 1024, D])
    w_out = din("w_out", [L, D, D])
    w_rg = din("w_router_group", [L, D, 4])
    b_rg = din("b_router_group", [L, 4])
    w_re = din("w_router_expert", [L, D, 16])
    b_re = din("b_router_expert", [L, 16])
    w_gate_e = din("w_gate_e", [L, 16, D, 512])
    w_up_e = din("w_up_e", [L, 16, D, 512])
    w_down_e = din("w_down_e", [L, 16, 512, D])
    c_ident = din("c_ident", [128, 128])
    c_tril = din("c_tril", [128, 128])
    c_mb = din("c_mb", [4, 128, 128], BF16)
    n_out_tok = n_tiles * TT - out_tokens_from
    out = nc.dram_tensor("out", [n_out_tok, D], F32, kind="ExternalOutput").ap()
    kvscr = nc.dram_tensor("kvscr", [L, 128, 4096], BF16, kind="Internal").ap()
    tap_aps = {k: nc.dram_tensor("tap_" + k, list(shp), dt, kind="ExternalOutput").ap() for k, (shp, dt) in taps.items()}

    es = ExitStack()
    with es:
        def sb(name, shape, dt):
            return es.enter_context(nc.sbuf_tensor(name, list(shape), dt))

        xT = sb("xT", [128, NKC, TT], F32)
        hT = sb("hT", [128, NKC, TT], BF16)
        Yr = sb("Yr", [128, 24 * TT], BF16)
        Rr = sb("Rr", [128, 16 * TT], BF16)
        wbuf = [sb("wbuf%d" % i, [128, 8 * 512], BF16) for i in range(NW)]
        SCb = sb("SC", [128, 10752], BF16)
        kvc = sb("kvc", [128, 4096], BF16)
        identf = sb("identf", [128, 128], F32)
        identb = sb("identb", [128, 128], BF16)
        onesb = sb("onesb", [128, 128], BF16)
        bdiag = sb("bdiag", [128, 128], BF16)
        onesf = sb("onesf", [128, 128], F32)
        mbt = sb("mbt", [128, 4, 128], BF16)
        gmix = sb("gmix", [128, L, NKC], F32)
        gffn = sb("gffn", [128, L, NKC], F32)
        gmem = sb("gmem", [128, L, NKC], F32)
        gqb = sb("gqb", [128, L], F32)
        gkb = sb("gkb", [128, L], F32)
        esink = sb("esink", [128, L, 8], F32)
        gqc = sb("gqc", [128, L, 2], F32)
        gkc = sb("gkc", [128, L, 2], F32)
        WmT = sb("WmT", [128, L, 8, 128], BF16)
        wr = sb("wr", [128, NKC, 20], BF16)
        rb = sb("rb", [128, 20], F32)
        gates = sb("gates", [128, NB, 16], F32)
        rt = sb("rt", [128, 96], F32)
        savekv = sb("savekv", [128, L, 768], BF16)
        dummy = sb("dmy0", [128, 8], F32)
        ps = [es.enter_context(nc.psum_tensor("ps%d" % i, [128, 512], F32)) for i in range(8)]

        Y4 = Yr[:, :].rearrange("p (c t) -> p c t", t=TT)
        yA, yB, yC = Y4[:, 0:8], Y4[:, 8:16], Y4[:, 16:24]
        hidT = Y4[:, 0:16]
        Ystage = Yr[:, :].bitcast(F32)
        stage = [Ystage[:, i * 2048:(i + 1) * 2048] for i in range(4)]
        mT = Rr[:, :].rearrange("p (c t) -> p c t", t=TT)
        vn = Rr[:, 0:NB * 1024].rearrange("p (b c) -> p b c", c=1024)
        kn2 = Rr[:, 0:4 * 896].rearrange("p (k t) -> p k t", t=896)
        vb = Rr[:, 4 * 896:4 * 896 + 7 * 256].rearrange("p (s c) -> p s c", c=256)
        qbuf = Rr[:, 8 * TT:16 * TT].rearrange("p (c t) -> p c t", t=TT)
        kcT = kvc[:, 0:2048].rearrange("p (c m) -> p c m", m=256)
        vc = kvc[:, 2048:4096].rearrange("p (m c) -> p m c", c=1024)
        SCf = SCb[:, :].bitcast(F32)
        kvcf = kvc[:, :].bitcast(F32)
        RMS0 = 4608

        def CK(name):
            if stop_after == name:
                S.halted = True

        bank_ctr = [0]

        def bank():
            i = bank_ctr[0] % 8
            bank_ctr[0] += 1
            return i

        dsem_ctr = [0]

        def dsem():
            i = dsem_ctr[0] % 6
            dsem_ctr[0] += 1
            return "d%d" % i

        def MM(out, lhsT, rhs, start, stop, r, w, **kw):
            return S.op("pe", lambda e: e.matmul(out, lhsT=lhsT, rhs=rhs, start=start, stop=stop, **kw), r, w)

        def TR(out, in_, ident, r, w):
            return S.op("pe", lambda e: e.transpose(out=out, in_=in_, identity=ident), r, w)

        def ACTF(out, in_, func, r, w, **kw):
            return S.op("act", lambda e: e.activation(out=out, in_=in_, func=func, **kw), r, w)

        def TT_(eng, out, in0, in1, op, r, w):
            return S.op(eng, lambda e: e.tensor_tensor(out=out, in0=in0, in1=in1, op=op), r, w)

        def TS(eng, out, in0, s1, s2, op0, op1, r, w):
            if op1 is None:
                return S.op(eng, lambda e: e.tensor_scalar(out=out, in0=in0, scalar1=s1, scalar2=None, op0=op0), r, w)
            return S.op(eng, lambda e: e.tensor_scalar(out=out, in0=in0, scalar1=s1, scalar2=s2, op0=op0, op1=op1), r, w)

        def STT(out, in0, scalar, in1, op0, op1, r, w):
            return S.op("dve", lambda e: e.scalar_tensor_tensor(out=out, in0=in0, scalar=scalar, in1=in1, op0=op0, op1=op1), r, w)

        def CP(eng, out, in_, r, w):
            if eng == "act":
                return S.op("act", lambda e: e.copy(out=out, in_=in_), r, w)
            return S.op(eng, lambda e: e.tensor_copy(out=out, in_=in_), r, w)

        def RECIP(out, in_, r, w):
            return S.op("dve", lambda e: e.reciprocal(out=out, in_=in_), r, w)

        def RED(out, in_, op, r, w):
            return S.op("dve", lambda e: e.tensor_reduce(out=out, in_=in_, axis=AX.X, op=op), r, w)

        def DMA(q, out, in_, r, w, sem=None, **kw):
            return S.op(q, lambda e: e.dma_start(out=out, in_=in_, **kw), r, w, dma=sem or dsem())

        def FENCE(key, eng="dve"):
            return S.op(eng, lambda e: e.memset(dummy[:, 0:1], 0.0), r=(), w=[key, "dummy_" + eng], fence=True)

        def TAP(name, src, r):
            if name in tap_aps:
                DMA("sp", tap_aps[name], src, r=r, w=["tap_" + name])

        wctr = [0]

        class WUnit:
            def __init__(self, parts, kh):
                self.parts = parts
                self.kh = kh

            def row(self, k):
                return self.parts[k // self.kh][0][:, k % self.kh, :]

            def key(self, k):
                return self.parts[k // self.kh][1]

            def keys(self, k):
                kk = self.parts[k // self.kh][1]
                return list(kk) if isinstance(kk, list) else [kk]

        def wslot():
            i = wctr[0] % NW
            wctr[0] += 1
            return i

        def wload(src_ap, kparts, cols):
            nparts = 1 if kparts * cols <= 4096 else 2
            kh = kparts // nparts
            parts = []
            for pi in range(nparts):
                i = wslot()
                view = wbuf[i][:, 0:kh * cols].rearrange("p (k c) -> p k c", c=cols)
                DMA("pool", view, src_ap[:, pi * kh:(pi + 1) * kh, :], r=(), w=[("w", i)], sem="w%d" % i)
                parts.append((view, ("w", i)))
            return WUnit(parts, kh), None

        def w_in_unit(l, c0, cols=512):
            return w_in[l].rearrange("(kc p) n -> p kc n", p=128)[:, :, c0:c0 + cols]

        DMA("sp", identf[:, :], c_ident[:, :], r=(), w=["identf"])
        DMA("sp", SCf[:, 0:128], c_tril[:, :], r=(), w=["tril", "SC"])
        DMA("sp", mbt[:, :, :], c_mb.rearrange("m k q -> k m q"), r=(), w=["mbt"])
        CP("dve", identb[:, :], identf[:, :], r=["identf"], w=["identb"])
        S.op("dve", lambda e: e.memset(onesb[:, :], 1.0), w=["onesb"])
        S.op("dve", lambda e: e.memset(onesf[:, :], 1.0), w=["onesf"])
        S.op("dve", lambda e: e.memset(bdiag[:, :], 0.0), w=["bdiag"])
        S.op("dve", lambda e: e.memset(bdiag[0:64, 0:64], 1.0), w=["bdiag"])
        S.op("dve", lambda e: e.memset(bdiag[64:128, 64:128], 1.0), w=["bdiag"])
        S.op("dve", lambda e: e.memset(savekv[:, :, :], 0.0), w=["savekv"])
        for (dst, src) in ((gmix, norm_mix), (gffn, norm_ffn), (gmem, norm_mem)):
            for l in range(L):
                DMA("sp", dst[:, l, :], src[l].rearrange("(kc p) -> p kc", p=128), r=(), w=["gains"],
                    allow_slow_non_contiguous=True)
        for l in range(L):
            for hf in range(2):
                DMA("sp", gqb[hf * 64:(hf + 1) * 64, l:l + 1], q_gain_b[l].rearrange("(d o) -> d o", o=1), r=(), w=["gains"])
                DMA("sp", gkb[hf * 64:(hf + 1) * 64, l:l + 1], k_gain_b[l].rearrange("(d o) -> d o", o=1), r=(), w=["gains"])
                DMA("sp", esink[hf * 64:(hf + 1) * 64, l, :],
                    sinks[l].rearrange("(c two) -> two c", two=2)[hf].partition_broadcast(64), r=(), w=["esink"],
                    allow_slow_non_contiguous=True)
            DMA("sp", gqc[:, l, :], q_gain_c[l].rearrange("(dc p) -> p dc", p=128), r=(), w=["gains"], allow_slow_non_contiguous=True)
            DMA("sp", gkc[:, l, :], k_gain_c[l].rearrange("(dc p) -> p dc", p=128), r=(), w=["gains"], allow_slow_non_contiguous=True)
        ACTF(esink[:, :, :], esink[:, :, :], AF.Exp, r=["esink"], w=["esink"])
        wsp = SCf[:, 128:128 + 1024].rearrange("p (g s) -> p g s", s=128)
        for l in range(L):
            DMA("sp", wsp, w_spatial[l].rearrange("g t s -> t g s"), r=(), w=["wsp", "SC"])
            TT_("dve", wsp, wsp, SCf[:, 0:128].unsqueeze(1).to_broadcast([128, 8, 128]), ALU.mult, r=["tril", "wsp"], w=["wsp", "SC"])
            for g4 in range(2):
                b = bank()
                for j in range(4):
                    g = g4 * 4 + j
                    TR(ps[b][:, j * 128:(j + 1) * 128], wsp[:, g, :], identf[:, :], r=["wsp", "SC", "identf"], w=[("ps", b)])
                CP("dve", WmT[:, l, g4 * 4:(g4 + 1) * 4, :], ps[b][:, :].rearrange("p (j t) -> p j t", t=128),
                   r=[("ps", b)], w=["WmT"])
        FENCE("SC")

        CK("params")
        def load_tokens_T(src_rows, nblk, dstT, dkey):
            for blk in range(nblk):
                st = stage[blk % 4]
                skey = ("stage", blk % 4)
                DMA("sp", st, src_rows[blk * 128:(blk + 1) * 128, :], r=["Y"], w=[skey])
                for k4 in range(4):
                    b = bank()
                    for j in range(4):
                        kc = k4 * 4 + j
                        TR(ps[b][:, j * 128:(j + 1) * 128], st[:, kc * 128:(kc + 1) * 128], identf[:, :],
                           r=[skey, "identf"], w=[("ps", b)])
                    CP("act" if k4 % 2 else "dve", dstT[:, k4 * 4:(k4 + 1) * 4, blk * 128:(blk + 1) * 128],
                       ps[b][:, :].rearrange("p (j t) -> p j t", t=128), r=[("ps", b)], w=[(dkey, blk * 128 // HT)])

        def rms_to_h(srcT, skey, gain_ap, halves, hw, ranges=None):
            rstd = SCf[:, 0:TT]
            sqr = [SCb[:, 2 * TT + i * HT:2 * TT + (i + 1) * HT] for i in range(3)]
            if ranges is None:
                ranges = [(hf * hw, hw) for hf in range(halves)]
            for hf in range(len(ranges)):
                st_, hw = ranges[hf]
                sl = slice(st_, st_ + hw)
                b = bank()
                for kc in range(NKC):
                    sq = sqr[kc % 3]
                    ACTF(sq[:, 0:hw], srcT[:, kc, sl], AF.Square, r=[(skey, hf)], w=[("sq", kc % 3), "SC"])
                    MM(ps[b][:, 0:hw], onesb[:, :], sq[:, 0:hw], kc == 0, kc == NKC - 1, r=[("sq", kc % 3), "onesb", "SC"], w=[("ps", b)])
                ACTF(rstd[:, sl], ps[b][:, 0:hw], AF.Sqrt, r=[("ps", b)], w=[("rstd", hf), "SC"], scale=1.0 / D, bias=EPS)
                RECIP(rstd[:, sl], rstd[:, sl], r=[("rstd", hf)], w=[("rstd", hf), "SC"])
                for kc in range(NKC):
                    STT(hT[:, kc, sl], srcT[:, kc, sl], gain_ap[:, kc:kc + 1], rstd[:, sl], ALU.mult, ALU.mult,
                        r=[(skey, hf), ("rstd", hf), "gains", "SC"], w=[("hT", hf)])
            FENCE("SC")

        deferred = []
        out_keys = []

        def defer(fn):
            deferred.append(fn)

        def flush(keep=0):
            while len(deferred) > keep:
                deferred.pop(0)()

        def fm_unit(wv, wkey, nchunks, ksz, rhsT, rkey, halves, hw, consumer, hf_outer=False, ranges=None):
            if ranges is None:
                ranges = [(hf * hw, hw) for hf in range(halves)]
            nh = len(ranges)
            order = [(c, hf) for hf in range(nh) for c in range(nchunks)] if hf_outer else \
                    [(c, hf) for c in range(nchunks) for hf in range(nh)]
            for (c, hf) in order:
                st_, w_ = ranges[hf]
                b = bank()
                for k in range(ksz):
                    MM(ps[b][:, 0:w_], wv.row(k)[:, c * 128:(c + 1) * 128], rhsT[:, k, st_:st_ + w_], k == 0, k == ksz - 1,
                       r=wv.keys(k) + [(rkey, hf)], w=[("ps", b)])
                consumer(b, c, hf)

        def qk_norm_consumer(nsum, ones_ap, okey, invd, gain_fn, dst_fn, dkey, hw, scbase, ranges=None):
            st = {}
            cnt = [0]

            def consumer(b, c, hf):
                i = cnt[0]
                cnt[0] += 1
                slot = i % 4
                if any(slot in getattr(f, "slots", ()) for f in deferred):
                    flush()
                w_ = hw if ranges is None else ranges[hf][1]
                sqb = SCb[:, scbase + slot * hw: scbase + slot * hw + w_]
                qraw = SCf[:, (scbase + 4 * hw) // 2 + slot * hw:(scbase + 4 * hw) // 2 + slot * hw + w_]
                ACTF(sqb, ps[b][:, 0:w_], AF.Square, r=[("ps", b)], w=[("sqb", slot), "SC"])
                CP("dve", qraw, ps[b][:, 0:w_], r=[("ps", b)], w=[("qraw", slot), "SC"])
                st.setdefault(hf, []).append((c, slot))
                if len(st[hf]) == nsum:
                    group = st.pop(hf)

                    def fin(group=group, hf=hf, w_=w_):
                        b2 = bank()
                        for j, (c_, s_) in enumerate(group):
                            MM(ps[b2][:, 0:w_], ones_ap, SCb[:, scbase + s_ * hw: scbase + s_ * hw + w_], j == 0, j == len(group) - 1,
                               r=[("sqb", s_), okey, "SC"], w=[("ps", b2)])
                        ro = (scbase + 12 * hw) // 2 + (group[0][1] % 2) * hw
                        rq = SCf[:, ro:ro + w_]
                        rk = ("rq", group[0][1] % 2)
                        ACTF(rq, ps[b2][:, 0:w_], AF.Sqrt, r=[("ps", b2)], w=[rk, "SC"], scale=invd, bias=EPS)
                        RECIP(rq, rq, r=[rk], w=[rk, "SC"])
                        for (c_, s_) in group:
                            qo = (scbase + 4 * hw) // 2 + s_ * hw
                            qr = SCf[:, qo:qo + w_]
                            STT(dst_fn(c_, hf), qr, gain_fn(c_), rq, ALU.mult, ALU.mult, r=[("qraw", s_), rk, "gains", "SC"], w=["R", dkey])
                    fin.slots = tuple(s_ for (_, s_) in group)
                    defer(fin)
                    flush(keep=1)
            return consumer

        def mem_prologue():
            load_tokens_T(mem_in, 2, xT, "xT")
            CK("mp_load")
            for l in range(L):
                rms_to_h(xT, "xT", gmem[:, l, :], 1, 256)
                CK("mp_rms")
                wkv = w_mem_kv[l].rearrange("(kc p) n -> p kc n", p=128)
                for u in range(2):
                    wv, wk = wload(wkv[:, :, u * 512:(u + 1) * 512], NKC, 512)
                    cons = qk_norm_consumer(2, onesb[:, :], "onesb", 1.0 / 256.0,
                                            lambda c, l=l: gkc[:, l, (c % 2):(c % 2) + 1],
                                            lambda c, hf, u=u: kcT[:, u * 4 + c, :], "kvc", 256, 0)
                    fm_unit(wv, wk, 4, NKC, hT, "hT", 1, 256, cons)
                flush()
                CK("mp_k")
                for u in range(2):
                    wv, wk = wload(wkv[:, :, 1024 + u * 512:1024 + (u + 1) * 512], NKC, 512)
                    for mb_ in range(2):
                        b = bank()
                        for k in range(NKC):
                            MM(ps[b][:, :], hT[:, k, mb_ * 128:(mb_ + 1) * 128], wv.row(k), k == 0, k == NKC - 1,
                               r=[wv.key(k), ("hT", 0)], w=[("ps", b)])
                        CP("act", vc[:, mb_, u * 512:(u + 1) * 512], ps[b][:, :], r=[("ps", b)], w=["kvc"])
                CK("mp_v")
                DMA("sp", kvscr[l], kvc[:, :], r=["kvc", "R"], w=[("kvscr", l)])
                FENCE("kvc")
                FENCE("SC")

        def mixer(ti, l, first_layer_in_launch, last_layer_in_launch):
            tok0 = ti * TT
            skf = (2 if last_layer_in_launch else 1) if ti == 0 else 0
            skk = max(skf - 1, 0)
            FR = [(skf * 128, HT - skf * 128), (HT, HT)]
            KR = [(skk * 128, HT - skk * 128), (HT, HT)]
            S.op("sp", lambda e: e.dma_start(out=kvc[:, :], in_=kvscr[l]), r=[("kvscr", l)], w=["kvc", "kvcR"], dma=dsem(), fence=True)
            if first_layer_in_launch:
                load_tokens_T(x_in[tok0:tok0 + TT, :], NB, xT, "xT")
                FENCE("Y")
            CK("load")
            rms_to_h(xT, "xT", gmix[:, l, :], 2, HT, ranges=KR)
            if ti == 0 and l == layers[0]:
                TAP("hT", hT[:, :, :], r=[("hT", 0), ("hT", 1)])
            CK("rms")

            vgrep = SCf[:, 0:1024]
            brow = SCf[0:1, 1024:2048]
            DMA("sp", vgrep, v_gain[l].rearrange("g c -> (g c)").partition_broadcast(128), r=(), w=["SC", "vgrep"])
            DMA("sp", brow, b_spatial[l].rearrange("g t -> (g t)").unsqueeze(0), r=(), w=["SC", "brow"])

            def u_cons(u):
                def cons(b, c, hf):
                    st_, w_ = FR[hf]
                    ACTF(yA[:, u * 4 + c, st_:st_ + w_], ps[b][:, 0:w_], AF.Gelu, r=[("ps", b)], w=["Y", ("yA", hf)])
                return cons
            for u in range(2):
                wv, wk = wload(w_in_unit(l, u * 512), NKC, 512)
                fm_unit(wv, wk, 4, NKC, hT, "hT", 2, HT, u_cons(u), hf_outer=(u == 0), ranges=FR)
            CK("A_u")
            vg = [SCf[:, 2048 + i * 512:2048 + (i + 1) * 512] for i in range(2)]
            junk = SCb[:, 6144 + 1024:6144 + 1024 + 128]
            ssv = rt[:, 88:96]
            for u in range(2):
                wv, wk = wload(w_in_unit(l, 1024 + u * 512), NKC, 512)
                for blk in range(skf, NB):
                    b = bank()
                    for k in range(NKC):
                        MM(ps[b][:, :], hT[:, k, blk * 128:(blk + 1) * 128], wv.row(k), k == 0, k == NKC - 1,
                           r=[wv.key(k), ("hT", blk // 3)], w=[("ps", b)])
                    i = (u * NB + blk) % 2
                    ACTF(vg[i], ps[b][:, :], AF.Gelu, r=[("ps", b)], w=[("vg", i), "SC"])
                    for g in range(4):
                        ACTF(junk, vg[i][:, g * 128:(g + 1) * 128], AF.Square, r=[("vg", i), "SC"], w=["junk", ("ssv", i)],
                             accum_out=ssv[:, i * 4 + g:i * 4 + g + 1])
                    ACTF(ssv[:, i * 4:(i + 1) * 4], ssv[:, i * 4:(i + 1) * 4], AF.Sqrt, r=[("ssv", i)], w=[("ssv", i)],
                         scale=1.0 / 128.0, bias=EPS)
                    RECIP(ssv[:, i * 4:(i + 1) * 4], ssv[:, i * 4:(i + 1) * 4], r=[("ssv", i)], w=[("ssv", i)])
                    for g in range(4):
                        gg = u * 4 + g
                        STT(vn[:, blk, gg * 128:(gg + 1) * 128], vg[i][:, g * 128:(g + 1) * 128], ssv[:, i * 4 + g:i * 4 + g + 1],
                            vgrep[:, gg * 128:(gg + 1) * 128], ALU.mult, ALU.mult,
                            r=[("vg", i), ("ssv", i), "vgrep", "SC"], w=["R", ("vn", blk // 3)])
            if ti == 0 and l == layers[0]:
                TAP("WmT", WmT[:, :, :, :], r=["WmT"])
                TAP("uT", yA, r=[("yA", 0), ("yA", 1)])
                TAP("vn", vn, r=[("vn", 0), ("vn", 1)])
            CK("A_v")
            for g in range(8):
                for hf in range(2):
                    st_, w_ = FR[hf]
                    js = [j for j in range(3) if hf * 3 + j >= skf]
                    b = bank()
                    for j in js:
                        MM(ps[b][:, j * 128:(j + 1) * 128], onesf[0:1, :], brow[:, g * 128:(g + 1) * 128], j == js[0], False,
                           r=["onesf", "brow", "SC"], w=[("ps", b)])
                    for j in js:
                        blk = hf * 3 + j
                        MM(ps[b][:, j * 128:(j + 1) * 128], vn[:, blk, g * 128:(g + 1) * 128], WmT[:, l, g, :], False, j == js[-1],
                           r=[("vn", hf), "R", "WmT"], w=[("ps", b)])
                    TT_("dve", yA[:, g, st_:st_ + w_], yA[:, g, st_:st_ + w_], ps[b][:, st_ - hf * HT:st_ - hf * HT + w_], ALU.mult,
                        r=[("ps", b), ("yA", hf)], w=["Y", ("yA", hf)])
            if ti == 0 and l == layers[0]:
                TAP("yA", yA, r=[("yA", 0), ("yA", 1)])
            FENCE("R")
            FENCE("SC")

            CK("A")
            CP("dve", kn2[:, :, 0:128], savekv[:, l, 0:512].rearrange("p (k t) -> p k t", t=128), r=["savekv"], w=["R", "kn2"])
            CP("dve", vb[:, 0, :], savekv[:, l, 512:768], r=["savekv"], w=["R", "vb"])
            for u in range(2):
                i = wslot()
                wview = wbuf[i][:, 0:NKC * 256].rearrange("p (k c) -> p k c", c=256)
                wk = ("w", i)
                src = w_in[l].rearrange("(kc p) n -> p kc n", p=128)
                pkeys = [wk]
                for kv in range(2):
                    for dup in range(2):
                        c0 = 3072 + (u * 2 + kv) * 64
                        pj = kv * 2 + dup
                        pkey = ("wkp", i, pj)
                        pkeys.append(pkey)
                        DMA("pool", wview[:, :, kv * 128 + dup * 64:kv * 128 + (dup + 1) * 64], src[:, :, c0:c0 + 64],
                            r=(), w=([wk, pkey] if pj == 0 else [pkey]), sem=("w%d" % i) if pj == 0 else "wk%d" % pj)
                wv = WUnit([(wview, pkeys)], NKC)
                cons = qk_norm_consumer(1, bdiag[:, :], "bdiag", 1.0 / 64.0, lambda c, l=l: gkb[:, l:l + 1],
                                        lambda c, hf, u=u: kn2[:, u * 2 + c, 128 + KR[hf][0]:128 + KR[hf][0] + KR[hf][1]], "kn2", HT, 0,
                                        ranges=KR)
                fm_unit(wv, wk, 2, NKC, hT, "hT", 2, HT, cons, ranges=KR)
            flush()
            CK("B_k")
            wv, wk = wload(w_in_unit(l, 3328, 256), NKC, 256)
            for blk in range(skk, NB):
                b = bank()
                for k in range(NKC):
                    MM(ps[b][:, 0:256], hT[:, k, blk * 128:(blk + 1) * 128], wv.row(k), k == 0, k == NKC - 1,
                       r=[wv.key(k), ("hT", blk // 3)], w=[("ps", b)])
                CP("act", vb[:, 1 + blk, :], ps[b][:, 0:256], r=[("ps", b)], w=["R", "vb"])
            CP("dve", savekv[:, l, 0:512].rearrange("p (k t) -> p k t", t=128), kn2[:, :, NB * 128:(NB + 1) * 128], r=["kn2", "R"], w=["savekv"])
            CP("dve", savekv[:, l, 512:768], vb[:, NB, :], r=["vb", "R"], w=["savekv"])
            CK("B_v")
            for u in range(2):
                wv, wk = wload(w_in_unit(l, 2048 + u * 512), NKC, 512)
                cons = qk_norm_consumer(1, bdiag[:, :], "bdiag", 1.0 / 64.0, lambda c, l=l: gqb[:, l:l + 1],
                                        lambda c, hf, u=u: qbuf[:, u * 4 + c, FR[hf][0]:FR[hf][0] + FR[hf][1]], "qbuf", HT, 0, ranges=FR)
                fm_unit(wv, wk, 4, NKC, hT, "hT", 2, HT, cons, ranges=FR)
            flush()
            if ti == 0 and l == layers[0]:
                TAP("qn", qbuf, r=["qbuf"])
                TAP("kn2", kn2, r=["kn2"])
            CK("B_q")
            PB = 6144
            Pbuf = [[SCb[:, PB + (i * 2 + j) * 512:PB + (i * 2 + j + 1) * 512] for j in range(2)] for i in range(2)]
            dnb = [SCf[:, 4096 + i * 256:4096 + (i + 1) * 256] for i in range(2)]
            step = [0]
            for blk in range(skf, NB):
                for kvh in range(4):
                    i = step[0] % 2
                    step[0] += 1
                    gblk = ti * NB + blk
                    if gblk == 0:
                        mprev = 3
                    elif gblk == 2:
                        mprev = 2
                    else:
                        mprev = 1
                    for par in range(2):
                        b = bank()
                        MM(ps[b][:, 0:256], identb[:, :], mbt[:, 0, :].unsqueeze(1).to_broadcast([128, 2, 128]), True, False,
                           r=["identb", "mbt"], w=[("ps", b)])
                        MM(ps[b][:, 256:512], identb[:, :], mbt[:, mprev, :].unsqueeze(1).to_broadcast([128, 2, 128]), False, False,
                           r=["identb", "mbt"], w=[("ps", b)])
                        for which, slot in enumerate((1 + blk, blk)):
                            MM(ps[b][:, which * 256:(which + 1) * 256], kn2[par * 64:(par + 1) * 64, kvh, slot * 128:(slot + 1) * 128],
                               qbuf[par * 64:(par + 1) * 64, 2 * kvh:2 * kvh + 2, blk * 128:(blk + 1) * 128], False, which == 1,
                               r=["kn2", "qbuf", "R"], w=[("ps", b)])
                        ACTF(Pbuf[i][par], ps[b][:, :], AF.Exp, r=[("ps", b)], w=[("P", i, par), "SC"], scale=0.125)

                    def pv(i=i, blk=blk, kvh=kvh):
                        b = bank()
                        for par in range(2):
                            for which, slot in ((1, blk), (0, 1 + blk)):
                                MM(ps[b][par * 64:(par + 1) * 64, 0:256], vb[:, slot, kvh * 64:(kvh + 1) * 64],
                                   Pbuf[i][par][:, which * 256:(which + 1) * 256], which == 1, which == 0,
                                   r=["vb", "R", ("P", i, par), "SC"], w=[("ps", b)])
                            for which in (1, 0):
                                MM(ps[b][par * 64:(par + 1) * 64, 256:512], onesb[:, 0:64],
                                   Pbuf[i][par][:, which * 256:(which + 1) * 256], which == 1, which == 0,
                                   r=["onesb", ("P", i, par), "SC"], w=[("ps", b)])
                        for j in range(2):
                            TS("dve", dnb[i][:, j * 128:(j + 1) * 128], ps[b][:, 256 + j * 128:256 + (j + 1) * 128],
                               esink[:, l, 2 * kvh + j:2 * kvh + j + 1], None, ALU.add, None, r=[("ps", b), "esink"], w=[("dn", i), "SC"])
                        RECIP(dnb[i], dnb[i], r=[("dn", i)], w=[("dn", i), "SC"])
                        TT_("dve", yB[:, 2 * kvh:2 * kvh + 2, blk * 128:(blk + 1) * 128],
                            ps[b][:, 0:256].rearrange("p (j q) -> p j q", q=128), dnb[i].rearrange("p (j q) -> p j q", q=128), ALU.mult,
                            r=[("ps", b), ("dn", i), "SC"], w=["Y", ("yB", blk // 3)])
                    defer(pv)
                    flush(keep=1)
            flush()
            if ti == 0 and l == layers[0]:
                TAP("yB", yB, r=[("yB", 0), ("yB", 1)])
            FENCE("SC")

            CK("B")
            Pm = [[SCb[:, PB + (i * 2 + j) * HT:PB + (i * 2 + j + 1) * HT] for j in range(2)] for i in range(2)]
            rdb = [SCf[:, 4096 + i * HT:4096 + (i + 1) * HT] for i in range(1)]
            for u in range(2):
                wv, wk = wload(w_in_unit(l, 3584 + u * 512), NKC, 512)
                cons = qk_norm_consumer(2, onesb[:, :], "onesb", 1.0 / 256.0, lambda c, l=l: gqc[:, l, (c % 2):(c % 2) + 1],
                                        lambda c, hf: qbuf[:, c, FR[hf][0]:FR[hf][0] + FR[hf][1]], "qbuf", HT, 0, ranges=FR)
                fm_unit(wv, wk, 4, NKC, hT, "hT", 2, HT, cons, ranges=FR)
                flush()
                for hl in range(2):
                    hc = u * 2 + hl
                    for hf in range(2):
                        i = step[0] % 2
                        step[0] += 1
                        st_, w_ = FR[hf]
                        for m in range(2):
                            b = bank()
                            for dc in range(2):
                                MM(ps[b][:, 0:w_], kcT[:, 2 * hc + dc, m * 128:(m + 1) * 128], qbuf[:, 2 * hl + dc, st_:st_ + w_],
                                   dc == 0, dc == 1, r=["kvc", "kvcR", "qbuf", "R"], w=[("ps", b)])
                            ACTF(Pm[i][m][:, 0:w_], ps[b][:, 0:w_], AF.Exp, r=[("ps", b)], w=[("P", i, m), "SC"], scale=1.0 / 16.0)

                        def pvc(i=i, hc=hc, hf=hf, st_=st_, w_=w_):
                            bd = bank()
                            for m in range(2):
                                MM(ps[bd][:, 0:w_], onesb[:, :], Pm[i][m][:, 0:w_], m == 0, m == 1, r=["onesb", ("P", i, m), "SC"], w=[("ps", bd)])
                            RECIP(rdb[0][:, 0:w_], ps[bd][:, 0:w_], r=[("ps", bd)], w=[("rd", 0), "SC"])
                            for dc in range(2):
                                b = bank()
                                for m in range(2):
                                    MM(ps[b][:, 0:w_], vc[:, m, hc * 256 + dc * 128:hc * 256 + (dc + 1) * 128], Pm[i][m][:, 0:w_], m == 0, m == 1,
                                       r=["kvc", "kvcR", ("P", i, m), "SC"], w=[("ps", b)])
                                TT_("dve", yC[:, 2 * hc + dc, st_:st_ + w_], ps[b][:, 0:w_], rdb[0][:, 0:w_], ALU.mult,
                                    r=[("ps", b), ("rd", 0), "SC"], w=["Y", ("yC", hf)])
                        defer(pvc)
                        flush(keep=1)
                flush()
            if ti == 0 and l == layers[0]:
                TAP("yC", yC, r=[("yC", 0), ("yC", 1)])
            FENCE("R")
            FENCE("SC")

            CK("C")
            FENCE("kvcR")
            pacc = SCf[:, 0:4 * TT].rearrange("p (c t) -> p c t", t=TT)
            sgA = kvcf[:, 0:2 * TT].rearrange("p (c t) -> p c t", t=TT)
            sgB = SCf[:, 4 * TT:6 * TT].rearrange("p (c t) -> p c t", t=TT)
            tb = [SCf[:, 6 * TT + i * HT:6 * TT + (i + 1) * HT] for i in range(2)]
            ys = (yA, yB, yC)
            ykeys = ("yA", "yB", "yC")
            mstep = [0]

            def sg_ap(dcl, sl):
                return sgA[:, dcl, sl] if dcl < 2 else sgB[:, dcl - 2, sl]

            for d4 in range(4):
                for n in range(3):
                    wgv, wgk = wload(w_in_unit(l, 4608 + n * D + d4 * 512), NKC, 512)

                    def gcons(b_, c, hf):
                        st_, w_ = FR[hf]
                        sl = slice(st_, st_ + w_)
                        ACTF(sg_ap(c, sl), ps[b_][:, 0:w_], AF.Sigmoid, r=[("ps", b_)], w=[("sg", c, hf), "SC", "kvcR"])
                    fm_unit(wgv, wgk, 4, NKC, hT, "hT", 2, HT, gcons, ranges=FR)
                    wbv, wbk = wload(w_branch[l, n].rearrange("(wc p) d -> p wc d", p=128)[:, :, d4 * 512:(d4 + 1) * 512], 8, 512)

                    def wcons(b_, c, hf, n=n, d4=d4):
                        st_, w_ = FR[hf]
                        sl = slice(st_, st_ + w_)
                        i = mstep[0] % 2
                        mstep[0] += 1
                        pk = ("pacc", c, hf)
                        if n == 0:
                            TT_("dve", pacc[:, c, sl], ps[b_][:, 0:w_], sg_ap(c, sl), ALU.mult,
                                r=[("ps", b_), ("sg", c, hf), "kvcR"], w=[pk, "SC"])
                        else:
                            TT_("dve", tb[i][:, 0:w_], ps[b_][:, 0:w_], sg_ap(c, sl), ALU.mult,
                                r=[("ps", b_), ("sg", c, hf), "kvcR"], w=[("tb", i), "SC"])
                            if n == 1:
                                TT_("dve", pacc[:, c, sl], pacc[:, c, sl], tb[i][:, 0:w_], ALU.add, r=[pk, ("tb", i)], w=[pk, "SC"])
                            else:
                                TT_("dve", mT[:, d4 * 4 + c, sl], pacc[:, c, sl], tb[i][:, 0:w_], ALU.add, r=[pk, ("tb", i), "SC"],
                                    w=["R", ("mT", hf)])
                    fm_unit(wbv, wbk, 4, 8, ys[n], ykeys[n], 2, HT, wcons, ranges=FR)
            if ti == 0 and l == layers[0]:
                TAP("mT", mT, r=[("mT", 0), ("mT", 1)])
            CK("merge")
            for d4 in range(4):
                wv, wk = wload(w_out[l].rearrange("(kc p) n -> p kc n", p=128)[:, :, d4 * 512:(d4 + 1) * 512], NKC, 512)

                def ocons(b, c, hf, d4=d4):
                    dc = d4 * 4 + c
                    st_, w_ = FR[hf]
                    sl = slice(st_, st_ + w_)
                    TT_("dve", xT[:, dc, sl], xT[:, dc, sl], ps[b][:, 0:w_], ALU.add, r=[("ps", b), ("xT", hf)], w=[("xT", hf)])
                fm_unit(wv, wk, 4, NKC, mT, "mT", 2, HT, ocons, ranges=FR)
            if ti == 0 and l == layers[0]:
                TAP("x1", xT[:, :, :], r=[("xT", 0), ("xT", 1)])
            FENCE("R")
            FENCE("Y")
            FENCE("SC")

        def moe(ti, l, last_layer_in_launch):
            CK("mixer")
            skipb = (2 if last_layer_in_launch else 1) if ti == 0 else 0
            MR = [(skipb * 128, HT - skipb * 128), (HT, HT)]
            DMA("pool", wr[:, :, 0:4], w_rg[l].rearrange("(kc p) g -> p kc g", p=128), r=(), w=["wr"], sem="wr",
                allow_slow_non_contiguous=True)
            DMA("pool", wr[:, :, 4:20], w_re[l].rearrange("(kc p) g -> p kc g", p=128), r=(), w=["wr"], sem="wr",
                allow_slow_non_contiguous=True)
            DMA("sp", rb[:, 0:4], b_rg[l].partition_broadcast(128), r=(), w=["rb"])
            DMA("sp", rb[:, 4:20], b_re[l].partition_broadcast(128), r=(), w=["rb"])
            rms_to_h(xT, "xT", gffn[:, l, :], 2, HT, ranges=MR)
            for blk in range(skipb, NB):
                b = bank()
                for k in range(NKC):
                    MM(ps[b][:, 0:20], hT[:, k, blk * 128:(blk + 1) * 128], wr[:, k, :], k == 0, k == NKC - 1,
                       r=["wr", ("hT", blk // 3)], w=[("ps", b)])
                lg = rt[:, 0:20]
                gl = rt[:, 0:4]
                el = rt[:, 4:20]
                gmax = rt[:, 20:21]
                ngmax = rt[:, 21:22]
                oh = rt[:, 22:26]
                sumg = rt[:, 26:27]
                pen = rt[:, 27:31]
                m1 = rt[:, 31:32]
                m2 = rt[:, 32:33]
                dd = rt[:, 33:34]
                w1 = rt[:, 34:35]
                w2 = rt[:, 35:36]
                junk4 = rt[:, 36:40]
                em = rt[:, 40:56]
                oh1 = rt[:, 56:72]
                em2 = rt[:, 72:88]
                RT = ["rt"]
                TT_("dve", lg, ps[b][:, 0:20], rb[:, :], ALU.add, r=[("ps", b), "rb"], w=RT)
                RED(gmax, gl, ALU.max, r=RT, w=RT)
                TS("dve", oh, gl, gmax, None, ALU.is_equal, None, r=RT, w=RT)
                TS("dve", ngmax, gmax, -1.0, None, ALU.mult, None, r=RT, w=RT)
                ACTF(junk4, gl, AF.Exp, r=RT, w=RT, bias=ngmax, accum_out=sumg)
                TS("dve", pen, oh, -1.0, 1e9, ALU.add, ALU.mult, r=RT, w=RT)
                TT_("dve", em.rearrange("p (g j) -> p g j", j=4), el.rearrange("p (g j) -> p g j", j=4),
                    pen.unsqueeze(2).to_broadcast([128, 4, 4]), ALU.add, r=RT, w=RT)
                RED(m1, em, ALU.max, r=RT, w=RT)
                TS("dve", oh1, em, m1, None, ALU.is_equal, None, r=RT, w=RT)
                STT(em2, oh1, -1e9, em, ALU.mult, ALU.add, r=RT, w=RT)
                RED(m2, em2, ALU.max, r=RT, w=RT)
                TS("dve", em2, em2, m2, None, ALU.is_equal, None, r=RT, w=RT)
                TT_("dve", dd, m2, m1, ALU.subtract, r=RT, w=RT)
                ACTF(dd, dd, AF.Exp, r=RT, w=RT)
                TS("dve", w1, dd, 1.0, None, ALU.add, None, r=RT, w=RT)
                TT_("dve", w1, w1, sumg, ALU.mult, r=RT, w=RT)
                RECIP(w1, w1, r=RT, w=RT)
                TT_("dve", w2, w1, dd, ALU.mult, r=RT, w=RT)
                TS("dve", oh1, oh1, w1, None, ALU.mult, None, r=RT, w=RT)
                STT(gates[:, blk, :], em2, w2, oh1, ALU.mult, ALU.add, r=RT, w=["gates"])
            if ti == 0 and l == layers[0]:
                TAP("gates", gates[:, :, :], r=["gates"])
            CK("router")
            De = [SCf[:, i * 128:(i + 1) * 128] for i in range(3)]
            rep = SCf[:, 384:384 + TT]
            sgl = SCf[:, 1152:1152 + 4 * TT].rearrange("p (c t) -> p c t", t=TT)
            t1 = [SCf[:, 4224 + i * HT:4224 + (i + 1) * HT] for i in range(2)]
            dctr = [0]
            estep = [0]
            for G in range(4):
                for el_ in range(4):
                    e_ = G * 4 + el_
                    wgv, wgk = wload(w_gate_e[l, e_].rearrange("(kc p) f -> p kc f", p=128), NKC, 512)

                    def gcons(b_, c, hf):
                        st_, w_ = MR[hf]
                        ACTF(sgl[:, c, st_:st_ + w_], ps[b_][:, 0:w_], AF.Silu, r=[("ps", b_)], w=[("sgl", c, hf), "SC"])
                    fm_unit(wgv, wgk, 4, NKC, hT, "hT", 2, HT, gcons, hf_outer=(e_ == 0), ranges=MR)
                    for hf in range(2):
                        st_, w_ = MR[hf]
                        b = bank()
                        for j in range(3):
                            blk = hf * 3 + j
                            if blk < skipb:
                                continue
                            di = dctr[0] % 3
                            dctr[0] += 1
                            TS("dve", De[di], identf[:, :], gates[:, blk, e_:e_ + 1], None, ALU.mult, None,
                               r=["identf", "gates"], w=[("De", di), "SC"])
                            MM(ps[b][:, j * 128:(j + 1) * 128], onesf[:, :], De[di], True, True, r=["onesf", ("De", di), "SC"], w=[("ps", b)])
                        CP("act", rep[:, st_:st_ + w_], ps[b][:, st_ - hf * HT:st_ - hf * HT + w_], r=[("ps", b)], w=[("rep", hf), "SC"])
                    wuv, wuk = wload(w_up_e[l, e_].rearrange("(kc p) f -> p kc f", p=128), NKC, 512)

                    def ucons(b_, c, hf, el_=el_):
                        st_, w_ = MR[hf]
                        sl = slice(st_, st_ + w_)
                        i = estep[0] % 2
                        estep[0] += 1
                        TT_("dve", t1[i][:, 0:w_], ps[b_][:, 0:w_], sgl[:, c, sl], ALU.mult, r=[("ps", b_), ("sgl", c, hf)], w=[("t1", i), "SC"])
                        TT_("dve", hidT[:, el_ * 4 + c, sl], t1[i][:, 0:w_], rep[:, sl], ALU.mult,
                            r=[("t1", i), ("rep", hf), "SC"], w=["Y", ("hid", hf)])
                    fm_unit(wuv, wuk, 4, NKC, hT, "hT", 2, HT, ucons, ranges=MR)
                if ti == 0 and l == layers[0] and G == 0:
                    TAP("hid", hidT, r=[("hid", 0), ("hid", 1)])
                wd = w_down_e[l].rearrange("e f d -> (e f) d").rearrange("(k p) d -> p k d", p=128)
                for d4 in range(4):
                    wv, wk = wload(wd[:, G * 16:(G + 1) * 16, d4 * 512:(d4 + 1) * 512], 16, 512)

                    def dcons(b, c, hf, d4=d4):
                        dc = d4 * 4 + c
                        st_, w_ = MR[hf]
                        sl = slice(st_, st_ + w_)
                        TT_("dve", xT[:, dc, sl], xT[:, dc, sl], ps[b][:, 0:w_], ALU.add, r=[("ps", b), ("xT", hf)], w=[("xT", hf)])
                    fm_unit(wv, wk, 4, 16, hidT, "hid", 2, HT, dcons, ranges=MR)
            if ti == 0 and l == layers[0]:
                TAP("x2", xT[:, :, :], r=[("xT", 0), ("xT", 1)])
            FENCE("Y")
            FENCE("SC")
            CK("moe")
            if last_layer_in_launch:
                for blk in range(NB):
                    gtok = ti * TT + blk * 128
                    if gtok < out_tokens_from:
                        continue
                    st = stage[blk % 4]
                    skey = ("stage", blk % 4)
                    for k4 in range(4):
                        b = bank()
                        for j in range(4):
                            kc = k4 * 4 + j
                            TR(ps[b][:, j * 128:(j + 1) * 128], xT[:, kc, blk * 128:(blk + 1) * 128], identf[:, :],
                               r=[("xT", blk // 3), "identf"], w=[("ps", b)])
                        CP("act" if k4 % 2 else "dve", st[:, k4 * 512:(k4 + 1) * 512], ps[b][:, :], r=[("ps", b)], w=[skey, "Y"])
                    okey_ = ("out", len(out_keys))
                    out_keys.append(okey_)
                    DMA("sp", out[gtok - out_tokens_from:gtok - out_tokens_from + 128, :], st, r=[skey], w=[okey_])
                FENCE("Y")

        mem_prologue()
        CK("memprol")
        for ti in range(n_tiles):
            for li, l in enumerate(layers):
                mixer(ti, l, li == 0, li == len(layers) - 1)
                moe(ti, l, li == len(layers) - 1)
        fin_keys = out_keys + ["tap_" + k for k in tap_aps]
        S.op("sp", lambda e: None, r=fin_keys, w=(), force=True)

        semnames = list(Sched.ENGS) + ["w%d" % i for i in range(NW)] + ["d%d" % i for i in range(6)] + ["wr", "wk1", "wk2", "wk3"]
        sems = {n: es.enter_context(nc.semaphore("s_" + n)) for n in semnames}
        block = es.enter_context(nc.Block())
        S.emit(nc, block, sems)
    return nc


def _consts():
    ident = np.eye(128, dtype=np.float32)
    tril = np.tril(np.ones((128, 128), dtype=np.float32))
    j = np.arange(128)[:, None]
    i = np.arange(128)[None, :]
    cur = np.where(i >= j, 0.0, NEG).astype(np.float32)
    prev = np.where(i < j, 0.0, NEG).astype(np.float32)
    allm = np.full((128, 128), NEG, dtype=np.float32)
    return ident, tril, cur, prev, allm


WEIGHT_KEYS = ["norm_mix", "norm_mem", "norm_ffn", "w_in", "v_gain", "w_spatial", "b_spatial", "q_gain_b", "k_gain_b",
               "sinks", "q_gain_c", "k_gain_c", "w_mem_kv", "w_branch", "w_out", "w_router_group", "b_router_group",
               "w_router_expert", "b_router_expert", "w_gate_e", "w_up_e", "w_down_e"]

FUSED = True
_NC_CACHE = {}


def _get_nc(layers):
    key = tuple(layers)
    if key not in _NC_CACHE:
        _NC_CACHE[key] = build_program(list(layers))
    return _NC_CACHE[key]


def _in_maps(xfull, inputs, n_cores=8):
    ident, tril, cur, prev, allm = _consts()
    maps = []
    for c in range(n_cores):
        b, half = c // 2, c % 2
        xs = np.zeros((TOK, D), dtype=np.float32)
        if half == 0:
            xs[256:] = xfull[b, 0:2048]
            pm = allm
        else:
            xs[:] = xfull[b, 2048 - 256:4096]
            pm = prev
        m = {"x_in": xs, "mem_in": np.ascontiguousarray(inputs["mem"][b]),
             "c_ident": ident, "c_tril": tril,
             "c_mb": np.stack([cur, prev, pm, allm]).astype(ml_dtypes.bfloat16)}
        for k in WEIGHT_KEYS:
            m[k] = inputs[k]
        maps.append(m)
    return maps


def kernel(**inputs):
    inputs = {k: np.ascontiguousarray(np.asarray(v)) for k, v in inputs.items()}
    x = inputs["x"]
    launches = [[0, 1]] if FUSED else [[0], [1]]
    for layers in launches:
        nc = _get_nc(layers)
        res = run_bass_kernel_spmd(nc, _in_maps(x, inputs), core_ids=list(range(8)))
        xn = np.empty_like(x)
        for c in range(8):
            b, half = c // 2, c % 2
            xn[b, half * 2048:(half + 1) * 2048] = res.results[c]["out"]
        x = xn
    return x
```

```python
import numpy as np
import ml_dtypes
from contextlib import ExitStack
import concourse.bass as bass
import concourse.mybir as mybir
from concourse.bass_utils import run_bass_kernel_spmd

F32 = mybir.dt.float32
BF16 = mybir.dt.bfloat16
AF = mybir.ActivationFunctionType
ALU = mybir.AluOpType
AX = mybir.AxisListType

D = 2048
NKC = 16
L = 2
IN_W = 10752
NB = 6
TT = NB * 128
HT = TT // 2
NT = 3
TOK = NT * TT
EPS = 1e-6
NEG = -30000.0
NW = 4
SAME_ENG_SYNC = True


class Sched:
    ENGS = ("pe", "act", "dve", "pool", "sp")

    def __init__(self):
        self.ops = {e: [] for e in self.ENGS}
        self.last_w = {}
        self.readers = {}
        self.waited = {e: {} for e in self.ENGS}
        self.dma_val = {}
        self.signal = set()
        self.halted = False

    def _need(self, eng, tok, waits):
        src, v = tok
        if src == eng and (eng == "pe" or not SAME_ENG_SYNC):
            return
        if self.waited[eng].get(src, -1) >= v:
            return
        self.waited[eng][src] = v
        waits.append(tok)
        if src in self.ENGS:
            self.signal.add((src, v))

    REGIONS = ("SC", "Y", "R", "kvcR")

    def op(self, eng, fn, r=(), w=(), dma=None, fence=False, force=False):
        if self.halted and not force:
            return None
        if not fence:
            mv = [k for k in w if k in self.REGIONS]
            if mv:
                w = [k for k in w if k not in self.REGIONS]
                r = list(r) + mv
        waits = []
        for k in r:
            t = self.last_w.get(k)
            if t is not None:
                self._need(eng, t, waits)
            if isinstance(k, tuple) and k[0] == "ps":
                for src, t in self.readers.get(k, {}).items():
                    if src != eng:
                        self._need(eng, t, waits)
        for k in w:
            t = self.last_w.get(k)
            if t is not None:
                self._need(eng, t, waits)
            for t in self.readers.get(k, {}).values():
                self._need(eng, t, waits)
        idx = len(self.ops[eng])
        if dma is not None:
            prev = self.dma_val.get(dma, 0)
            if prev:
                self._need(eng, (dma, prev), waits)
            tok = (dma, prev + 16)
            self.dma_val[dma] = prev + 16
        else:
            tok = (eng, idx)
        self.ops[eng].append((fn, waits, dma))
        for k in r:
            self.readers.setdefault(k, {})[tok[0]] = tok
        for k in w:
            self.last_w[k] = tok
            self.readers[k] = {}
        return tok

    def emit(self, nc, block, sems):
        sigval = {}
        for e in self.ENGS:
            c = 0
            for i in range(len(self.ops[e])):
                if (e, i) in self.signal:
                    c += 1
                    sigval[(e, i)] = c
        ENGS = self.ENGS

        def run(e, eng):
            for i, (fn, waits, dma) in enumerate(self.ops[e]):
                for (src, v) in waits:
                    if src in ENGS:
                        eng.wait_ge(sems[src], sigval[(src, v)])
                    else:
                        eng.wait_ge(sems[src], v)
                ins = fn(eng)
                if ins is None:
                    continue
                if dma is not None:
                    ins.then_inc(sems[dma], 16)
                elif (e, i) in self.signal:
                    ins.then_inc(sems[e], 1)

        @block.tensor
        def _(eng):
            run("pe", eng)

        @block.scalar
        def _(eng):
            run("act", eng)

        @block.vector
        def _(eng):
            run("dve", eng)

        @block.gpsimd
        def _(eng):
            run("pool", eng)

        @block.sync
        def _(eng):
            run("sp", eng)


def build_program(layers, n_tiles=NT, taps=None, out_tokens_from=256, stop_after=None):
    taps = taps or {}
    nc = bass.Bass("TRN2", target_bir_lowering=False)
    S = Sched()

    def din(name, shape, dt=F32):
        return nc.dram_tensor(name, list(shape), dt, kind="ExternalInput").ap()

    x_in = din("x_in", [TOK, D])
    mem_in = din("mem_in", [256, D])
    norm_mix = din("norm_mix", [L, D])
    norm_mem = din("norm_mem", [L, D])
    norm_ffn = din("norm_ffn", [L, D])
    w_in = din("w_in", [L, D, IN_W])
    v_gain = din("v_gain", [L, 8, 128])
    w_spatial = din("w_spatial", [L, 8, 128, 128])
    b_spatial = din("b_spatial", [L, 8, 128])
    q_gain_b = din("q_gain_b", [L, 64])
    k_gain_b = din("k_gain_b", [L, 64])
    sinks = din("sinks", [L, 16])
    q_gain_c = din("q_gain_c", [L, 256])
    k_gain_c = din("k_gain_c", [L, 256])
    w_mem_kv = din("w_mem_kv", [L, D, D])
    w_branch = din("w_branch", [L, 3, 1024, D])
    w_out = din("w_out", [L, D, D])
    w_rg = din("w_router_group", [L, D, 4])
    b_rg = din("b_router_group", [L, 4])
    w_re = din("w_router_expert", [L, D, 16])
    b_re = din("b_router_expert", [L, 16])
    w_gate_e = din("w_gate_e", [L, 16, D, 512])
    w_up_e = din("w_up_e", [L, 16, D, 512])
    w_down_e = din("w_down_e", [L, 16, 512, D])
    c_ident = din("c_ident", [128, 128])
    c_tril = din("c_tril", [128, 128])
    c_mb = din("c_mb", [4, 128, 128], BF16)
    n_out_tok = n_tiles * TT - out_tokens_from
    out = nc.dram_tensor("out", [n_out_tok, D], F32, kind="ExternalOutput").ap()
    kvscr = nc.dram_tensor("kvscr", [L, 128, 4096], BF16, kind="Internal").ap()
    tap_aps = {k: nc.dram_tensor("tap_" + k, list(shp), dt, kind="ExternalOutput").ap() for k, (shp, dt) in taps.items()}

    es = ExitStack()
    with es:
        def sb(name, shape, dt):
            return es.enter_context(nc.sbuf_tensor(name, list(shape), dt))

        xT = sb("xT", [128, NKC, TT], F32)
        hT = sb("hT", [128, NKC, TT], BF16)
        Yr = sb("Yr", [128, 24 * TT], BF16)
        Rr = sb("Rr", [128, 16 * TT], BF16)
        wbuf = [sb("wbuf%d" % i, [128, 8 * 512], BF16) for i in range(NW)]
        SCb = sb("SC", [128, 10752], BF16)
        kvc = sb("kvc", [128, 4096], BF16)
        identf = sb("identf", [128, 128], F32)
        identb = sb("identb", [128, 128], BF16)
        onesb = sb("onesb", [128, 128], BF16)
        bdiag = sb("bdiag", [128, 128], BF16)
        onesf = sb("onesf", [128, 128], F32)
        mbt = sb("mbt", [128, 4, 128], BF16)
        gmix = sb("gmix", [128, L, NKC], F32)
        gffn = sb("gffn", [128, L, NKC], F32)
        gmem = sb("gmem", [128, L, NKC], F32)
        gqb = sb("gqb", [128, L], F32)
        gkb = sb("gkb", [128, L], F32)
        esink = sb("esink", [128, L, 8], F32)
        gqc = sb("gqc", [128, L, 2], F32)
        gkc = sb("gkc", [128, L, 2], F32)
        WmT = sb("WmT", [128, L, 8, 128], BF16)
        wr = sb("wr", [128, NKC, 20], BF16)
        rb = sb("rb", [128, 20], F32)
        gates = sb("gates", [128, NB, 16], F32)
        rt = sb("rt", [128, 96], F32)
        savekv = sb("savekv", [128, L, 768], BF16)
        dummy = sb("dmy0", [128, 8], F32)
        ps = [es.enter_context(nc.psum_tensor("ps%d" % i, [128, 512], F32)) for i in range(8)]

        Y4 = Yr[:, :].rearrange("p (c t) -> p c t", t=TT)
        yA, yB, yC = Y4[:, 0:8], Y4[:, 8:16], Y4[:, 16:24]
        hidT = Y4[:, 0:16]
        Ystage = Yr[:, :].bitcast(F32)
        stage = [Ystage[:, i * 2048:(i + 1) * 2048] for i in range(4)]
        mT = Rr[:, :].rearrange("p (c t) -> p c t", t=TT)
        vn = Rr[:, 0:NB * 1024].rearrange("p (b c) -> p b c", c=1024)
        kn2 = Rr[:, 0:4 * 896].rearrange("p (k t) -> p k t", t=896)
        vb = Rr[:, 4 * 896:4 * 896 + 7 * 256].rearrange("p (s c) -> p s c", c=256)
        qbuf = Rr[:, 8 * TT:16 * TT].rearrange("p (c t) -> p c t", t=TT)
        kcT = kvc[:, 0:2048].rearrange("p (c m) -> p c m", m=256)
        vc = kvc[:, 2048:4096].rearrange("p (m c) -> p m c", c=1024)
        SCf = SCb[:, :].bitcast(F32)
        kvcf = kvc[:, :].bitcast(F32)
        RMS0 = 4608

        def CK(name):
            if stop_after == name:
                S.halted = True

        bank_ctr = [0]

        def bank():
            i = bank_ctr[0] % 8
            bank_ctr[0] += 1
            return i

        dsem_ctr = [0]

        def dsem():
            i = dsem_ctr[0] % 6
            dsem_ctr[0] += 1
            return "d%d" % i

        def MM(out, lhsT, rhs, start, stop, r, w, **kw):
            return S.op("pe", lambda e: e.matmul(out, lhsT=lhsT, rhs=rhs, start=start, stop=stop, **kw), r, w)

        def TR(out, in_, ident, r, w):
            return S.op("pe", lambda e: e.transpose(out=out, in_=in_, identity=ident), r, w)

        def ACTF(out, in_, func, r, w, **kw):
            return S.op("act", lambda e: e.activation(out=out, in_=in_, func=func, **kw), r, w)

        def TT_(eng, out, in0, in1, op, r, w):
            return S.op(eng, lambda e: e.tensor_tensor(out=out, in0=in0, in1=in1, op=op), r, w)

        def TS(eng, out, in0, s1, s2, op0, op1, r, w):
            if op1 is None:
                return S.op(eng, lambda e: e.tensor_scalar(out=out, in0=in0, scalar1=s1, scalar2=None, op0=op0), r, w)
            return S.op(eng, lambda e: e.tensor_scalar(out=out, in0=in0, scalar1=s1, scalar2=s2, op0=op0, op1=op1), r, w)

        def STT(out, in0, scalar, in1, op0, op1, r, w):
            return S.op("dve", lambda e: e.scalar_tensor_tensor(out=out, in0=in0, scalar=scalar, in1=in1, op0=op0, op1=op1), r, w)

        def CP(eng, out, in_, r, w):
            if eng == "act":
                return S.op("act", lambda e: e.copy(out=out, in_=in_), r, w)
            return S.op(eng, lambda e: e.tensor_copy(out=out, in_=in_), r, w)

        def RECIP(out, in_, r, w):
            return S.op("dve", lambda e: e.reciprocal(out=out, in_=in_), r, w)

        def RED(out, in_, op, r, w):
            return S.op("dve", lambda e: e.tensor_reduce(out=out, in_=in_, axis=AX.X, op=op), r, w)

        def DMA(q, out, in_, r, w, sem=None, **kw):
            return S.op(q, lambda e: e.dma_start(out=out, in_=in_, **kw), r, w, dma=sem or dsem())

        def FENCE(key, eng="dve"):
            return S.op(eng, lambda e: e.memset(dummy[:, 0:1], 0.0), r=(), w=[key, "dummy_" + eng], fence=True)

        def TAP(name, src, r):
            if name in tap_aps:
                DMA("sp", tap_aps[name], src, r=r, w=["tap_" + name])

        wctr = [0]

        class WUnit:
            def __init__(self, parts, kh):
                self.parts = parts
                self.kh = kh

            def row(self, k):
                return self.parts[k // self.kh][0][:, k % self.kh, :]

            def key(self, k):
                return self.parts[k // self.kh][1]

            def keys(self, k):
                kk = self.parts[k // self.kh][1]
                return list(kk) if isinstance(kk, list) else [kk]

        def wslot():
            i = wctr[0] % NW
            wctr[0] += 1
            return i

        def wload(src_ap, kparts, cols):
            nparts = 1 if kparts * cols <= 4096 else 2
            kh = kparts // nparts
            parts = []
            for pi in range(nparts):
                i = wslot()
                view = wbuf[i][:, 0:kh * cols].rearrange("p (k c) -> p k c", c=cols)
                DMA("pool", view, src_ap[:, pi * kh:(pi + 1) * kh, :], r=(), w=[("w", i)], sem="w%d" % i)
                parts.append((view, ("w", i)))
            return WUnit(parts, kh), None

        def w_in_unit(l, c0, cols=512):
            return w_in[l].rearrange("(kc p) n -> p kc n", p=128)[:, :, c0:c0 + cols]

        DMA("sp", identf[:, :], c_ident[:, :], r=(), w=["identf"])
        DMA("sp", SCf[:, 0:128], c_tril[:, :], r=(), w=["tril", "SC"])
        DMA("sp", mbt[:, :, :], c_mb.rearrange("m k q -> k m q"), r=(), w=["mbt"])
        CP("dve", identb[:, :], identf[:, :], r=["identf"], w=["identb"])
        S.op("dve", lambda e: e.memset(onesb[:, :], 1.0), w=["onesb"])
        S.op("dve", lambda e: e.memset(onesf[:, :], 1.0), w=["onesf"])
        S.op("dve", lambda e: e.memset(bdiag[:, :], 0.0), w=["bdiag"])
        S.op("dve", lambda e: e.memset(bdiag[0:64, 0:64], 1.0), w=["bdiag"])
        S.op("dve", lambda e: e.memset(bdiag[64:128, 64:128], 1.0), w=["bdiag"])
        S.op("dve", lambda e: e.memset(savekv[:, :, :], 0.0), w=["savekv"])
        for (dst, src) in ((gmix, norm_mix), (gffn, norm_ffn), (gmem, norm_mem)):
            for l in range(L):
                DMA("sp", dst[:, l, :], src[l].rearrange("(kc p) -> p kc", p=128), r=(), w=["gains"],
                    allow_slow_non_contiguous=True)
        for l in range(L):
            for hf in range(2):
                DMA("sp", gqb[hf * 64:(hf + 1) * 64, l:l + 1], q_gain_b[l].rearrange("(d o) -> d o", o=1), r=(), w=["gains"])
                DMA("sp", gkb[hf * 64:(hf + 1) * 64, l:l + 1], k_gain_b[l].rearrange("(d o) -> d o", o=1), r=(), w=["gains"])
                DMA("sp", esink[hf * 64:(hf + 1) * 64, l, :],
                    sinks[l].rearrange("(c two) -> two c", two=2)[hf].partition_broadcast(64), r=(), w=["esink"],
                    allow_slow_non_contiguous=True)
            DMA("sp", gqc[:, l, :], q_gain_c[l].rearrange("(dc p) -> p dc", p=128), r=(), w=["gains"], allow_slow_non_contiguous=True)
            DMA("sp", gkc[:, l, :], k_gain_c[l].rearrange("(dc p) -> p dc", p=128), r=(), w=["gains"], allow_slow_non_contiguous=True)
        ACTF(esink[:, :, :], esink[:, :, :], AF.Exp, r=["esink"], w=["esink"])
        wsp = SCf[:, 128:128 + 1024].rearrange("p (g s) -> p g s", s=128)
        for l in range(L):
            DMA("sp", wsp, w_spatial[l].rearrange("g t s -> t g s"), r=(), w=["wsp", "SC"])
            TT_("dve", wsp, wsp, SCf[:, 0:128].unsqueeze(1).to_broadcast([128, 8, 128]), ALU.mult, r=["tril", "wsp"], w=["wsp", "SC"])
            for g4 in range(2):
                b = bank()
                for j in range(4):
                    g = g4 * 4 + j
                    TR(ps[b][:, j * 128:(j + 1) * 128], wsp[:, g, :], identf[:, :], r=["wsp", "SC", "identf"], w=[("ps", b)])
                CP("dve", WmT[:, l, g4 * 4:(g4 + 1) * 4, :], ps[b][:, :].rearrange("p (j t) -> p j t", t=128),
                   r=[("ps", b)], w=["WmT"])
        FENCE("SC")

        CK("params")
        def load_tokens_T(src_rows, nblk, dstT, dkey):
            for blk in range(nblk):
                st = stage[blk % 4]
                skey = ("stage", blk % 4)
                DMA("sp", st, src_rows[blk * 128:(blk + 1) * 128, :], r=["Y"], w=[skey])
                for k4 in range(4):
                    b = bank()
                    for j in range(4):
                        kc = k4 * 4 + j
                        TR(ps[b][:, j * 128:(j + 1) * 128], st[:, kc * 128:(kc + 1) * 128], identf[:, :],
                           r=[skey, "identf"], w=[("ps", b)])
                    CP("act" if k4 % 2 else "dve", dstT[:, k4 * 4:(k4 + 1) * 4, blk * 128:(blk + 1) * 128],
                       ps[b][:, :].rearrange("p (j t) -> p j t", t=128), r=[("ps", b)], w=[(dkey, blk * 128 // HT)])

        def rms_to_h(srcT, skey, gain_ap, halves, hw, ranges=None):
            rstd = SCf[:, 0:TT]
            sqr = [SCb[:, 2 * TT + i * HT:2 * TT + (i + 1) * HT] for i in range(3)]
            if ranges is None:
                ranges = [(hf * hw, hw) for hf in range(halves)]
            for hf in range(len(ranges)):
                st_, hw = ranges[hf]
                sl = slice(st_, st_ + hw)
                b = bank()
                for kc in range(NKC):
                    sq = sqr[kc % 3]
                    ACTF(sq[:, 0:hw], srcT[:, kc, sl], AF.Square, r=[(skey, hf)], w=[("sq", kc % 3), "SC"])
                    MM(ps[b][:, 0:hw], onesb[:, :], sq[:, 0:hw], kc == 0, kc == NKC - 1, r=[("sq", kc % 3), "onesb", "SC"], w=[("ps", b)])
                ACTF(rstd[:, sl], ps[b][:, 0:hw], AF.Sqrt, r=[("ps", b)], w=[("rstd", hf), "SC"], scale=1.0 / D, bias=EPS)
                RECIP(rstd[:, sl], rstd[:, sl], r=[("rstd", hf)], w=[("rstd", hf), "SC"])
                for kc in range(NKC):
                    STT(hT[:, kc, sl], srcT[:, kc, sl], gain_ap[:, kc:kc + 1], rstd[:, sl], ALU.mult, ALU.mult,
                        r=[(skey, hf), ("rstd", hf), "gains", "SC"], w=[("hT", hf)])
            FENCE("SC")

        deferred = []
        out_keys = []

        def defer(fn):
            deferred.append(fn)

        def flush(keep=0):
            while len(deferred) > keep:
                deferred.pop(0)()

        def fm_unit(wv, wkey, nchunks, ksz, rhsT, rkey, halves, hw, consumer, hf_outer=False, ranges=None):
            if ranges is None:
                ranges = [(hf * hw, hw) for hf in range(halves)]
            nh = len(ranges)
            order = [(c, hf) for hf in range(nh) for c in range(nchunks)] if hf_outer else \
                    [(c, hf) for c in range(nchunks) for hf in range(nh)]
            for (c, hf) in order:
                st_, w_ = ranges[hf]
                b = bank()
                for k in range(ksz):
                    MM(ps[b][:, 0:w_], wv.row(k)[:, c * 128:(c + 1) * 128], rhsT[:, k, st_:st_ + w_], k == 0, k == ksz - 1,
                       r=wv.keys(k) + [(rkey, hf)], w=[("ps", b)])
                consumer(b, c, hf)

        def qk_norm_consumer(nsum, ones_ap, okey, invd, gain_fn, dst_fn, dkey, hw, scbase, ranges=None):
            st = {}
            cnt = [0]

            def consumer(b, c, hf):
                i = cnt[0]
                cnt[0] += 1
                slot = i % 4
                if any(slot in getattr(f, "slots", ()) for f in deferred):
                    flush()
                w_ = hw if ranges is None else ranges[hf][1]
                sqb = SCb[:, scbase + slot * hw: scbase + slot * hw + w_]
                qraw = SCf[:, (scbase + 4 * hw) // 2 + slot * hw:(scbase + 4 * hw) // 2 + slot * hw + w_]
                ACTF(sqb, ps[b][:, 0:w_], AF.Square, r=[("ps", b)], w=[("sqb", slot), "SC"])
                CP("dve", qraw, ps[b][:, 0:w_], r=[("ps", b)], w=[("qraw", slot), "SC"])
                st.setdefault(hf, []).append((c, slot))
                if len(st[hf]) == nsum:
                    group = st.pop(hf)

                    def fin(group=group, hf=hf, w_=w_):
                        b2 = bank()
                        for j, (c_, s_) in enumerate(group):
                            MM(ps[b2][:, 0:w_], ones_ap, SCb[:, scbase + s_ * hw: scbase + s_ * hw + w_], j == 0, j == len(group) - 1,
                               r=[("sqb", s_), okey, "SC"], w=[("ps", b2)])
                        ro = (scbase + 12 * hw) // 2 + (group[0][1] % 2) * hw
                        rq = SCf[:, ro:ro + w_]
                        rk = ("rq", group[0][1] % 2)
                        ACTF(rq, ps[b2][:, 0:w_], AF.Sqrt, r=[("ps", b2)], w=[rk, "SC"], scale=invd, bias=EPS)
                        RECIP(rq, rq, r=[rk], w=[rk, "SC"])
                        for (c_, s_) in group:
                            qo = (scbase + 4 * hw) // 2 + s_ * hw
                            qr = SCf[:, qo:qo + w_]
                            STT(dst_fn(c_, hf), qr, gain_fn(c_), rq, ALU.mult, ALU.mult, r=[("qraw", s_), rk, "gains", "SC"], w=["R", dkey])
                    fin.slots = tuple(s_ for (_, s_) in group)
                    defer(fin)
                    flush(keep=1)
            return consumer

        def mem_prologue():
            load_tokens_T(mem_in, 2, xT, "xT")
            CK("mp_load")
            for l in range(L):
                rms_to_h(xT, "xT", gmem[:, l, :], 1, 256)
                CK("mp_rms")
                wkv = w_mem_kv[l].rearrange("(kc p) n -> p kc n", p=128)
                for u in range(2):
                    wv, wk = wload(wkv[:, :, u * 512:(u + 1) * 512], NKC, 512)
                    cons = qk_norm_consumer(2, onesb[:, :], "onesb", 1.0 / 256.0,
                                            lambda c, l=l: gkc[:, l, (c % 2):(c % 2) + 1],
                                            lambda c, hf, u=u: kcT[:, u * 4 + c, :], "kvc", 256, 0)
                    fm_unit(wv, wk, 4, NKC, hT, "hT", 1, 256, cons)
                flush()
                CK("mp_k")
                for u in range(2):
                    wv, wk = wload(wkv[:, :, 1024 + u * 512:1024 + (u + 1) * 512], NKC, 512)
                    for mb_ in range(2):
                        b = bank()
                        for k in range(NKC):
                            MM(ps[b][:, :], hT[:, k, mb_ * 128:(mb_ + 1) * 128], wv.row(k), k == 0, k == NKC - 1,
                               r=[wv.key(k), ("hT", 0)], w=[("ps", b)])
                        CP("act", vc[:, mb_, u * 512:(u + 1) * 512], ps[b][:, :], r=[("ps", b)], w=["kvc"])
                CK("mp_v")
                DMA("sp", kvscr[l], kvc[:, :], r=["kvc", "R"], w=[("kvscr", l)])
                FENCE("kvc")
                FENCE("SC")

        def mixer(ti, l, first_layer_in_launch, last_layer_in_launch):
            tok0 = ti * TT
            skf = (2 if last_layer_in_launch else 1) if ti == 0 else 0
            skk = max(skf - 1, 0)
            FR = [(skf * 128, HT - skf * 128), (HT, HT)]
            KR = [(skk * 128, HT - skk * 128), (HT, HT)]
            S.op("sp", lambda e: e.dma_start(out=kvc[:, :], in_=kvscr[l]), r=[("kvscr", l)], w=["kvc", "kvcR"], dma=dsem(), fence=True)
            if first_layer_in_launch:
                load_tokens_T(x_in[tok0:tok0 + TT, :], NB, xT, "xT")
                FENCE("Y")
            CK("load")
            rms_to_h(xT, "xT", gmix[:, l, :], 2, HT, ranges=KR)
            if ti == 0 and l == layers[0]:
                TAP("hT", hT[:, :, :], r=[("hT", 0), ("hT", 1)])
            CK("rms")

            vgrep = SCf[:, 0:1024]
            brow = SCf[0:1, 1024:2048]
            DMA("sp", vgrep, v_gain[l].rearrange("g c -> (g c)").partition_broadcast(128), r=(), w=["SC", "vgrep"])
            DMA("sp", brow, b_spatial[l].rearrange("g t -> (g t)").unsqueeze(0), r=(), w=["SC", "brow"])

            CK("A_u")
            vg = [SCf[:, 2048 + i * 512:2048 + (i + 1) * 512] for i in range(2)]
            junk = SCb[:, 6144 + 1024:6144 + 1024 + 128]
            ssv = rt[:, 88:96]
            for u in range(2):
                wv, wk = wload(w_in_unit(l, 1024 + u * 512), NKC, 512)
                for blk in range(skf, NB):
                    b = bank()
                    for k in range(NKC):
                        MM(ps[b][:, :], hT[:, k, blk * 128:(blk + 1) * 128], wv.row(k), k == 0, k == NKC - 1,
                           r=[wv.key(k), ("hT", blk // 3)], w=[("ps", b)])
                    i = (u * NB + blk) % 2
                    ACTF(vg[i], ps[b][:, :], AF.Gelu, r=[("ps", b)], w=[("vg", i), "SC"])
                    for g in range(4):
                        ACTF(junk, vg[i][:, g * 128:(g + 1) * 128], AF.Square, r=[("vg", i), "SC"], w=["junk", ("ssv", i)],
                             accum_out=ssv[:, i * 4 + g:i * 4 + g + 1])
                    ACTF(ssv[:, i * 4:(i + 1) * 4], ssv[:, i * 4:(i + 1) * 4], AF.Sqrt, r=[("ssv", i)], w=[("ssv", i)],
                         scale=1.0 / 128.0, bias=EPS)
                    RECIP(ssv[:, i * 4:(i + 1) * 4], ssv[:, i * 4:(i + 1) * 4], r=[("ssv", i)], w=[("ssv", i)])
                    for g in range(4):
                        gg = u * 4 + g
                        STT(vn[:, blk, gg * 128:(gg + 1) * 128], vg[i][:, g * 128:(g + 1) * 128], ssv[:, i * 4 + g:i * 4 + g + 1],
                            vgrep[:, gg * 128:(gg + 1) * 128], ALU.mult, ALU.mult,
                            r=[("vg", i), ("ssv", i), "vgrep", "SC"], w=["R", ("vn", blk // 3)])
            def u_cons(u):
                def cons(b, c, hf):
                    st_, w_ = FR[hf]
                    ACTF(yA[:, u * 4 + c, st_:st_ + w_], ps[b][:, 0:w_], AF.Gelu, r=[("ps", b)], w=["Y", ("yA", hf)])
                return cons
            for u in range(2):
                wv, wk = wload(w_in_unit(l, u * 512), NKC, 512)
                fm_unit(wv, wk, 4, NKC, hT, "hT", 2, HT, u_cons(u), ranges=FR)
            if ti == 0 and l == layers[0]:
                TAP("WmT", WmT[:, :, :, :], r=["WmT"])
                TAP("uT", yA, r=[("yA", 0), ("yA", 1)])
                TAP("vn", vn, r=[("vn", 0), ("vn", 1)])
            CK("A_v")
            for g in range(8):
                for hf in range(2):
                    st_, w_ = FR[hf]
                    js = [j for j in range(3) if hf * 3 + j >= skf]
                    b = bank()
                    for j in js:
                        MM(ps[b][:, j * 128:(j + 1) * 128], onesf[0:1, :], brow[:, g * 128:(g + 1) * 128], j == js[0], False,
                           r=["onesf", "brow", "SC"], w=[("ps", b)])
                    for j in js:
                        blk = hf * 3 + j
                        MM(ps[b][:, j * 128:(j + 1) * 128], vn[:, blk, g * 128:(g + 1) * 128], WmT[:, l, g, :], False, j == js[-1],
                           r=[("vn", hf), "R", "WmT"], w=[("ps", b)])
                    TT_("dve", yA[:, g, st_:st_ + w_], yA[:, g, st_:st_ + w_], ps[b][:, st_ - hf * HT:st_ - hf * HT + w_], ALU.mult,
                        r=[("ps", b), ("yA", hf)], w=["Y", ("yA", hf)])
            if ti == 0 and l == layers[0]:
                TAP("yA", yA, r=[("yA", 0), ("yA", 1)])
            FENCE("R")
            FENCE("SC")

            CK("A")
            CP("dve", kn2[:, :, 0:128], savekv[:, l, 0:512].rearrange("p (k t) -> p k t", t=128), r=["savekv"], w=["R", "kn2"])
            CP("dve", vb[:, 0, :], savekv[:, l, 512:768], r=["savekv"], w=["R", "vb"])
            for u in range(2):
                i = wslot()
                wview = wbuf[i][:, 0:NKC * 256].rearrange("p (k c) -> p k c", c=256)
                wk = ("w", i)
                src = w_in[l].rearrange("(kc p) n -> p kc n", p=128)
                pkeys = [wk]
                for kv in range(2):
                    for dup in range(2):
                        c0 = 3072 + (u * 2 + kv) * 64
                        pj = kv * 2 + dup
                        pkey = ("wkp", i, pj)
                        pkeys.append(pkey)
                        DMA("pool", wview[:, :, kv * 128 + dup * 64:kv * 128 + (dup + 1) * 64], src[:, :, c0:c0 + 64],
                            r=(), w=([wk, pkey] if pj == 0 else [pkey]), sem=("w%d" % i) if pj == 0 else "wk%d" % pj)
                wv = WUnit([(wview, pkeys)], NKC)
                cons = qk_norm_consumer(1, bdiag[:, :], "bdiag", 1.0 / 64.0, lambda c, l=l: gkb[:, l:l + 1],
                                        lambda c, hf, u=u: kn2[:, u * 2 + c, 128 + KR[hf][0]:128 + KR[hf][0] + KR[hf][1]], "kn2", HT, 0,
                                        ranges=KR)
                fm_unit(wv, wk, 2, NKC, hT, "hT", 2, HT, cons, ranges=KR)
            flush()
            CK("B_k")
            wv, wk = wload(w_in_unit(l, 3328, 256), NKC, 256)
            for blk in range(skk, NB):
                b = bank()
                for k in range(NKC):
                    MM(ps[b][:, 0:256], hT[:, k, blk * 128:(blk + 1) * 128], wv.row(k), k == 0, k == NKC - 1,
                       r=[wv.key(k), ("hT", blk // 3)], w=[("ps", b)])
                CP("act", vb[:, 1 + blk, :], ps[b][:, 0:256], r=[("ps", b)], w=["R", "vb"])
            CP("dve", savekv[:, l, 0:512].rearrange("p (k t) -> p k t", t=128), kn2[:, :, NB * 128:(NB + 1) * 128], r=["kn2", "R"], w=["savekv"])
            CP("dve", savekv[:, l, 512:768], vb[:, NB, :], r=["vb", "R"], w=["savekv"])
            CK("B_v")
            for u in range(2):
                wv, wk = wload(w_in_unit(l, 2048 + u * 512), NKC, 512)
                cons = qk_norm_consumer(1, bdiag[:, :], "bdiag", 1.0 / 64.0, lambda c, l=l: gqb[:, l:l + 1],
                                        lambda c, hf, u=u: qbuf[:, u * 4 + c, FR[hf][0]:FR[hf][0] + FR[hf][1]], "qbuf", HT, 0, ranges=FR)
                fm_unit(wv, wk, 4, NKC, hT, "hT", 2, HT, cons, ranges=FR)
            flush()
            if ti == 0 and l == layers[0]:
                TAP("qn", qbuf, r=["qbuf"])
                TAP("kn2", kn2, r=["kn2"])
            CK("B_q")
            PB = 6144
            Pbuf = [[SCb[:, PB + (i * 2 + j) * 512:PB + (i * 2 + j + 1) * 512] for j in range(2)] for i in range(2)]
            dnb = [SCf[:, 4096 + i * 256:4096 + (i + 1) * 256] for i in range(2)]
            step = [0]
            for blk in range(skf, NB):
                for kvh in range(4):
                    i = step[0] % 2
                    step[0] += 1
                    gblk = ti * NB + blk
                    if gblk == 0:
                        mprev = 3
                    elif gblk == 2:
                        mprev = 2
                    else:
                        mprev = 1
                    for par in range(2):
                        b = bank()
                        MM(ps[b][:, 0:256], identb[:, :], mbt[:, 0, :].unsqueeze(1).to_broadcast([128, 2, 128]), True, False,
                           r=["identb", "mbt"], w=[("ps", b)])
                        MM(ps[b][:, 256:512], identb[:, :], mbt[:, mprev, :].unsqueeze(1).to_broadcast([128, 2, 128]), False, False,
                           r=["identb", "mbt"], w=[("ps", b)])
                        for which, slot in enumerate((1 + blk, blk)):
                            MM(ps[b][:, which * 256:(which + 1) * 256], kn2[par * 64:(par + 1) * 64, kvh, slot * 128:(slot + 1) * 128],
                               qbuf[par * 64:(par + 1) * 64, 2 * kvh:2 * kvh + 2, blk * 128:(blk + 1) * 128], False, which == 1,
                               r=["kn2", "qbuf", "R"], w=[("ps", b)])
                        ACTF(Pbuf[i][par], ps[b][:, :], AF.Exp, r=[("ps", b)], w=[("P", i, par), "SC"], scale=0.125)

                    def pv(i=i, blk=blk, kvh=kvh):
                        b = bank()
                        for par in range(2):
                            for which, slot in ((1, blk), (0, 1 + blk)):
                                MM(ps[b][par * 64:(par + 1) * 64, 0:256], vb[:, slot, kvh * 64:(kvh + 1) * 64],
                                   Pbuf[i][par][:, which * 256:(which + 1) * 256], which == 1, which == 0,
                                   r=["vb", "R", ("P", i, par), "SC"], w=[("ps", b)])
                            for which in (1, 0):
                                MM(ps[b][par * 64:(par + 1) * 64, 256:512], onesb[:, 0:64],
                                   Pbuf[i][par][:, which * 256:(which + 1) * 256], which == 1, which == 0,
                                   r=["onesb", ("P", i, par), "SC"], w=[("ps", b)])
                        for j in range(2):
                            TS("dve", dnb[i][:, j * 128:(j + 1) * 128], ps[b][:, 256 + j * 128:256 + (j + 1) * 128],
                               esink[:, l, 2 * kvh + j:2 * kvh + j + 1], None, ALU.add, None, r=[("ps", b), "esink"], w=[("dn", i), "SC"])
                        RECIP(dnb[i], dnb[i], r=[("dn", i)], w=[("dn", i), "SC"])
                        TT_("dve", yB[:, 2 * kvh:2 * kvh + 2, blk * 128:(blk + 1) * 128],
                            ps[b][:, 0:256].rearrange("p (j q) -> p j q", q=128), dnb[i].rearrange("p (j q) -> p j q", q=128), ALU.mult,
                            r=[("ps", b), ("dn", i), "SC"], w=["Y", ("yB", blk // 3)])
                    defer(pv)
                    flush(keep=1)
            flush()
            if ti == 0 and l == layers[0]:
                TAP("yB", yB, r=[("yB", 0), ("yB", 1)])
            FENCE("SC")

            CK("B")
            Pm = [[SCb[:, PB + (i * 2 + j) * HT:PB + (i * 2 + j + 1) * HT] for j in range(2)] for i in range(2)]
            rdb = [SCf[:, 4096 + i * HT:4096 + (i + 1) * HT] for i in range(1)]
            for u in range(2):
                wv, wk = wload(w_in_unit(l, 3584 + u * 512), NKC, 512)
                cons = qk_norm_consumer(2, onesb[:, :], "onesb", 1.0 / 256.0, lambda c, l=l: gqc[:, l, (c % 2):(c % 2) + 1],
                                        lambda c, hf: qbuf[:, c, FR[hf][0]:FR[hf][0] + FR[hf][1]], "qbuf", HT, 0, ranges=FR)
                fm_unit(wv, wk, 4, NKC, hT, "hT", 2, HT, cons, ranges=FR)
                flush()
                for hl in range(2):
                    hc = u * 2 + hl
                    for hf in range(2):
                        i = step[0] % 2
                        step[0] += 1
                        st_, w_ = FR[hf]
                        for m in range(2):
                            b = bank()
                            for dc in range(2):
                                MM(ps[b][:, 0:w_], kcT[:, 2 * hc + dc, m * 128:(m + 1) * 128], qbuf[:, 2 * hl + dc, st_:st_ + w_],
                                   dc == 0, dc == 1, r=["kvc", "kvcR", "qbuf", "R"], w=[("ps", b)])
                            ACTF(Pm[i][m][:, 0:w_], ps[b][:, 0:w_], AF.Exp, r=[("ps", b)], w=[("P", i, m), "SC"], scale=1.0 / 16.0)

                        def pvc(i=i, hc=hc, hf=hf, st_=st_, w_=w_):
                            bd = bank()
                            for m in range(2):
                                MM(ps[bd][:, 0:w_], onesb[:, :], Pm[i][m][:, 0:w_], m == 0, m == 1, r=["onesb", ("P", i, m), "SC"], w=[("ps", bd)])
                            RECIP(rdb[0][:, 0:w_], ps[bd][:, 0:w_], r=[("ps", bd)], w=[("rd", 0), "SC"])
                            for dc in range(2):
                                b = bank()
                                for m in range(2):
                                    MM(ps[b][:, 0:w_], vc[:, m, hc * 256 + dc * 128:hc * 256 + (dc + 1) * 128], Pm[i][m][:, 0:w_], m == 0, m == 1,
                                       r=["kvc", "kvcR", ("P", i, m), "SC"], w=[("ps", b)])
                                TT_("dve", yC[:, 2 * hc + dc, st_:st_ + w_], ps[b][:, 0:w_], rdb[0][:, 0:w_], ALU.mult,
                                    r=[("ps", b), ("rd", 0), "SC"], w=["Y", ("yC", hf)])
                        defer(pvc)
                        flush(keep=1)
                flush()
            if ti == 0 and l == layers[0]:
                TAP("yC", yC, r=[("yC", 0), ("yC", 1)])
            FENCE("R")
            FENCE("SC")

            CK("C")
            FENCE("kvcR")
            pacc = SCf[:, 0:4 * TT].rearrange("p (c t) -> p c t", t=TT)
            sgA = kvcf[:, 0:2 * TT].rearrange("p (c t) -> p c t", t=TT)
            sgB = SCf[:, 4 * TT:6 * TT].rearrange("p (c t) -> p c t", t=TT)
            tb = [SCf[:, 6 * TT + i * HT:6 * TT + (i + 1) * HT] for i in range(2)]
            ys = (yA, yB, yC)
            ykeys = ("yA", "yB", "yC")
            mstep = [0]

            def sg_ap(dcl, sl):
                return sgA[:, dcl, sl] if dcl < 2 else sgB[:, dcl - 2, sl]

            for d4 in range(4):
                for n in range(3):
                    wgv, wgk = wload(w_in_unit(l, 4608 + n * D + d4 * 512), NKC, 512)

                    def gcons(b_, c, hf):
                        st_, w_ = FR[hf]
                        sl = slice(st_, st_ + w_)
                        ACTF(sg_ap(c, sl), ps[b_][:, 0:w_], AF.Sigmoid, r=[("ps", b_)], w=[("sg", c, hf), "SC", "kvcR"])
                    fm_unit(wgv, wgk, 4, NKC, hT, "hT", 2, HT, gcons, ranges=FR)
                    wbv, wbk = wload(w_branch[l, n].rearrange("(wc p) d -> p wc d", p=128)[:, :, d4 * 512:(d4 + 1) * 512], 8, 512)

                    def wcons(b_, c, hf, n=n, d4=d4):
                        st_, w_ = FR[hf]
                        sl = slice(st_, st_ + w_)
                        i = mstep[0] % 2
                        mstep[0] += 1
                        pk = ("pacc", c, hf)
                        if n == 0:
                            TT_("dve", pacc[:, c, sl], ps[b_][:, 0:w_], sg_ap(c, sl), ALU.mult,
                                r=[("ps", b_), ("sg", c, hf), "kvcR"], w=[pk, "SC"])
                        else:
                            TT_("dve", tb[i][:, 0:w_], ps[b_][:, 0:w_], sg_ap(c, sl), ALU.mult,
                                r=[("ps", b_), ("sg", c, hf), "kvcR"], w=[("tb", i), "SC"])
                            if n == 1:
                                TT_("dve", pacc[:, c, sl], pacc[:, c, sl], tb[i][:, 0:w_], ALU.add, r=[pk, ("tb", i)], w=[pk, "SC"])
                            else:
                                TT_("dve", mT[:, d4 * 4 + c, sl], pacc[:, c, sl], tb[i][:, 0:w_], ALU.add, r=[pk, ("tb", i), "SC"],
                                    w=["R", ("mT", hf)])
                    fm_unit(wbv, wbk, 4, 8, ys[n], ykeys[n], 2, HT, wcons, ranges=FR)
            if ti == 0 and l == layers[0]:
                TAP("mT", mT, r=[("mT", 0), ("mT", 1)])
            CK("merge")
            for d4 in range(4):
                wv, wk = wload(w_out[l].rearrange("(kc p) n -> p kc n", p=128)[:, :, d4 * 512:(d4 + 1) * 512], NKC, 512)

                def ocons(b, c, hf, d4=d4):
                    dc = d4 * 4 + c
                    st_, w_ = FR[hf]
                    sl = slice(st_, st_ + w_)
                    TT_("dve", xT[:, dc, sl], xT[:, dc, sl], ps[b][:, 0:w_], ALU.add, r=[("ps", b), ("xT", hf)], w=[("xT", hf)])
                fm_unit(wv, wk, 4, NKC, mT, "mT", 2, HT, ocons, ranges=FR)
            if ti == 0 and l == layers[0]:
                TAP("x1", xT[:, :, :], r=[("xT", 0), ("xT", 1)])
            FENCE("R")
            FENCE("Y")
            FENCE("SC")

        def moe(ti, l, last_layer_in_launch):
            CK("mixer")
            skipb = (2 if last_layer_in_launch else 1) if ti == 0 else 0
            MR = [(skipb * 128, HT - skipb * 128), (HT, HT)]
            DMA("pool", wr[:, :, 0:4], w_rg[l].rearrange("(kc p) g -> p kc g", p=128), r=(), w=["wr"], sem="wr",
                allow_slow_non_contiguous=True)
            DMA("pool", wr[:, :, 4:20], w_re[l].rearrange("(kc p) g -> p kc g", p=128), r=(), w=["wr"], sem="wr",
                allow_slow_non_contiguous=True)
            DMA("sp", rb[:, 0:4], b_rg[l].partition_broadcast(128), r=(), w=["rb"])
            DMA("sp", rb[:, 4:20], b_re[l].partition_broadcast(128), r=(), w=["rb"])
            rms_to_h(xT, "xT", gffn[:, l, :], 2, HT, ranges=MR)
            for blk in range(skipb, NB):
                b = bank()
                for k in range(NKC):
                    MM(ps[b][:, 0:20], hT[:, k, blk * 128:(blk + 1) * 128], wr[:, k, :], k == 0, k == NKC - 1,
                       r=["wr", ("hT", blk // 3)], w=[("ps", b)])
                lg = rt[:, 0:20]
                gl = rt[:, 0:4]
                el = rt[:, 4:20]
                gmax = rt[:, 20:21]
                ngmax = rt[:, 21:22]
                oh = rt[:, 22:26]
                sumg = rt[:, 26:27]
                pen = rt[:, 27:31]
                m1 = rt[:, 31:32]
                m2 = rt[:, 32:33]
                dd = rt[:, 33:34]
                w1 = rt[:, 34:35]
                w2 = rt[:, 35:36]
                junk4 = rt[:, 36:40]
                em = rt[:, 40:56]
                oh1 = rt[:, 56:72]
                em2 = rt[:, 72:88]
                RT = ["rt"]
                TT_("dve", lg, ps[b][:, 0:20], rb[:, :], ALU.add, r=[("ps", b), "rb"], w=RT)
                RED(gmax, gl, ALU.max, r=RT, w=RT)
                TS("dve", oh, gl, gmax, None, ALU.is_equal, None, r=RT, w=RT)
                TS("dve", ngmax, gmax, -1.0, None, ALU.mult, None, r=RT, w=RT)
                ACTF(junk4, gl, AF.Exp, r=RT, w=RT, bias=ngmax, accum_out=sumg)
                TS("dve", pen, oh, -1.0, 1e9, ALU.add, ALU.mult, r=RT, w=RT)
                TT_("dve", em.rearrange("p (g j) -> p g j", j=4), el.rearrange("p (g j) -> p g j", j=4),
                    pen.unsqueeze(2).to_broadcast([128, 4, 4]), ALU.add, r=RT, w=RT)
                RED(m1, em, ALU.max, r=RT, w=RT)
                TS("dve", oh1, em, m1, None, ALU.is_equal, None, r=RT, w=RT)
                STT(em2, oh1, -1e9, em, ALU.mult, ALU.add, r=RT, w=RT)
                RED(m2, em2, ALU.max, r=RT, w=RT)
                TS("dve", em2, em2, m2, None, ALU.is_equal, None, r=RT, w=RT)
                TT_("dve", dd, m2, m1, ALU.subtract, r=RT, w=RT)
                ACTF(dd, dd, AF.Exp, r=RT, w=RT)
                TS("dve", w1, dd, 1.0, None, ALU.add, None, r=RT, w=RT)
                TT_("dve", w1, w1, sumg, ALU.mult, r=RT, w=RT)
                RECIP(w1, w1, r=RT, w=RT)
                TT_("dve", w2, w1, dd, ALU.mult, r=RT, w=RT)
                TS("dve", oh1, oh1, w1, None, ALU.mult, None, r=RT, w=RT)
                STT(gates[:, blk, :], em2, w2, oh1, ALU.mult, ALU.add, r=RT, w=["gates"])
            if ti == 0 and l == layers[0]:
                TAP("gates", gates[:, :, :], r=["gates"])
            CK("router")
            De = [SCf[:, i * 128:(i + 1) * 128] for i in range(3)]
            rep = SCf[:, 384:384 + TT]
            sgl = SCf[:, 1152:1152 + 4 * TT].rearrange("p (c t) -> p c t", t=TT)
            t1 = [SCf[:, 4224 + i * HT:4224 + (i + 1) * HT] for i in range(2)]
            dctr = [0]
            estep = [0]
            for G in range(4):
                for el_ in range(4):
                    e_ = G * 4 + el_
                    wgv, wgk = wload(w_gate_e[l, e_].rearrange("(kc p) f -> p kc f", p=128), NKC, 512)

                    def gcons(b_, c, hf):
                        st_, w_ = MR[hf]
                        ACTF(sgl[:, c, st_:st_ + w_], ps[b_][:, 0:w_], AF.Silu, r=[("ps", b_)], w=[("sgl", c, hf), "SC"])
                    fm_unit(wgv, wgk, 4, NKC, hT, "hT", 2, HT, gcons, hf_outer=(e_ == 0), ranges=MR)
                    for hf in range(2):
                        st_, w_ = MR[hf]
                        b = bank()
                        for j in range(3):
                            blk = hf * 3 + j
                            if blk < skipb:
                                continue
                            di = dctr[0] % 3
                            dctr[0] += 1
                            TS("dve", De[di], identf[:, :], gates[:, blk, e_:e_ + 1], None, ALU.mult, None,
                               r=["identf", "gates"], w=[("De", di), "SC"])
                            MM(ps[b][:, j * 128:(j + 1) * 128], onesf[:, :], De[di], True, True, r=["onesf", ("De", di), "SC"], w=[("ps", b)])
                        CP("act", rep[:, st_:st_ + w_], ps[b][:, st_ - hf * HT:st_ - hf * HT + w_], r=[("ps", b)], w=[("rep", hf), "SC"])
                    wuv, wuk = wload(w_up_e[l, e_].rearrange("(kc p) f -> p kc f", p=128), NKC, 512)

                    def ucons(b_, c, hf, el_=el_):
                        st_, w_ = MR[hf]
                        sl = slice(st_, st_ + w_)
                        i = estep[0] % 2
                        estep[0] += 1
                        TT_("dve", t1[i][:, 0:w_], ps[b_][:, 0:w_], sgl[:, c, sl], ALU.mult, r=[("ps", b_), ("sgl", c, hf)], w=[("t1", i), "SC"])
                        TT_("dve", hidT[:, el_ * 4 + c, sl], t1[i][:, 0:w_], rep[:, sl], ALU.mult,
                            r=[("t1", i), ("rep", hf), "SC"], w=["Y", ("hid", hf)])
                    fm_unit(wuv, wuk, 4, NKC, hT, "hT", 2, HT, ucons, ranges=MR)
                if ti == 0 and l == layers[0] and G == 0:
                    TAP("hid", hidT, r=[("hid", 0), ("hid", 1)])
                wd = w_down_e[l].rearrange("e f d -> (e f) d").rearrange("(k p) d -> p k d", p=128)
                for d4 in range(4):
                    wv, wk = wload(wd[:, G * 16:(G + 1) * 16, d4 * 512:(d4 + 1) * 512], 16, 512)

                    def dcons(b, c, hf, d4=d4):
                        dc = d4 * 4 + c
                        st_, w_ = MR[hf]
                        sl = slice(st_, st_ + w_)
                        TT_("dve", xT[:, dc, sl], xT[:, dc, sl], ps[b][:, 0:w_], ALU.add, r=[("ps", b), ("xT", hf)], w=[("xT", hf)])
                    fm_unit(wv, wk, 4, 16, hidT, "hid", 2, HT, dcons, ranges=MR)
            if ti == 0 and l == layers[0]:
                TAP("x2", xT[:, :, :], r=[("xT", 0), ("xT", 1)])
            FENCE("Y")
            FENCE("SC")
            CK("moe")
            if last_layer_in_launch:
                for blk in range(NB):
                    gtok = ti * TT + blk * 128
                    if gtok < out_tokens_from:
                        continue
                    st = stage[blk % 4]
                    skey = ("stage", blk % 4)
                    for k4 in range(4):
                        b = bank()
                        for j in range(4):
                            kc = k4 * 4 + j
                            TR(ps[b][:, j * 128:(j + 1) * 128], xT[:, kc, blk * 128:(blk + 1) * 128], identf[:, :],
                               r=[("xT", blk // 3), "identf"], w=[("ps", b)])
                        CP("act" if k4 % 2 else "dve", st[:, k4 * 512:(k4 + 1) * 512], ps[b][:, :], r=[("ps", b)], w=[skey, "Y"])
                    okey_ = ("out", len(out_keys))
                    out_keys.append(okey_)
                    DMA("sp", out[gtok - out_tokens_from:gtok - out_tokens_from + 128, :], st, r=[skey], w=[okey_])
                FENCE("Y")

        mem_prologue()
        CK("memprol")
        for ti in range(n_tiles):
            for li, l in enumerate(layers):
                mixer(ti, l, li == 0, li == len(layers) - 1)
                moe(ti, l, li == len(layers) - 1)
        fin_keys = out_keys + ["tap_" + k for k in tap_aps]
        S.op("sp", lambda e: None, r=fin_keys, w=(), force=True)

        semnames = list(Sched.ENGS) + ["w%d" % i for i in range(NW)] + ["d%d" % i for i in range(6)] + ["wr", "wk1", "wk2", "wk3"]
        sems = {n: es.enter_context(nc.semaphore("s_" + n)) for n in semnames}
        block = es.enter_context(nc.Block())
        S.emit(nc, block, sems)
    return nc


def _consts():
    ident = np.eye(128, dtype=np.float32)
    tril = np.tril(np.ones((128, 128), dtype=np.float32))
    j = np.arange(128)[:, None]
    i = np.arange(128)[None, :]
    cur = np.where(i >= j, 0.0, NEG).astype(np.float32)
    prev = np.where(i < j, 0.0, NEG).astype(np.float32)
    allm = np.full((128, 128), NEG, dtype=np.float32)
    return ident, tril, cur, prev, allm


WEIGHT_KEYS = ["norm_mix", "norm_mem", "norm_ffn", "w_in", "v_gain", "w_spatial", "b_spatial", "q_gain_b", "k_gain_b",
               "sinks", "q_gain_c", "k_gain_c", "w_mem_kv", "w_branch", "w_out", "w_router_group", "b_router_group",
               "w_router_expert", "b_router_expert", "w_gate_e", "w_up_e", "w_down_e"]

FUSED = True
_NC_CACHE = {}


def _get_nc(layers):
    key = tuple(layers)
    if key not in _NC_CACHE:
        _NC_CACHE[key] = build_program(list(layers))
    return _NC_CACHE[key]


def _in_maps(xfull, inputs, n_cores=8):
    ident, tril, cur, prev, allm = _consts()
    maps = []
    for c in range(n_cores):
        b, half = c // 2, c % 2
        xs = np.zeros((TOK, D), dtype=np.float32)
        if half == 0:
            xs[256:] = xfull[b, 0:2048]
            pm = allm
        else:
            xs[:] = xfull[b, 2048 - 256:4096]
            pm = prev
        m = {"x_in": xs, "mem_in": np.ascontiguousarray(inputs["mem"][b]),
             "c_ident": ident, "c_tril": tril,
             "c_mb": np.stack([cur, prev, pm, allm]).astype(ml_dtypes.bfloat16)}
        for k in WEIGHT_KEYS:
            m[k] = inputs[k]
        maps.append(m)
    return maps


def kernel(**inputs):
    inputs = {k: np.ascontiguousarray(np.asarray(v)) for k, v in inputs.items()}
    x = inputs["x"]
    launches = [[0, 1]] if FUSED else [[0], [1]]
    for layers in launches:
        nc = _get_nc(layers)
        res = run_bass_kernel_spmd(nc, _in_maps(x, inputs), core_ids=list(range(8)))
        xn = np.empty_like(x)
        for c in range(8):
            b, half = c // 2, c % 2
            xn[b, half * 2048:(half + 1) * 2048] = res.results[c]["out"]
        x = xn
    return x
```
